# Optimizing a Trainium2 kernel written in Bass

```python
import math
import jax
import jax.numpy as jnp
from jax import lax
import numpy as np

D_MODEL = 1024
BATCH = 8
SEQ = 4096
DEPTH = 2

GRID_W = 64
CTX_LEN = 256
NORM_EPS = 1e-6
DN_HEADS = 4
DN_DK = 128
DN_DV = 128
DN_CONV = 3
DN_CHUNK = 64
SC_WIDTH = 512
SC_CONV = 3
MLA_HEADS = 4
MLA_Q_LORA = 256
MLA_KV_LORA = 128
MLA_NOPE = 128
MLA_ROPE = 64
MLA_V = 128
MLA_SCALE = (MLA_NOPE + MLA_ROPE) ** -0.5
Q_BLOCK = 128
ROPE_BASE = 10000.0
N_EXPERTS = 32
TOP_K = 4
EXPERT_FF = 1024
SWIGLU_ALPHA = 1.702
SWIGLU_LIMIT = 7.0
EXPERT_BLOCK = 256
IN_WIDTHS = (DN_HEADS * DN_DK, DN_HEADS * DN_DK, DN_HEADS * DN_DV, DN_HEADS * DN_DV, 2 * DN_HEADS, 2 * DN_HEADS,
             SC_WIDTH, SC_WIDTH, SC_WIDTH, MLA_Q_LORA, MLA_KV_LORA + MLA_ROPE)
IN_COLS = sum(IN_WIDTHS)

kernel_name = "hybrid_gdn_shortconv_mla_moe_dit"

F32 = jnp.float32


def rms_norm(x, g):
    xf = x.astype(F32)
    y = xf * lax.rsqrt(jnp.mean(xf * xf, axis=-1, keepdims=True) + NORM_EPS)
    return (y * g.astype(F32)).astype(x.dtype)


def l2_norm(x):
    xf = x.astype(F32)
    return xf * lax.rsqrt(jnp.sum(xf * xf, axis=-1, keepdims=True) + 1e-6)


def dw_conv(x, w):
    k = w.shape[0]
    return lax.conv_general_dilated(x, w[:, None, :].astype(x.dtype), (1,), [(k // 2, k // 2)],
                                    dimension_numbers=('NWC', 'WIO', 'NWC'), feature_group_count=x.shape[-1])


def split_in_proj(p):
    idx = []
    s = 0
    for wd in IN_WIDTHS[:-1]:
        s += wd
        idx.append(s)
    return jnp.split(p, idx, axis=-1)


def axial_rope_tables(rows):
    row = jnp.repeat(jnp.arange(rows, dtype=F32), GRID_W)
    col = jnp.tile(jnp.arange(GRID_W, dtype=F32), rows)
    axis_dims = MLA_ROPE // 2
    inv = ROPE_BASE ** (-jnp.arange(0, axis_dims, 2, dtype=F32) / axis_dims)
    ang = jnp.stack([row[:, None] * inv, col[:, None] * inv], axis=1)
    return jnp.cos(ang), jnp.sin(ang)


def apply_axial_rope(x, cos, sin):
    xf = x.astype(F32)
    xa = xf.reshape(x.shape[:-1] + (2, MLA_ROPE // 2))
    bshape = (1, x.shape[1]) + (1,) * (x.ndim - 3) + (2, cos.shape[-1])
    c, s = cos.reshape(bshape), sin.reshape(bshape)
    x1, x2 = jnp.split(xa, 2, axis=-1)
    out = jnp.concatenate([x1 * c - x2 * s, x2 * c + x1 * s], axis=-1)
    return out.reshape(x.shape).astype(x.dtype)


def chunk_gated_delta(q, k, v, beta, g, s0):
    bsz, h, t, dk = q.shape
    dv = v.shape[-1]
    n = t // DN_CHUNK

    def chunks(a):
        return a.reshape((bsz, h, n, DN_CHUNK) + a.shape[3:])

    q = chunks(q.astype(F32) * dk ** -0.5)
    k = chunks(k.astype(F32))
    v = chunks(v.astype(F32))
    beta = chunks(beta.astype(F32))
    gc = jnp.cumsum(chunks(g.astype(F32)), axis=-1)
    causal = jnp.tril(jnp.ones((DN_CHUNK, DN_CHUNK), bool))
    strict = jnp.tril(jnp.ones((DN_CHUNK, DN_CHUNK), bool), -1)
    decay = jnp.exp(jnp.where(causal, gc[..., :, None] - gc[..., None, :], -jnp.inf))
    kb = k * beta[..., None]
    lower = jnp.where(strict, jnp.einsum('bhnid,bhnjd->bhnij', kb, k) * decay, 0.0)
    a_mat = lower + jnp.eye(DN_CHUNK, dtype=F32)
    rhs = jnp.concatenate([v * beta[..., None], kb * jnp.exp(gc)[..., None]], axis=-1)
    sol = lax.linalg.triangular_solve(a_mat, rhs, left_side=True, lower=True, unit_diagonal=True)
    u, w = sol[..., :dv], sol[..., dv:]
    intra = jnp.einsum('bhnid,bhnjd->bhnij', q, k) * decay
    q_dec = q * jnp.exp(gc)[..., None]
    k_dec = k * jnp.exp(gc[..., -1:] - gc)[..., None]
    g_tot = jnp.exp(gc[..., -1])

    def step(s, xs):
        q_c, k_c, u_c, w_c, a_c, gt = xs
        v_new = u_c - jnp.einsum('bhcd,bhde->bhce', w_c, s)
        o = jnp.einsum('bhcd,bhde->bhce', q_c, s) + jnp.einsum('bhij,bhje->bhie', a_c, v_new)
        s = s * gt[..., None, None] + jnp.einsum('bhcd,bhce->bhde', k_c, v_new)
        return s, o

    xs = tuple(jnp.moveaxis(a, 2, 0) for a in (q_dec, k_dec, u, w, intra, g_tot))
    s_fin, o = lax.scan(step, s0.astype(F32), xs)
    o = jnp.moveaxis(o, 0, 2).reshape(bsz, h, t, dv)
    return o, s_fin


def deltanet_inputs(q, k, v, a, b, conv_w, a_log, dt_bias):
    bsz, t, _ = q.shape
    qkv = jax.nn.silu(dw_conv(jnp.concatenate([q, k, v], axis=-1), conv_w))
    nqk = DN_HEADS * DN_DK
    q, k, v = jnp.split(qkv, [nqk, 2 * nqk], axis=-1)

    def heads(z, dim):
        return z.reshape(bsz, t, DN_HEADS, dim).transpose(0, 2, 1, 3)

    q = l2_norm(heads(q, DN_DK))
    k = l2_norm(heads(k, DN_DK))
    v = heads(v, DN_DV).astype(F32)
    a = a.astype(F32).reshape(bsz, t, 2, DN_HEADS)
    b = b.astype(F32).reshape(bsz, t, 2, DN_HEADS)
    beta = jax.nn.sigmoid(b).transpose(2, 0, 3, 1)
    g = (-jnp.exp(a_log.astype(F32))[None, None] * jax.nn.softplus(a + dt_bias.astype(F32))).transpose(2, 0, 3, 1)
    return q, k, v, beta, g


def bidirectional_deltanet(st_c, st_l):
    qc, kc, vc, bc, gc = st_c
    ql, kl, vl, bl, gl = st_l
    s0 = jnp.zeros((qc.shape[0], DN_HEADS, DN_DK, DN_DV), F32)
    outs_l = []
    outs_c = []
    for d in range(2):
        if d == 0:
            f = lambda a: a
        else:
            f = lambda a: jnp.flip(a, axis=2)
        oc, s_ctx = chunk_gated_delta(f(qc), f(kc), f(vc), f(bc[d]), f(gc[d]), s0)
        ol, _ = chunk_gated_delta(f(ql), f(kl), f(vl), f(bl[d]), f(gl[d]), s_ctx)
        outs_l.append(f(ol))
        outs_c.append(f(oc))
    return outs_l[0] + outs_l[1], outs_c[0] + outs_c[1]


def deltanet_output(o, z, norm_g):
    bsz, h, t, dv = o.shape
    o = o.transpose(0, 2, 1, 3)
    zz = z.reshape(bsz, t, h, dv)
    y = rms_norm(o, norm_g).astype(z.dtype) * jax.nn.silu(zz)
    return y.reshape(bsz, t, h * dv)


def short_conv_mixer(hh, b_gate, c_gate, conv_w):
    return b_gate * dw_conv(c_gate * hh, conv_w)


def mla_queries(qa, q_norm_g, w_qb, rope):
    bsz, t, _ = qa.shape
    q = (rms_norm(qa, q_norm_g) @ w_qb).reshape(bsz, t, MLA_HEADS, MLA_NOPE + MLA_ROPE)
    if rope is None:
        return q
    return jnp.concatenate([q[..., :MLA_NOPE], apply_axial_rope(q[..., MLA_NOPE:], *rope)], axis=-1)


def mla_keys(kva, kv_norm_g, w_kvb, rope):
    bsz, t, _ = kva.shape
    c_kv, k_rope = kva[..., :MLA_KV_LORA], kva[..., MLA_KV_LORA:]
    kv = (rms_norm(c_kv, kv_norm_g) @ w_kvb).reshape(bsz, t, MLA_HEADS, MLA_NOPE + MLA_V)
    if rope is not None:
        k_rope = apply_axial_rope(k_rope, *rope)
    k = jnp.concatenate([kv[..., :MLA_NOPE],
                         jnp.broadcast_to(k_rope[:, :, None, :], (bsz, t, MLA_HEADS, MLA_ROPE))], axis=-1)
    return k, kv[..., MLA_NOPE:]


def attend(q, k, v):
    s = jnp.einsum('bqhd,bkhd->bhqk', q, k, preferred_element_type=F32) * MLA_SCALE
    p = jax.nn.softmax(s, axis=-1).astype(v.dtype)
    return jnp.einsum('bhqk,bkhd->bqhd', p, v)


def mla_latent_attention(q_lat, k_all, v_all):
    bsz, t, h, dq = q_lat.shape
    nb = t // Q_BLOCK
    qb = jnp.moveaxis(q_lat.reshape(bsz, nb, Q_BLOCK, h, dq), 1, 0)
    out = lax.map(lambda qq: attend(qq, k_all, v_all), qb)
    return jnp.moveaxis(out, 0, 1).reshape(bsz, t, h * MLA_V)


def merge_branches(xm, y_dn, y_sc, y_mla, w_gate, b_gate, w_dn, w_sc, w_mla, w_out):
    gates = jax.nn.sigmoid(xm @ w_gate + b_gate)
    g_dn, g_sc, g_mla = jnp.split(gates, 3, axis=-1)
    merged = g_dn * (y_dn @ w_dn) + g_sc * (y_sc @ w_sc) + g_mla * (y_mla @ w_mla)
    return merged @ w_out


def mixer_sublayer(xm_lat, xm_ctx, rope, need_ctx, w_in, dn_conv_w, dn_a_log, dn_dt_bias, dn_norm_g, sc_conv_w,
                   mla_q_norm_g, mla_w_qb, mla_kv_norm_g, mla_w_kvb, w_branch_gate, b_branch_gate,
                   w_branch_dn, w_branch_sc, w_branch_mla, w_out):
    (dq_l, dk_l, dv_l, dz_l, da_l, db_l, sh_l, sb_l, scg_l, qa_l, kva_l) = split_in_proj(xm_lat @ w_in)
    (dq_c, dk_c, dv_c, dz_c, da_c, db_c, sh_c, sb_c, scg_c, qa_c, kva_c) = split_in_proj(xm_ctx @ w_in)
    st_l = deltanet_inputs(dq_l, dk_l, dv_l, da_l, db_l, dn_conv_w, dn_a_log, dn_dt_bias)
    st_c = deltanet_inputs(dq_c, dk_c, dv_c, da_c, db_c, dn_conv_w, dn_a_log, dn_dt_bias)
    o_l, o_c = bidirectional_deltanet(st_c, st_l)
    y_dn_l = deltanet_output(o_l, dz_l, dn_norm_g)
    y_sc_l = short_conv_mixer(sh_l, sb_l, scg_l, sc_conv_w)
    k_c, v_c = mla_keys(kva_c, mla_kv_norm_g, mla_w_kvb, None)
    k_l, v_l = mla_keys(kva_l, mla_kv_norm_g, mla_w_kvb, rope)
    q_l = mla_queries(qa_l, mla_q_norm_g, mla_w_qb, rope)
    y_mla_l = mla_latent_attention(q_l, jnp.concatenate([k_l, k_c], axis=1), jnp.concatenate([v_l, v_c], axis=1))
    y_lat = merge_branches(xm_lat, y_dn_l, y_sc_l, y_mla_l, w_branch_gate, b_branch_gate,
                           w_branch_dn, w_branch_sc, w_branch_mla, w_out)
    if not need_ctx:
        return y_lat, None
    bsz, tc, _ = xm_ctx.shape
    y_dn_c = deltanet_output(o_c, dz_c, dn_norm_g)
    y_sc_c = short_conv_mixer(sh_c, sb_c, scg_c, sc_conv_w)
    q_c = mla_queries(qa_c, mla_q_norm_g, mla_w_qb, None)
    y_mla_c = attend(q_c, k_c, v_c).reshape(bsz, tc, MLA_HEADS * MLA_V)
    y_ctx = merge_branches(xm_ctx, y_dn_c, y_sc_c, y_mla_c, w_branch_gate, b_branch_gate,
                           w_branch_dn, w_branch_sc, w_branch_mla, w_out)
    return y_lat, y_ctx


def moe_ffn(xt, router_w, router_b, w1, b1, w2, b2):
    n_tok, d = xt.shape
    logits = jnp.dot(xt, router_w, preferred_element_type=F32) + router_b.astype(F32)
    top_val, top_idx = lax.top_k(logits, TOP_K)
    gate = jax.nn.softmax(top_val, axis=-1)
    n_assign = n_tok * TOP_K
    flat_e = top_idx.reshape(n_assign)
    order = jnp.argsort(flat_e)
    sorted_e = flat_e[order]
    counts = jnp.bincount(flat_e, length=N_EXPERTS)
    padded = (counts + EXPERT_BLOCK - 1) // EXPERT_BLOCK * EXPERT_BLOCK
    pad_end = jnp.cumsum(padded)
    pad_start = pad_end - padded
    start = jnp.cumsum(counts) - counts
    dest = pad_start[sorted_e] + jnp.arange(n_assign) - start[sorted_e]
    n_blocks = -(-n_assign // EXPERT_BLOCK) + N_EXPERTS
    n_rows = n_blocks * EXPERT_BLOCK
    row_token = jnp.full((n_rows,), n_tok, jnp.int32).at[dest].set((order // TOP_K).astype(jnp.int32))
    row_gate = jnp.zeros((n_rows,), F32).at[dest].set(gate.reshape(n_assign)[order])
    block_expert = jnp.minimum(jnp.searchsorted(pad_end, jnp.arange(n_blocks) * EXPERT_BLOCK, side='right'),
                               N_EXPERTS - 1)
    x_pad = jnp.concatenate([xt, jnp.zeros((1, d), xt.dtype)], axis=0)

    def expert_block(args):
        tok, gw, e = args
        hdn = x_pad[tok] @ w1[e] + b1[e]
        glu = jnp.minimum(hdn[..., 0::2], SWIGLU_LIMIT)
        lin = jnp.clip(hdn[..., 1::2], -SWIGLU_LIMIT, SWIGLU_LIMIT)
        act = glu * jax.nn.sigmoid(SWIGLU_ALPHA * glu) * (lin + 1.0)
        return (act @ w2[e] + b2[e]) * gw[:, None].astype(xt.dtype)

    y_rows = lax.map(expert_block, (row_token.reshape(n_blocks, EXPERT_BLOCK),
                                    row_gate.reshape(n_blocks, EXPERT_BLOCK), block_expert))
    y = jax.ops.segment_sum(y_rows.reshape(n_rows, d), row_token, num_segments=n_tok + 1)
    return y[:n_tok]


def setup_inputs(seed: int = 0) -> dict:
    key = jax.random.key(seed)
    keys = jax.random.split(key, 40)
    counter = [0]

    def nxt():
        k = keys[counter[0]]
        counter[0] += 1
        return k

    def nrm(shape, scale):
        return jax.random.normal(nxt(), shape, F32) * scale

    def gain(shape):
        return 1.0 + 0.05 * jax.random.normal(nxt(), shape, F32)

    L, D = DEPTH, D_MODEL
    x = nrm((BATCH, SEQ, D), 1.0)
    c = nrm((BATCH, D), 1.0)
    ctx = nrm((BATCH, CTX_LEN, D), 1.0)
    c_ctx = nrm((D,), 1.0)
    ada_w = nrm((L, D, 6 * D), 0.5 * D ** -0.5)
    ada_b = nrm((L, 6 * D), 0.01)
    norm1_g = gain((L, D))
    norm2_g = gain((L, D))
    w_in = nrm((L, D, IN_COLS), D ** -0.5)
    dn_conv_w = nrm((L, DN_CONV, 2 * DN_HEADS * DN_DK + DN_HEADS * DN_DV), DN_CONV ** -0.5)
    dn_a_log = jnp.log(jax.random.uniform(nxt(), (L, 2, DN_HEADS), F32, 1.0, 16.0))
    dt = jnp.exp(jax.random.uniform(nxt(), (L, 2, DN_HEADS), F32, math.log(1e-3), math.log(1e-1)))
    dn_dt_bias = dt + jnp.log(-jnp.expm1(-dt))
    dn_norm_g = gain((L, DN_DV))
    sc_conv_w = nrm((L, SC_CONV, SC_WIDTH), SC_CONV ** -0.5)
    mla_q_norm_g = gain((L, MLA_Q_LORA))
    mla_w_qb = nrm((L, MLA_Q_LORA, MLA_HEADS * (MLA_NOPE + MLA_ROPE)), MLA_Q_LORA ** -0.5)
    mla_kv_norm_g = gain((L, MLA_KV_LORA))
    mla_w_kvb = nrm((L, MLA_KV_LORA, MLA_HEADS * (MLA_NOPE + MLA_V)), MLA_KV_LORA ** -0.5)
    w_branch_gate = nrm((L, D, 3 * D), D ** -0.5)
    b_branch_gate = nrm((L, 3 * D), 0.01)
    w_branch_dn = nrm((L, DN_HEADS * DN_DV, D), (DN_HEADS * DN_DV) ** -0.5)
    w_branch_sc = nrm((L, SC_WIDTH, D), SC_WIDTH ** -0.5)
    w_branch_mla = nrm((L, MLA_HEADS * MLA_V, D), (MLA_HEADS * MLA_V) ** -0.5)
    w_out = nrm((L, D, D), D ** -0.5)
    router_w = nrm((L, D, N_EXPERTS), D ** -0.5)
    router_b = nrm((L, N_EXPERTS), 0.01)
    expert_w1 = nrm((L, N_EXPERTS, D, 2 * EXPERT_FF), D ** -0.5)
    expert_b1 = nrm((L, N_EXPERTS, 2 * EXPERT_FF), 0.01)
    expert_w2 = nrm((L, N_EXPERTS, EXPERT_FF, D), EXPERT_FF ** -0.5)
    expert_b2 = nrm((L, N_EXPERTS, D), 0.01)
    final_norm_g = gain((D,))
    return {"x": x, "c": c, "ctx": ctx, "c_ctx": c_ctx, "ada_w": ada_w, "ada_b": ada_b,
            "norm1_g": norm1_g, "norm2_g": norm2_g, "w_in": w_in, "dn_conv_w": dn_conv_w,
            "dn_a_log": dn_a_log, "dn_dt_bias": dn_dt_bias, "dn_norm_g": dn_norm_g, "sc_conv_w": sc_conv_w,
            "mla_q_norm_g": mla_q_norm_g, "mla_w_qb": mla_w_qb, "mla_kv_norm_g": mla_kv_norm_g,
            "mla_w_kvb": mla_w_kvb, "w_branch_gate": w_branch_gate, "b_branch_gate": b_branch_gate,
            "w_branch_dn": w_branch_dn, "w_branch_sc": w_branch_sc, "w_branch_mla": w_branch_mla,
            "w_out": w_out, "router_w": router_w, "router_b": router_b, "expert_w1": expert_w1,
            "expert_b1": expert_b1, "expert_w2": expert_w2, "expert_b2": expert_b2,
            "final_norm_g": final_norm_g}


def reference(x, c, ctx, c_ctx, ada_w, ada_b, norm1_g, norm2_g, w_in, dn_conv_w, dn_a_log, dn_dt_bias, dn_norm_g,
              sc_conv_w, mla_q_norm_g, mla_w_qb, mla_kv_norm_g, mla_w_kvb, w_branch_gate, b_branch_gate,
              w_branch_dn, w_branch_sc, w_branch_mla, w_out, router_w, router_b, expert_w1, expert_b1,
              expert_w2, expert_b2, final_norm_g):
    bsz, t, d = x.shape
    rows = t // GRID_W
    rope = axial_rope_tables(rows)
    silu_c = jax.nn.silu(c)
    silu_cc = jax.nn.silu(c_ctx)[None]
    h_lat = x
    h_ctx = ctx
    for l in range(DEPTH):
        need_ctx = l < DEPTH - 1
        mod_lat = (silu_c @ ada_w[l] + ada_b[l])[:, None, :]
        mod_ctx = (silu_cc @ ada_w[l] + ada_b[l])[:, None, :]
        sh1, sc1, g1, sh2, sc2, g2 = jnp.split(mod_lat, 6, axis=-1)
        csh1, csc1, cg1, csh2, csc2, cg2 = jnp.split(mod_ctx, 6, axis=-1)
        xm_lat = rms_norm(h_lat, norm1_g[l]) * (1.0 + sc1) + sh1
        xm_ctx = rms_norm(h_ctx, norm1_g[l]) * (1.0 + csc1) + csh1
        y_lat, y_ctx = mixer_sublayer(xm_lat, xm_ctx, rope, need_ctx, w_in[l], dn_conv_w[l], dn_a_log[l],
                                      dn_dt_bias[l], dn_norm_g[l], sc_conv_w[l], mla_q_norm_g[l], mla_w_qb[l],
                                      mla_kv_norm_g[l], mla_w_kvb[l], w_branch_gate[l], b_branch_gate[l],
                                      w_branch_dn[l], w_branch_sc[l], w_branch_mla[l], w_out[l])
        h_lat = h_lat + g1 * y_lat
        xm2_lat = rms_norm(h_lat, norm2_g[l]) * (1.0 + sc2) + sh2
        if need_ctx:
            h_ctx = h_ctx + cg1 * y_ctx
            xm2_ctx = rms_norm(h_ctx, norm2_g[l]) * (1.0 + csc2) + csh2
            tokens = jnp.concatenate([xm2_lat.reshape(-1, d), xm2_ctx.reshape(-1, d)], axis=0)
            y = moe_ffn(tokens, router_w[l], router_b[l], expert_w1[l], expert_b1[l], expert_w2[l], expert_b2[l])
            n_lat = bsz * t
            h_lat = h_lat + g2 * y[:n_lat].reshape(bsz, t, d)
            h_ctx = h_ctx + cg2 * y[n_lat:].reshape(h_ctx.shape)
        else:
            y = moe_ffn(xm2_lat.reshape(-1, d), router_w[l], router_b[l], expert_w1[l], expert_b1[l],
                        expert_w2[l], expert_b2[l])
            h_lat = h_lat + g2 * y.reshape(bsz, t, d)
    return rms_norm(h_lat, final_norm_g)
```

```python
import numpy as np
from contextlib import ExitStack
import concourse.bass as bass
import concourse.mybir as mybir
from concourse.bass_utils import run_bass_kernel_spmd

F32 = mybir.dt.float32
BF16 = mybir.dt.bfloat16
AF = mybir.ActivationFunctionType
ALU = mybir.AluOpType

D = 1024
SEQ = 4096
CTX = 256
NT = SEQ + CTX
NCH = D // 128
DEPTH = 2
IN_COLS = 4048
NE = 32
FF = 1024
EPS = 1e-6

O_Q, O_K, O_V, O_Z, O_A, O_B, O_SH, O_SB, O_SC, O_QA, O_KV = 0, 512, 1024, 1536, 2048, 2056, 2064, 2576, 3088, 3600, 3856
PROJ_SRC = [(O_Q, 512), (O_K, 512), (O_V, 512), (O_Z, 512), (O_SH, 512), (O_SB, 512), (O_SC, 512), (O_QA, 256), (O_KV, 192)]
R_Q, R_K, R_V, R_Z, R_SH, R_SB, R_SC, R_QA, R_CKV, R_KR = 0, 512, 1024, 1536, 2048, 2560, 3072, 3584, 3840, 3968
PROJ_ROWS = 4032

V_ADAB, V_N1, V_N2, V_BG, V_DNCW, V_SCCW, V_QNG, V_KVNG, V_DNG, V_ALOG, V_DTB, V_RB, V_B1G, V_B1L, V_FNG = (
    0, 48, 56, 64, 88, 124, 136, 138, 139, 140, 148, 156, 188, 444, 700)
NV = 708


def _is_ap(v):
    return hasattr(v, "tensor") and hasattr(v, "ap")


class Prog:
    def __init__(self, nc, n_dma=24):
        self.nc = nc
        self.es = ExitStack()
        self.eng = {"pe": nc.tensor, "act": nc.scalar, "dve": nc.vector, "pool": nc.gpsimd, "sp": nc.sync}
        self.semobj = {}
        self.cnt = {}
        for e in ["pe", "act", "dve", "pool"]:
            self.semobj[e] = self.es.enter_context(nc.semaphore(f"s_{e}"))
            self.cnt[e] = 0
        self.ring = {}
        self.rnext = {}
        for q, n in (("sp", 12), ("act", 6), ("pool", 6), ("dve", 2), ("pe", 2)):
            self.ring[q] = []
            self.rnext[q] = 0
            for i in range(n):
                nm = f"d{q}{i}"
                self.semobj[nm] = self.es.enter_context(nc.semaphore(f"s_{nm}"))
                self.cnt[nm] = 0
                self.ring[q].append(nm)
        self.known = {e: {} for e in self.eng}
        self.W = {}
        self.R = {}
        self.nops = 0
        self.mute = False
        self._nm = ""
        import os as _os
        self.trace = bool(_os.environ.get("KDBG_TRACE", ""))
        self.maxops = int(_os.environ.get("KDBG_MAXOPS", "100000000"))

    @staticmethod
    def key(ap):
        return ap.tensor.name

    def op(self, eng, fn, reads=(), writes=(), signal=True, dma=False):
        if self.mute or self.nops >= self.maxops:
            return ("x", 0)
        writes = list(writes) + [k for k in reads if isinstance(k, str) and k.startswith("ps") and k not in writes]
        need = {}
        for k in reads:
            for s, v in self.W.get(k, {}).items():
                if need.get(s, 0) < v:
                    need[s] = v
        for k in writes:
            for s, v in self.W.get(k, {}).items():
                if need.get(s, 0) < v:
                    need[s] = v
            for s, v in self.R.get(k, {}).items():
                if need.get(s, 0) < v:
                    need[s] = v
        e = self.eng[eng]
        kn = self.known[eng]
        for s, v in need.items():
            if eng == "pe" and s == "pe":
                continue
            if kn.get(s, 0) >= v:
                continue
            e.wait_ge(self.semobj[s], v)
            kn[s] = v
        if dma:
            rs_ = self.ring[eng][self.rnext[eng]]
            if self.cnt[rs_] > kn.get(rs_, 0):
                e.wait_ge(self.semobj[rs_], self.cnt[rs_])
                kn[rs_] = self.cnt[rs_]
        ins = fn(e)
        if self.trace:
            print("OP", self.nops, eng, self._nm, list(writes), list(reads))
        self.nops += 1
        if dma:
            s = self.ring[eng][self.rnext[eng]]
            self.rnext[eng] = (self.rnext[eng] + 1) % len(self.ring[eng])
            self.cnt[s] += 16
            ins.then_inc(self.semobj[s], 16)
            ref = (s, self.cnt[s])
        elif signal:
            self.cnt[eng] += 1
            ins.then_inc(self.semobj[eng], 1)
            ref = (eng, self.cnt[eng])
        else:
            ref = (eng, self.cnt[eng] + 1)
        for k in reads:
            d = self.R.setdefault(k, {})
            if d.get(ref[0], 0) < ref[1]:
                d[ref[0]] = ref[1]
        for k in writes:
            d = self.W.setdefault(k, {})
            if d.get(ref[0], 0) < ref[1]:
                d[ref[0]] = ref[1]
        return ref

    def barrier(self, engines=None):
        if self.mute:
            return
        for eng, e in self.eng.items():
            if engines is not None and eng not in engines:
                continue
            kn = self.known[eng]
            for s, v in self.cnt.items():
                if v == 0 or (eng == "pe" and s == "pe"):
                    continue
                if kn.get(s, 0) >= v:
                    continue
                e.wait_ge(self.semobj[s], v)
                kn[s] = v

    def mm(self, out, lhsT, rhs, start=True, stop=True, rk=None, wk=None, **kw):
        reads = rk if rk is not None else [self.key(lhsT), self.key(rhs)]
        writes = wk if wk is not None else [self.key(out)]
        self._nm = "matmul"
        return self.op("pe", lambda e: e.matmul(out, lhsT, rhs, start=start, stop=stop, **kw), reads, writes, signal=stop)

    def tr(self, out, in_, ident, rk=None, wk=None):
        reads = rk if rk is not None else [self.key(in_), self.key(ident)]
        writes = wk if wk is not None else [self.key(out)]
        self._nm = "transpose"
        return self.op("pe", lambda e: e.transpose(out, in_, ident), reads, writes)

    def I(self, eng, meth, rk=None, wk=None, **kw):
        if rk is None:
            rk = [self.key(v) for k_, v in kw.items() if k_ not in ("out", "accum_out", "ap") and _is_ap(v)]
        if wk is None:
            wk = [self.key(kw[k_]) for k_ in ("out", "accum_out", "ap") if k_ in kw and kw[k_] is not None]
        self._nm = meth
        return self.op(eng, lambda e: getattr(e, meth)(**kw), rk, wk)

    def dma(self, q, out, in_, rk=None, wk=None, **kw):
        reads = rk if rk is not None else [self.key(in_)]
        writes = wk if wk is not None else [self.key(out)]
        self._nm = "dma"
        return self.op(q, lambda e: e.dma_start(out=out, in_=in_, **kw), reads, writes, dma=True)


def build(stop_after=None, dbg=(), ne=NE):
    nc = bass.Bass("TRN2", target_bir_lowering=False)
    P = Prog(nc)
    es = P.es
    dbg = set(dbg)

    def dram_in(name, shape, dt=F32):
        return nc.dram_tensor(name, list(shape), dt, kind="ExternalInput").ap()

    def dram_scr(name, shape, dt=F32):
        kind = "ExternalOutput" if name in dbg else "Internal"
        return nc.dram_tensor(name, list(shape), dt, kind=kind).ap()

    _zero = [False]

    _uid = [0]

    def sb(st, name, shape, dt=F32):
        _uid[0] += 1
        t = st.enter_context(nc.sbuf_tensor(f"{name}_u{_uid[0]}", list(shape), dt))
        if _zero[0]:
            P.I("dve", "memset", ap=t[:], constant=0.0)
        return t

    x_in = dram_in("x", [SEQ, D])
    ctx_in = dram_in("ctx", [CTX, D])
    cvec_in = dram_in("cvec", [128, 16])
    consts_in = dram_in("consts", [128, 7 * 128])
    vecs_in = dram_in("vecs", [DEPTH, 128, NV])
    ada_w_in = dram_in("ada_w", [DEPTH, D, 6 * D])
    w_in_in = dram_in("w_in", [DEPTH, D, IN_COLS])
    mla_w_qb_in = dram_in("mla_w_qb", [DEPTH, 256, 768])
    mla_w_kvb_in = dram_in("mla_w_kvb", [DEPTH, 128, 1024])
    w_gate_in = dram_in("w_branch_gate", [DEPTH, D, 3 * D])
    w_dn_in = dram_in("w_branch_dn", [DEPTH, 512, D])
    w_sc_in = dram_in("w_branch_sc", [DEPTH, 512, D])
    w_mla_in = dram_in("w_branch_mla", [DEPTH, 512, D])
    w_out_in = dram_in("w_out", [DEPTH, D, D])
    router_w_in = dram_in("router_w", [DEPTH, D, NE])
    w1_in = dram_in("expert_w1", [DEPTH, ne, D, 2 * FF])
    w2_in = dram_in("expert_w2", [DEPTH, ne, FF, D])
    expert_b2_in = dram_in("expert_b2", [DEPTH, NE, D])
    rope_in = dram_in("rope", [3, 128, NT])
    out_dram = nc.dram_tensor("out", [SEQ, D], F32, kind="ExternalOutput").ap()

    hT = dram_scr("hT", [D, NT])
    xmT = dram_scr("xmT", [D, NT], BF16)
    projT = dram_scr("projT", [PROJ_ROWS, NT])
    ab_tm = dram_scr("ab_tm", [NT, 16])
    mods_d = dram_scr("mods_d", [DEPTH, 128, 96])
    bg_tm = dram_scr("bg_tm", [NT, 16])
    qnT = dram_scr("qnT", [512, NT])
    knT = dram_scr("knT", [512, NT])
    qnTb = dram_scr("qnTb", [512, NT], BF16)
    knTb = dram_scr("knTb", [512, NT], BF16)
    k_tm = dram_scr("k_tm", [NT, 512])
    v_tm = dram_scr("v_tm", [NT, 512])
    o_dir = dram_scr("o_dir", [2, NT, 512])
    y_dnT = dram_scr("y_dnT", [512, NT], BF16)
    y_scT = dram_scr("y_scT", [512, NT], BF16)
    y_mlaT = dram_scr("y_mlaT", [512, NT], BF16)
    qnopeT = dram_scr("qnopeT", [512, NT], BF16)
    qropeT = dram_scr("qropeT", [256, NT], BF16)
    knopeT = dram_scr("knopeT", [512, NT], BF16)
    kropeT = dram_scr("kropeT", [128, NT], BF16)
    v_mla = dram_scr("v_mla", [NT, 512], BF16)
    xm2T = dram_scr("xm2T", [D, NT], BF16)
    gatesT = dram_scr("gatesT", [32, NT])
    yaccT = dram_scr("yaccT", [D, NT])

    ident = sb(es, "ident", [128, 128])
    cU = sb(es, "cU", [128, 128])
    cLo = sb(es, "cLo", [128, 128])
    cSU = sb(es, "cSU", [128, 128])
    cSLo = sb(es, "cSLo", [128, 128])
    cSC0 = sb(es, "cSC0", [128, 128])
    cSC1 = sb(es, "cSC1", [128, 128])
    ones = sb(es, "ones", [128, 128])
    ropeP = sb(es, "ropeP", [128, 128])
    cvec = sb(es, "cvec_sb", [128, 16])
    vecs = [sb(es, f"vecs{l}", [128, NV]) for l in range(DEPTH)]
    mods = [sb(es, f"mods{l}", [128, 2, 6, 8]) for l in range(DEPTH)]
    ps = [es.enter_context(nc.psum_tensor(f"ps{i}", [128, 512], F32)) for i in range(8)]

    import os as _os
    _only = _os.environ.get("KDBG_ONLY", "")
    for i, t in enumerate([ident, cU, cLo, cSU, cSLo, cSC0, cSC1]):
        P.dma("sp", t[:], consts_in[:, i * 128:(i + 1) * 128])
    P.dma("sp", cvec[:], cvec_in[:])
    P.dma("sp", ropeP[:], rope_in[2, :, 0:128])
    for l in range(DEPTH):
        P.dma("sp", vecs[l][:], vecs_in[l])
    P.I("dve", "memset", ap=ones[:], constant=1.0)
    for i in range(8):
        P.I("dve", "memset", ap=ps[i][:], constant=0.0)
    P.mute = bool(_only)

    with ExitStack() as st:
        xin = [sb(st, f"xin{i}", [128, D]) for i in range(2)]
        xo = [sb(st, f"xo{i}", [128, 8, 128]) for i in range(2)]
        for ti in range(NT // 128):
            src = x_in[ti * 128:(ti + 1) * 128, :] if ti < SEQ // 128 else ctx_in[(ti - SEQ // 128) * 128:(ti - SEQ // 128 + 1) * 128, :]
            xi = xin[ti % 2]
            P.dma("sp", xi[:], src)
            pt = ps[(ti % 2) * 2:(ti % 2) * 2 + 2]
            for c in range(8):
                P.tr(pt[c // 4][:, (c % 4) * 128:(c % 4 + 1) * 128], xi[:, c * 128:(c + 1) * 128], ident[:])
            o = xo[ti % 2]
            P.I("act", "activation", out=o[:, 0:4, :], in_=pt[0][:].rearrange("p (c t) -> p c t", c=4), func=AF.Copy)
            P.I("dve", "tensor_copy", out=o[:, 4:8, :], in_=pt[1][:].rearrange("p (c t) -> p c t", c=4))
            P.dma("sp", hT.rearrange("(c p) t -> p c t", p=128)[:, :, ti * 128:(ti + 1) * 128], o[:])
    P.barrier()

    with ExitStack() as st:
        silu_c = sb(st, "silu_c", [128, 8, 2])
        P.I("act", "activation", out=silu_c[:].rearrange("p c s -> p s c"), in_=cvec[:].rearrange("p (s c) -> p s c", s=2), func=AF.Silu)
        adaw = [sb(st, f"adaw{i}", [128, 8, 1024]) for i in range(2)]
        modraw = sb(st, "modraw", [128, 48, 2])
        for l in range(DEPTH):
            mp = ps[0]
            for jg in range(6):
                aw = adaw[jg % 2]
                P.dma("sp" if jg % 2 == 0 else "act", aw[:], ada_w_in[l].rearrange("(c p) n -> p c n", p=128)[:, :, jg * 1024:(jg + 1) * 1024])
                for jj in range(8):
                    j = jg * 8 + jj
                    for c in range(8):
                        P.mm(mp[:, j * 2:j * 2 + 2], aw[:, c, jj * 128:(jj + 1) * 128], silu_c[:, c, :], start=(c == 0), stop=(c == 7))
            P.I("dve", "tensor_copy", out=modraw[:].rearrange("p j s -> p (j s)"), in_=mp[:, 0:96])
            vv = vecs[l]
            m = mods[l]
            for s in range(2):
                md = sb(st, f"md{l}{s}", [128, 48])
                P.I("dve", "tensor_tensor", out=md[:], in0=modraw[:, :, s], in1=vv[:, V_ADAB:V_ADAB + 48], op=ALU.add)
                for half, vn in ((0, V_N1), (1, V_N2)):
                    b0 = half * 24
                    P.I("dve", "scalar_tensor_tensor", out=m[:, s, half * 3 + 0, :], in0=md[:, b0 + 8:b0 + 16], scalar=1.0,
                        in1=vv[:, vn:vn + 8], op0=ALU.add, op1=ALU.mult)
                    P.I("dve", "tensor_copy", out=m[:, s, half * 3 + 1, :], in_=md[:, b0:b0 + 8])
                    P.I("dve", "tensor_copy", out=m[:, s, half * 3 + 2, :], in_=md[:, b0 + 16:b0 + 24])
            if "mods_d" in dbg:
                P.dma("sp", mods_d[l], m[:].rearrange("p s k c -> p (s k c)"))
    P.barrier()
    if stop_after == "P0":
        return finish(nc, P, out_dram)

    for l in range(DEPTH):
        vv = vecs[l]
        m = mods[l]
        with ExitStack() as st:
            winb = sb(st, "winb", [128, 8, IN_COLS], BF16)
            wab = sb(st, "wab", [128, 8, 16])
            for c in range(8):
                P.dma("pool", winb[:, c, :], w_in_in[l, c * 128:(c + 1) * 128, :])
            P.dma("sp", wab[:], w_in_in[l].rearrange("(c p) n -> p c n", p=128)[:, :, O_A:O_A + 16])
            htl = [sb(st, f"htl{i}", [128, 8, 512]) for i in range(2)]
            sq = sb(st, "sq", [128, 8, 512])
            rstd = sb(st, "rstd", [128, 512])
            tmp = sb(st, "tmp1", [128, 512])
            xm32 = sb(st, "xm32", [128, 8, 512])
            xmb = [sb(st, f"xmb{i}", [128, 8, 512], BF16) for i in range(2)]
            abt = sb(st, "abt", [128, 4, 16])
            stg = [sb(st, f"stg{i}", [128, 512]) for i in range(4)]
            nstg = 0
            tiles = [(t0, 512, 0) for t0 in range(0, SEQ, 512)] + [(SEQ, 256, 1)]
            for ti, (t0, tw, s) in enumerate(tiles):
                ht = htl[ti % 2]
                xb = xmb[ti % 2]
                P.dma("sp", ht[:, :, 0:tw], hT.rearrange("(c p) t -> p c t", p=128)[:, :, t0:t0 + tw])
                P.I("act", "activation", out=sq[:, :, 0:tw], in_=ht[:, :, 0:tw], func=AF.Square)
                for c in range(8):
                    P.mm(ps[0][:, 0:tw], ones[:], sq[:, c, 0:tw], start=(c == 0), stop=(c == 7))
                P.I("act", "activation", out=rstd[:, 0:tw], in_=ps[0][:, 0:tw], func=AF.Sqrt, scale=1.0 / D, bias=EPS)
                P.I("dve", "reciprocal", out=rstd[:, 0:tw], in_=rstd[:, 0:tw])
                for c in range(8):
                    P.I("dve", "scalar_tensor_tensor", out=tmp[:, 0:tw], in0=ht[:, c, 0:tw], scalar=m[:, s, 0, c:c + 1],
                        in1=rstd[:, 0:tw], op0=ALU.mult, op1=ALU.mult)
                    P.I("act", "activation", out=xm32[:, c, 0:tw], in_=tmp[:, 0:tw], func=AF.Identity, bias=m[:, s, 1, c:c + 1])
                    P.I("dve", "tensor_copy", out=xb[:, c, 0:tw], in_=xm32[:, c, 0:tw])
                P.dma("sp", xmT.rearrange("(c p) t -> p c t", p=128)[:, :, t0:t0 + tw], xb[:, :, 0:tw])
                nsub = tw // 128
                for sbk in range(nsub):
                    for c in range(8):
                        P.mm(ps[1][:, sbk * 16:(sbk + 1) * 16], xm32[:, c, sbk * 128:(sbk + 1) * 128], wab[:, c, :], start=(c == 0), stop=(c == 7))
                P.I("dve", "tensor_copy", out=abt[:, 0:nsub, :], in_=ps[1][:, 0:nsub * 16].rearrange("p (s n) -> p s n", n=16))
                P.dma("sp", ab_tm[t0:t0 + tw, :].rearrange("(s p) n -> p s n", p=128), abt[:, 0:nsub, :])
                row = 0
                k = 0
                for (c0, wdt) in PROJ_SRC:
                    for o in range(0, wdt, 128):
                        mw = min(128, wdt - o)
                        pp = ps[2 + k % 4]
                        for c in range(8):
                            P.mm(pp[0:mw, 0:tw], winb[:, c, c0 + o:c0 + o + mw], xb[:, c, 0:tw], start=(c == 0), stop=(c == 7))
                        sg = stg[nstg % 4]
                        nstg += 1
                        if k % 2 == 0:
                            P.I("act", "activation", out=sg[0:mw, 0:tw], in_=pp[0:mw, 0:tw], func=AF.Copy)
                        else:
                            P.I("dve", "tensor_copy", out=sg[0:mw, 0:tw], in_=pp[0:mw, 0:tw])
                        P.dma("sp", projT[row:row + mw, t0:t0 + tw], sg[0:mw, 0:tw])
                        row += mw
                        k += 1
                assert row == PROJ_ROWS
        P.barrier()
        if stop_after == f"P1_{l}":
            return finish(nc, P, out_dram)

        with ExitStack() as st:
            abt_all = sb(st, "abt_all", [128, 34, 16])
            bg_all = sb(st, "bg_all", [128, 34, 16])
            gt1 = sb(st, "g_t1", [128, 34, 8])
            ea = sb(st, "ea", [128, 8])
            P.dma("sp", abt_all[:], ab_tm.rearrange("(s p) n -> p s n", p=128))
            P.I("dve", "tensor_tensor", out=gt1[:], in0=abt_all[:, :, 0:8],
                in1=vv[:, V_DTB:V_DTB + 8].unsqueeze(1).to_broadcast([128, 34, 8]), op=ALU.add)
            P.I("act", "activation", out=gt1[:], in_=gt1[:], func=AF.Exp)
            P.I("act", "activation", out=gt1[:], in_=gt1[:], func=AF.Ln, bias=1.0)
            P.I("act", "activation", out=ea[:], in_=vv[:, V_ALOG:V_ALOG + 8], func=AF.Exp)
            P.I("dve", "scalar_tensor_tensor", out=bg_all[:, :, 8:16], in0=gt1[:], scalar=-1.0,
                in1=ea[:].unsqueeze(1).to_broadcast([128, 34, 8]), op0=ALU.mult, op1=ALU.mult)
            P.I("act", "activation", out=gt1[:], in_=abt_all[:, :, 8:16], func=AF.Exp, scale=-1.0)
            P.I("dve", "tensor_scalar_add", out=gt1[:], in0=gt1[:], scalar1=1.0)
            P.I("dve", "reciprocal", out=bg_all[:, :, 0:8], in_=gt1[:])
            P.dma("sp", bg_tm.rearrange("(s p) n -> p s n", p=128), bg_all[:])
        P.barrier()

        with ExitStack() as st:
            raw = sb(st, "d1raw", [128, 12, 514])
            acc = sb(st, "d1acc", [128, 12, 512])
            sl = sb(st, "d1sl", [128, 12, 512])
            sq8 = sb(st, "d1sq", [128, 8, 512])
            rn = sb(st, "d1rn", [128, 512])
            qk = sb(st, "d1qk", [128, 8, 512])
            qkb = sb(st, "d1qkb", [128, 8, 512], BF16)
            tmt = sb(st, "d1tm", [128, 4, 1024])
            tiles = [(t0, 512, 0, SEQ) for t0 in range(0, SEQ, 512)] + [(SEQ, 256, SEQ, NT)]
            for ti, (t0, tw, s0, s1) in enumerate(tiles):
                lo, hi = max(t0 - 1, s0), min(t0 + tw + 1, s1)
                P.I("dve", "memset", ap=raw[:, :, 0:1], constant=0.0)
                P.I("dve", "memset", ap=raw[:, :, tw + 1:tw + 2], constant=0.0)
                P.dma("sp", raw[:, :, lo - (t0 - 1):hi - (t0 - 1)],
                      projT[R_Q:R_Q + 1536, :].rearrange("(c p) t -> p c t", p=128)[:, :, lo:hi])
                for j in range(12):
                    P.I("dve", "tensor_scalar", out=acc[:, j, 0:tw], in0=raw[:, j, 0:tw], scalar1=vv[:, V_DNCW + j:V_DNCW + j + 1],
                        scalar2=None, op0=ALU.mult)
                    P.I("dve", "scalar_tensor_tensor", out=acc[:, j, 0:tw], in0=raw[:, j, 1:tw + 1], scalar=vv[:, V_DNCW + 12 + j:V_DNCW + 13 + j],
                        in1=acc[:, j, 0:tw], op0=ALU.mult, op1=ALU.add)
                    P.I("dve", "scalar_tensor_tensor", out=acc[:, j, 0:tw], in0=raw[:, j, 2:tw + 2], scalar=vv[:, V_DNCW + 24 + j:V_DNCW + 25 + j],
                        in1=acc[:, j, 0:tw], op0=ALU.mult, op1=ALU.add)
                P.I("act", "activation", out=sl[:, :, 0:tw], in_=acc[:, :, 0:tw], func=AF.Silu)
                P.I("act", "activation", out=sq8[:, :, 0:tw], in_=sl[:, 0:8, 0:tw], func=AF.Square)
                for j in range(8):
                    pp = ps[j % 2]
                    P.mm(pp[:, 0:tw], ones[:], sq8[:, j, 0:tw])
                    P.I("act", "activation", out=rn[:, 0:tw], in_=pp[:, 0:tw], func=AF.Sqrt, bias=1e-6)
                    P.I("dve", "reciprocal", out=rn[:, 0:tw], in_=rn[:, 0:tw])
                    P.I("dve", "scalar_tensor_tensor", out=qk[:, j, 0:tw], in0=sl[:, j, 0:tw], scalar=(128.0 ** -0.5 if j < 4 else 1.0),
                        in1=rn[:, 0:tw], op0=ALU.mult, op1=ALU.mult)
                P.I("act", "activation", out=qkb[:, :, 0:tw], in_=qk[:, :, 0:tw], func=AF.Copy)
                P.dma("act", qnTb.rearrange("(c p) t -> p c t", p=128)[:, :, t0:t0 + tw], qkb[:, 0:4, 0:tw])
                P.dma("act", knTb.rearrange("(c p) t -> p c t", p=128)[:, :, t0:t0 + tw], qkb[:, 4:8, 0:tw])
                P.dma("sp", qnT.rearrange("(c p) t -> p c t", p=128)[:, :, t0:t0 + tw], qk[:, 0:4, 0:tw])
                P.dma("sp", knT.rearrange("(c p) t -> p c t", p=128)[:, :, t0:t0 + tw], qk[:, 4:8, 0:tw])
                nb = tw // 128
                for blk in range(nb):
                    for j in range(8):
                        src = qk[:, 4 + j, blk * 128:(blk + 1) * 128] if j < 4 else sl[:, 4 + j, blk * 128:(blk + 1) * 128]
                        P.tr(ps[2 + j // 4][:, (j % 4) * 128:(j % 4 + 1) * 128], src, ident[:])
                    P.I("act", "activation", out=tmt[:, blk, 0:512], in_=ps[2][:], func=AF.Copy)
                    P.I("dve", "tensor_copy", out=tmt[:, blk, 512:1024], in_=ps[3][:])
                P.dma("sp", k_tm[t0:t0 + tw, :].rearrange("(b p) d -> p b d", p=128), tmt[:, 0:nb, 0:512])
                P.dma("sp", v_tm[t0:t0 + tw, :].rearrange("(b p) d -> p b d", p=128), tmt[:, 0:nb, 512:1024])
        P.barrier()
        if stop_after == f"D1_{l}":
            return finish(nc, P, out_dram)

        if _only == "D2":
            P.mute = False
        _zero[0] = False
        with ExitStack() as st:
            cSame = sb(st, "cSame", [128, 128])
            P.I("dve", "tensor_tensor", out=cSame[:], in0=cLo[:], in1=cSU[:], op=ALU.add)
            Sst = [sb(st, f"S{h}", [128, 128]) for h in range(4)]
            qT = sb(st, "d2qT", [128, 4, 128])
            qTb = sb(st, "d2qTb", [128, 4, 128], BF16)
            kTb = sb(st, "d2kTb", [128, 4, 128], BF16)
            identb = sb(st, "identb", [128, 128], BF16)
            P.I("dve", "tensor_copy", out=identb[:], in_=ident[:])
            ktm = sb(st, "d2ktm", [128, 512])
            vtm = sb(st, "d2vtm", [128, 512])
            ktm2 = sb(st, "d2ktm2", [64, 2, 512])
            bgt = sb(st, "d2bg", [128, 16])
            gs = sb(st, "d2gs", [128, 24])
            egc = sb(st, "d2egc", [128, 4])
            sb1 = sb(st, "d2sb1", [128, 4])
            nbeta = sb(st, "d2nbeta", [128, 4])
            egcc = sb(st, "d2egcc", [64, 2, 4])
            edec = sb(st, "d2edec", [64, 2, 4])
            gtc = sb(st, "d2gtc", [128, 2, 4])
            Dm = [sb(st, f"d2D{h}", [128, 128]) for h in range(4)]
            D1m = [sb(st, f"d2D1{h}", [128, 128]) for h in range(4)]
            tmpm = [sb(st, f"d2tmp{h}", [128, 128]) for h in range(4)]
            Nm = [[sb(st, f"d2N{h}_{i}", [128, 128], BF16) for i in range(2)] for h in range(4)]
            NTm = [[sb(st, f"d2NT{h}_{i}", [128, 128], BF16) for i in range(2)] for h in range(4)]
            RT = [sb(st, f"d2RT{h}", [128, 128], BF16) for h in range(4)]
            Am = [sb(st, f"d2A{h}", [128, 128], BF16) for h in range(4)]
            vb = [sb(st, f"d2vb{h}", [128, 128], BF16) for h in range(4)]
            kbg = [sb(st, f"d2kbg{h}", [128, 128], BF16) for h in range(4)]
            um = [sb(st, f"d2u{h}", [64, 2, 128]) for h in range(4)]
            wT = [sb(st, f"d2wT{h}", [128, 128]) for h in range(4)]
            ATm = [sb(st, f"d2AT{h}", [64, 2, 128]) for h in range(4)]
            kdec = [sb(st, f"d2kdec{h}", [64, 2, 128]) for h in range(4)]
            vnew = [sb(st, f"d2vnew{h}", [64, 128]) for h in range(4)]
            avs = [sb(st, f"d2avs{h}", [64, 128]) for h in range(4)]
            osb = sb(st, "d2o", [64, 2, 512])
            H4 = range(4)
            for dr in range(2):
                Mcs = cU if dr == 0 else cLo
                Mstrict = cSLo if dr == 0 else cSU
                Mincl = cLo if dr == 0 else cU
                for h in H4:
                    P.I("dve", "memset", ap=Sst[h][:], constant=0.0)
                lat = list(range(0, 32)) if dr == 0 else list(range(31, -1, -1))
                cxt = [32, 33] if dr == 0 else [33, 32]
                import os as _os
                _lim = int(_os.environ.get("KDBG_D2TILES", "1000"))
                for tix in (cxt + lat)[:_lim]:
                    t0 = tix * 128
                    P.dma("sp", qT[:], qnT.rearrange("(h p) t -> p h t", p=128)[:, :, t0:t0 + 128])
                    P.dma("sp", qTb[:], qnTb.rearrange("(h p) t -> p h t", p=128)[:, :, t0:t0 + 128])
                    P.dma("act", kTb[:], knTb.rearrange("(h p) t -> p h t", p=128)[:, :, t0:t0 + 128])
                    P.dma("act", ktm[:], k_tm[t0:t0 + 128, :])
                    P.dma("act", vtm[:], v_tm[t0:t0 + 128, :])
                    P.dma("sp", ktm2[:], k_tm[t0:t0 + 128, :].rearrange("(c p) d -> p c d", p=64))
                    P.dma("sp", bgt[:], bg_tm[t0:t0 + 128, :])
                    g4 = bgt[:, 8 + dr * 4:12 + dr * 4]
                    b4 = bgt[:, dr * 4:dr * 4 + 4]
                    pg = ps[0]
                    P.mm(pg[:, 0:4], Mcs[:], g4)
                    P.mm(pg[:, 4:8], cSame[:], g4)
                    P.mm(pg[:, 8:12], cSC0[:], g4)
                    P.mm(pg[:, 12:16], cSC1[:], g4)
                    P.mm(pg[0:64, 16:20], Mcs[:, 0:64], g4)
                    P.mm(pg[0:64, 20:24], Mcs[:, 64:128], g4)
                    P.I("dve", "tensor_copy", out=gs[:, 0:16], in_=pg[:, 0:16])
                    P.I("dve", "tensor_copy", out=gs[0:64, 16:24], in_=pg[0:64, 16:24])
                    P.I("act", "activation", out=egc[:], in_=gs[:, 0:4], func=AF.Exp)
                    P.I("dve", "tensor_tensor", out=sb1[:], in0=egc[:], in1=b4, op=ALU.mult)
                    P.I("dve", "tensor_scalar", out=nbeta[:], in0=b4, scalar1=-1.0, scalar2=None, op0=ALU.mult)
                    P.I("act", "activation", out=egcc[:].rearrange("p c h -> p (c h)"), in_=gs[0:64, 16:24], func=AF.Exp)
                    P.I("dve", "tensor_tensor", out=edec[:].rearrange("p c h -> p (c h)"), in0=gs[0:64, 8:16], in1=gs[0:64, 16:24], op=ALU.subtract)
                    P.I("act", "activation", out=edec[:].rearrange("p c h -> p (c h)"), in_=edec[:].rearrange("p c h -> p (c h)"), func=AF.Exp)
                    P.I("act", "activation", out=gtc[:].rearrange("p c h -> p (c h)"), in_=gs[:, 8:16], func=AF.Exp)
                    for h in H4:
                        P.I("dve", "tensor_scalar", out=Dm[h][:], in0=ident[:], scalar1=gs[:, h:h + 1], scalar2=None, op0=ALU.mult)
                    for h in H4:
                        hs = slice(h * 128, (h + 1) * 128)
                        P.mm(ps[1][:, hs], ones[:], Dm[h][:])
                        P.mm(ps[2][:, hs], kTb[:, h, :], kTb[:, h, :])
                        P.mm(ps[3][:, hs], qTb[:, h, :], kTb[:, h, :])
                    for h in H4:
                        hs = slice(h * 128, (h + 1) * 128)
                        P.I("dve", "tensor_scalar", out=D1m[h][:], in0=ps[1][:, hs], scalar1=gs[:, h:h + 1], scalar2=0.0, op0=ALU.subtract, op1=ALU.max)
                        P.I("act", "activation", out=D1m[h][:], in_=D1m[h][:], func=AF.Exp, scale=-1.0)
                        P.I("dve", "tensor_tensor", out=tmpm[h][:], in0=ps[2][:, hs], in1=D1m[h][:], op=ALU.mult)
                        P.I("dve", "tensor_scalar", out=tmpm[h][:], in0=tmpm[h][:], scalar1=nbeta[:, h:h + 1], scalar2=None, op0=ALU.mult)
                        P.I("dve", "tensor_tensor", out=Nm[h][0][:], in0=tmpm[h][:], in1=Mstrict[:], op=ALU.mult)
                        P.I("dve", "tensor_tensor", out=Am[h][:], in0=ps[3][:, hs], in1=D1m[h][:], op=ALU.mult)
                        P.I("dve", "tensor_tensor", out=Am[h][:], in0=Am[h][:], in1=Mincl[:], op=ALU.mult)
                    for h in H4:
                        hs = slice(h * 128, (h + 1) * 128)
                        P.mm(ps[4 + h // 2][:, hs], Nm[h][0][:], identb[:])
                    for h in H4:
                        hs = slice(h * 128, (h + 1) * 128)
                        P.I("dve", "tensor_copy", out=NTm[h][0][:], in_=ps[4 + h // 2][:, hs])
                        P.I("dve", "tensor_tensor", out=RT[h][:], in0=ps[4 + h // 2][:, hs], in1=ident[:], op=ALU.add)
                    cur = 0
                    for kk in range(5):
                        nxt = 1 - cur
                        for h in H4:
                            hs = slice(h * 128, (h + 1) * 128)
                            P.mm(ps[4][:, hs], NTm[h][cur][:], Nm[h][cur][:])
                            if kk < 4:
                                P.mm(ps[5][:, hs], Nm[h][cur][:], NTm[h][cur][:])
                        for h in H4:
                            hs = slice(h * 128, (h + 1) * 128)
                            P.I("act", "activation", out=Nm[h][nxt][:], in_=ps[4][:, hs], func=AF.Copy)
                            if kk < 4:
                                P.I("dve", "tensor_copy", out=NTm[h][nxt][:], in_=ps[5][:, hs])
                        for h in H4:
                            hs = slice(h * 128, (h + 1) * 128)
                            P.mm(ps[6][:, hs], Nm[h][nxt][:], RT[h][:])
                        for h in H4:
                            hs = slice(h * 128, (h + 1) * 128)
                            P.I("dve", "tensor_tensor", out=RT[h][:], in0=ps[6][:, hs], in1=RT[h][:], op=ALU.add)
                        cur = nxt
                    for h in H4:
                        hs = slice(h * 128, (h + 1) * 128)
                        P.I("dve", "tensor_scalar", out=vb[h][:], in0=vtm[:, hs], scalar1=b4[:, h:h + 1], scalar2=None, op0=ALU.mult)
                        P.I("dve", "tensor_scalar", out=kbg[h][:], in0=ktm[:, hs], scalar1=sb1[:, h:h + 1], scalar2=None, op0=ALU.mult)
                        for c in range(2):
                            P.I("dve", "tensor_scalar", out=kdec[h][:, c, :], in0=ktm2[:, c, hs], scalar1=edec[:, c, h:h + 1], scalar2=None, op0=ALU.mult)
                    for h in H4:
                        hs = slice(h * 128, (h + 1) * 128)
                        P.mm(ps[1][0:64, hs], RT[h][:, 0:64], vb[h][:])
                        P.mm(ps[2][0:64, hs], RT[h][:, 64:128], vb[h][:])
                        P.mm(ps[3][:, hs], kbg[h][:], RT[h][:])
                        P.mm(ps[4][0:64, hs], Am[h][:, 0:64], identb[:])
                        P.mm(ps[5][0:64, hs], Am[h][:, 64:128], identb[:])
                    for h in H4:
                        hs = slice(h * 128, (h + 1) * 128)
                        P.I("act", "activation", out=um[h][:, 0, :], in_=ps[1][0:64, hs], func=AF.Copy)
                        P.I("dve", "tensor_copy", out=um[h][:, 1, :], in_=ps[2][0:64, hs])
                        P.I("act", "activation", out=wT[h][:], in_=ps[3][:, hs], func=AF.Copy)
                        P.I("dve", "tensor_copy", out=ATm[h][:, 0, :], in_=ps[4][0:64, hs])
                        P.I("act", "activation", out=ATm[h][:, 1, :], in_=ps[5][0:64, hs], func=AF.Copy)
                    for c in ([0, 1] if dr == 0 else [1, 0]):
                        cs = slice(c * 64, (c + 1) * 64)
                        for h in H4:
                            hs = slice(h * 128, (h + 1) * 128)
                            P.mm(ps[6][0:64, hs], wT[h][:, cs], Sst[h][:])
                            P.mm(ps[7][0:64, hs], qT[:, h, cs], Sst[h][:])
                        for h in H4:
                            hs = slice(h * 128, (h + 1) * 128)
                            P.I("dve", "tensor_tensor", out=vnew[h][:], in0=um[h][:, c, :], in1=ps[6][0:64, hs], op=ALU.subtract)
                        for h in H4:
                            hs = slice(h * 128, (h + 1) * 128)
                            P.mm(ps[1][0:64, hs], ATm[h][:, c, cs], vnew[h][:])
                            P.mm(ps[2][:, hs], kdec[h][:, c, :], vnew[h][:])
                        for h in H4:
                            hs = slice(h * 128, (h + 1) * 128)
                            P.I("act", "activation", out=avs[h][:], in_=ps[1][0:64, hs], func=AF.Copy)
                            P.I("dve", "scalar_tensor_tensor", out=osb[:, c, hs], in0=ps[7][0:64, hs], scalar=egcc[:, c, h:h + 1],
                                in1=avs[h][:], op0=ALU.mult, op1=ALU.add)
                            P.I("dve", "scalar_tensor_tensor", out=Sst[h][:], in0=Sst[h][:], scalar=gtc[:, c, h:h + 1],
                                in1=ps[2][:, hs], op0=ALU.mult, op1=ALU.add)
                    P.dma("sp", o_dir[dr, t0:t0 + 128, :].rearrange("(c p) d -> p c d", p=64), osb[:])
        _zero[0] = False
        P.barrier()
        if stop_after == f"D2_{l}":
            return finish(nc, P, out_dram)

        TILES = [(t0, 512, 0, 0, SEQ) for t0 in range(0, SEQ, 512)] + [(SEQ, 256, 1, SEQ, NT)]
        with ExitStack() as st:
            of = sb(st, "d3of", [128, 4, 512])
            ob = sb(st, "d3ob", [128, 4, 512])
            osq = sb(st, "d3sq", [128, 4, 512])
            ms = sb(st, "d3ms", [128, 16])
            zt = sb(st, "d3z", [128, 4, 512])
            ydn = sb(st, "d3y", [128, 4, 512], BF16)
            for (t0, tw, s, s0, s1) in TILES:
                nb = tw // 128
                P.dma("sp", of[:, 0:nb, :], o_dir[0, t0:t0 + tw, :].rearrange("(b p) d -> p b d", p=128))
                P.dma("act", ob[:, 0:nb, :], o_dir[1, t0:t0 + tw, :].rearrange("(b p) d -> p b d", p=128))
                P.dma("sp", zt[:, :, 0:tw], projT[R_Z:R_Z + 512, :].rearrange("(c p) t -> p c t", p=128)[:, :, t0:t0 + tw])
                P.I("dve", "tensor_tensor", out=of[:, 0:nb, :], in0=of[:, 0:nb, :], in1=ob[:, 0:nb, :], op=ALU.add)
                P.I("dve", "tensor_tensor", out=osq[:, 0:nb, :], in0=of[:, 0:nb, :], in1=of[:, 0:nb, :], op=ALU.mult)
                P.I("dve", "reduce_sum", out=ms[:, 0:nb * 4], in_=osq[:, 0:nb, :].rearrange("p b (h d) -> p (b h) d", h=4), axis=mybir.AxisListType.X)
                P.I("act", "activation", out=ms[:, 0:nb * 4], in_=ms[:, 0:nb * 4], func=AF.Sqrt, scale=1.0 / 128, bias=EPS)
                P.I("dve", "reciprocal", out=ms[:, 0:nb * 4], in_=ms[:, 0:nb * 4])
                P.I("dve", "tensor_tensor", out=of[:, 0:nb, :].rearrange("p b (h d) -> p (b h) d", h=4),
                    in0=of[:, 0:nb, :].rearrange("p b (h d) -> p (b h) d", h=4),
                    in1=ms[:, 0:nb * 4].unsqueeze(2).to_broadcast([128, nb * 4, 128]), op=ALU.mult)
                P.I("act", "activation", out=zt[:, :, 0:tw], in_=zt[:, :, 0:tw], func=AF.Silu)
                for h in range(4):
                    pp = ps[h % 2]
                    for b in range(nb):
                        P.tr(pp[:, b * 128:(b + 1) * 128], of[:, b, h * 128:(h + 1) * 128], ident[:])
                    P.I("dve", "scalar_tensor_tensor", out=ydn[:, h, 0:tw], in0=pp[:, 0:tw], scalar=vv[:, V_DNG:V_DNG + 1],
                        in1=zt[:, h, 0:tw], op0=ALU.mult, op1=ALU.mult)
                P.dma("sp", y_dnT.rearrange("(c p) t -> p c t", p=128)[:, :, t0:t0 + tw], ydn[:, :, 0:tw])
        P.barrier()

        with ExitStack() as st:
            shh = sb(st, "p3sh", [128, 4, 514])
            scc = sb(st, "p3sc", [128, 4, 514])
            sbb = sb(st, "p3sb", [128, 4, 512])
            acc3 = sb(st, "p3acc", [128, 4, 512])
            ysc = sb(st, "p3y", [128, 4, 512], BF16)
            for (t0, tw, s, s0, s1) in TILES:
                lo, hi = max(t0 - 1, s0), min(t0 + tw + 1, s1)
                for tt, r0 in ((shh, R_SH), (scc, R_SC)):
                    P.I("dve", "memset", ap=tt[:, :, 0:1], constant=0.0)
                    P.I("dve", "memset", ap=tt[:, :, tw + 1:tw + 2], constant=0.0)
                    P.dma("sp", tt[:, :, lo - (t0 - 1):hi - (t0 - 1)],
                          projT[r0:r0 + 512, :].rearrange("(c p) t -> p c t", p=128)[:, :, lo:hi])
                P.dma("act", sbb[:, :, 0:tw], projT[R_SB:R_SB + 512, :].rearrange("(c p) t -> p c t", p=128)[:, :, t0:t0 + tw])
                P.I("dve", "tensor_tensor", out=shh[:, :, 0:tw + 2], in0=shh[:, :, 0:tw + 2], in1=scc[:, :, 0:tw + 2], op=ALU.mult)
                for j in range(4):
                    P.I("dve", "tensor_scalar", out=acc3[:, j, 0:tw], in0=shh[:, j, 0:tw], scalar1=vv[:, V_SCCW + j:V_SCCW + j + 1],
                        scalar2=None, op0=ALU.mult)
                    P.I("dve", "scalar_tensor_tensor", out=acc3[:, j, 0:tw], in0=shh[:, j, 1:tw + 1], scalar=vv[:, V_SCCW + 4 + j:V_SCCW + 5 + j],
                        in1=acc3[:, j, 0:tw], op0=ALU.mult, op1=ALU.add)
                    P.I("dve", "scalar_tensor_tensor", out=acc3[:, j, 0:tw], in0=shh[:, j, 2:tw + 2], scalar=vv[:, V_SCCW + 8 + j:V_SCCW + 9 + j],
                        in1=acc3[:, j, 0:tw], op0=ALU.mult, op1=ALU.add)
                P.I("dve", "tensor_tensor", out=ysc[:, :, 0:tw], in0=acc3[:, :, 0:tw], in1=sbb[:, :, 0:tw], op=ALU.mult)
                P.dma("sp", y_scT.rearrange("(c p) t -> p c t", p=128)[:, :, t0:t0 + tw], ysc[:, :, 0:tw])
        P.barrier()
        if stop_after == f"P3_{l}":
            return finish(nc, P, out_dram)

        with ExitStack() as st:
            wqn = sb(st, "wqn", [128, 2, 4, 128], BF16)
            wqr = sb(st, "wqr", [128, 2, 4, 64], BF16)
            wkn = sb(st, "wkn", [128, 4, 128], BF16)
            wkv = sb(st, "wkv", [128, 4, 128], BF16)
            wq_v = mla_w_qb_in[l].rearrange("(kc p) (h x) -> p kc h x", p=128, x=192)
            for kc in range(2):
                P.dma("pool", wqn[:, kc, :, :], wq_v[:, kc, :, 0:128])
                P.dma("pool", wqr[:, kc, :, :], wq_v[:, kc, :, 128:192])
            wk_v = mla_w_kvb_in[l].rearrange("p (h x) -> p h x", x=256)
            P.dma("pool", wkn[:], wk_v[:, :, 0:128])
            P.dma("pool", wkv[:], wk_v[:, :, 128:256])
            qa = sb(st, "p4qa", [128, 2, 512])
            qsq = sb(st, "p4qsq", [128, 2, 512])
            rr = sb(st, "p4rr", [128, 512])
            qan = sb(st, "p4qan", [128, 2, 512], BF16)
            qn_o = sb(st, "p4qn", [128, 4, 512], BF16)
            qr_o = sb(st, "p4qr", [128, 2, 512], BF16)
            xr = sb(st, "p4xr", [128, 512])
            t1 = sb(st, "p4t1", [128, 512])
            t2 = sb(st, "p4t2", [128, 512])
            rc = sb(st, "p4rc", [128, 512])
            rs = sb(st, "p4rs", [128, 512])
            ckv = sb(st, "p4ckv", [128, 512])
            ckvn = sb(st, "p4ckvn", [128, 512], BF16)
            kn_o = sb(st, "p4kn", [128, 4, 512], BF16)
            v_o = sb(st, "p4v", [128, 4, 512], BF16)
            kr = sb(st, "p4kr", [128, 512])
            kr_o = sb(st, "p4kro", [128, 512], BF16)
            for (t0, tw, s, s0, s1) in TILES:
                nb = tw // 128
                P.dma("sp", qa[:, :, 0:tw], projT[R_QA:R_QA + 256, :].rearrange("(c p) t -> p c t", p=128)[:, :, t0:t0 + tw])
                P.dma("act", ckv[:, 0:tw], projT[R_CKV:R_CKV + 128, t0:t0 + tw])
                P.dma("sp", kr[0:64, 0:tw], projT[R_KR:R_KR + 64, t0:t0 + tw])
                P.dma("sp", kr[64:128, 0:tw], projT[R_KR:R_KR + 64, t0:t0 + tw])
                P.dma("act", rc[:, 0:tw], rope_in[0, :, t0:t0 + tw])
                P.dma("act", rs[:, 0:tw], rope_in[1, :, t0:t0 + tw])
                P.I("act", "activation", out=qsq[:, :, 0:tw], in_=qa[:, :, 0:tw], func=AF.Square)
                for c in range(2):
                    P.mm(ps[0][:, 0:tw], ones[:], qsq[:, c, 0:tw], start=(c == 0), stop=(c == 1))
                P.I("act", "activation", out=rr[:, 0:tw], in_=ps[0][:, 0:tw], func=AF.Sqrt, scale=1.0 / 256, bias=EPS)
                P.I("dve", "reciprocal", out=rr[:, 0:tw], in_=rr[:, 0:tw])
                for c in range(2):
                    P.I("dve", "scalar_tensor_tensor", out=qan[:, c, 0:tw], in0=qa[:, c, 0:tw], scalar=vv[:, V_QNG + c:V_QNG + c + 1],
                        in1=rr[:, 0:tw], op0=ALU.mult, op1=ALU.mult)
                for h in range(4):
                    pp = ps[1 + h % 2]
                    for kc in range(2):
                        P.mm(pp[:, 0:tw], wqn[:, kc, h, :], qan[:, kc, 0:tw], start=(kc == 0), stop=(kc == 1))
                    P.I("act", "activation", out=qn_o[:, h, 0:tw], in_=pp[:, 0:tw], func=AF.Copy)
                for r in range(2):
                    pp = ps[3]
                    for kc in range(2):
                        P.mm(pp[:, 0:tw], wqr[:, kc, 2 * r:2 * r + 2, :], qan[:, kc, 0:tw], start=(kc == 0), stop=(kc == 1))
                    P.I("act", "activation", out=xr[:, 0:tw], in_=pp[:, 0:tw], func=AF.Copy)
                    P.mm(ps[4][:, 0:tw], ropeP[:], xr[:, 0:tw])
                    P.I("dve", "tensor_tensor", out=t1[:, 0:tw], in0=xr[:, 0:tw], in1=rc[:, 0:tw], op=ALU.mult)
                    P.I("dve", "tensor_tensor", out=t2[:, 0:tw], in0=ps[4][:, 0:tw], in1=rs[:, 0:tw], op=ALU.mult)
                    P.I("dve", "tensor_tensor", out=qr_o[:, r, 0:tw], in0=t1[:, 0:tw], in1=t2[:, 0:tw], op=ALU.add)
                P.dma("sp", qnopeT.rearrange("(c p) t -> p c t", p=128)[:, :, t0:t0 + tw], qn_o[:, :, 0:tw])
                P.dma("sp", qropeT.rearrange("(c p) t -> p c t", p=128)[:, :, t0:t0 + tw], qr_o[:, :, 0:tw])
                P.I("act", "activation", out=t1[:, 0:tw], in_=ckv[:, 0:tw], func=AF.Square)
                P.mm(ps[0][:, 0:tw], ones[:], t1[:, 0:tw])
                P.I("act", "activation", out=rr[:, 0:tw], in_=ps[0][:, 0:tw], func=AF.Sqrt, scale=1.0 / 128, bias=EPS)
                P.I("dve", "reciprocal", out=rr[:, 0:tw], in_=rr[:, 0:tw])
                P.I("dve", "scalar_tensor_tensor", out=ckvn[:, 0:tw], in0=ckv[:, 0:tw], scalar=vv[:, V_KVNG:V_KVNG + 1],
                    in1=rr[:, 0:tw], op0=ALU.mult, op1=ALU.mult)
                for h in range(4):
                    pp = ps[1 + h % 2]
                    P.mm(pp[:, 0:tw], wkn[:, h, :], ckvn[:, 0:tw])
                    P.I("act", "activation", out=kn_o[:, h, 0:tw], in_=pp[:, 0:tw], func=AF.Copy)
                for b in range(nb):
                    pp = ps[5 + b % 2]
                    P.mm(pp[:, :], ckvn[:, b * 128:(b + 1) * 128], wkv[:].rearrange("p h d -> p (h d)"))
                    P.I("act", "activation", out=v_o[:, b, :], in_=pp[:, :], func=AF.Copy)
                P.dma("sp", knopeT.rearrange("(c p) t -> p c t", p=128)[:, :, t0:t0 + tw], kn_o[:, :, 0:tw])
                P.dma("sp", v_mla[t0:t0 + tw, :].rearrange("(b p) d -> p b d", p=128), v_o[:, 0:nb, :])
                P.mm(ps[4][:, 0:tw], ropeP[:], kr[:, 0:tw])
                P.I("dve", "tensor_tensor", out=t1[:, 0:tw], in0=kr[:, 0:tw], in1=rc[:, 0:tw], op=ALU.mult)
                P.I("dve", "tensor_tensor", out=t2[:, 0:tw], in0=ps[4][:, 0:tw], in1=rs[:, 0:tw], op=ALU.mult)
                P.I("dve", "tensor_tensor", out=kr_o[:, 0:tw], in0=t1[:, 0:tw], in1=t2[:, 0:tw], op=ALU.add)
                P.dma("sp", kropeT[:, t0:t0 + tw], kr_o[:, 0:tw])
        P.barrier()
        if stop_after == f"P4a_{l}":
            return finish(nc, P, out_dram)

        with ExitStack() as st:
            kn_all = sb(st, "kn_all", [128, 4, NT], BF16)
            kr_all = sb(st, "kr_all", [128, NT], BF16)
            v_all = sb(st, "v_all", [128, 34, 512], BF16)
            onesb = sb(st, "onesb", [128, 128], BF16)
            P.I("dve", "tensor_copy", out=onesb[:], in_=ones[:])
            P.dma("sp", kn_all[:], knopeT.rearrange("(c p) t -> p c t", p=128))
            P.dma("act", kr_all[:], kropeT[:, :])
            P.dma("sp", v_all[:], v_mla.rearrange("(b p) d -> p b d", p=128))
            qn_t = [sb(st, f"qn_t{i}", [128, 4, 512], BF16) for i in range(2)]
            qr_t = [sb(st, f"qr_t{i}", [128, 2, 512], BF16) for i in range(2)]
            ptb = [sb(st, f"ptb{i}", [128, 512], BF16) for i in range(2)]
            rinv = sb(st, "rinv", [128, 512])
            ym = sb(st, "ym", [128, 4, 512], BF16)
            SCALE = 192.0 ** -0.5
            for ti, (t0, tw, s, s0, s1) in enumerate(TILES):
                kbs = list(range(34)) if s == 0 else [32, 33]
                qn_ = qn_t[ti % 2]
                qr_ = qr_t[ti % 2]
                P.dma("sp", qn_[:, :, 0:tw], qnopeT.rearrange("(c p) t -> p c t", p=128)[:, :, t0:t0 + tw])
                P.dma("act", qr_[:, :, 0:tw], qropeT.rearrange("(c p) t -> p c t", p=128)[:, :, t0:t0 + tw])
                for h in range(4):
                    po = ps[2 + (h % 2) * 2]
                    pl = ps[3 + (h % 2) * 2]
                    hp = (h % 2) * 64

                    def emit_st(i, kb):
                        pst = ps[i % 2]
                        ks = slice(kb * 128, (kb + 1) * 128)
                        P.mm(pst[:, 0:tw], kn_all[:, h, ks], qn_[:, h, 0:tw], start=True, stop=False)
                        P.mm(pst[:, 0:tw], kr_all[hp:hp + 64, ks], qr_[hp:hp + 64, h // 2, 0:tw], start=False, stop=True)

                    emit_st(0, kbs[0])
                    for i, kb in enumerate(kbs):
                        if i + 1 < len(kbs):
                            emit_st(i + 1, kbs[i + 1])
                        pt_ = ptb[i % 2]
                        P.I("act", "activation", out=pt_[:, 0:tw], in_=ps[i % 2][:, 0:tw], func=AF.Exp, scale=SCALE)
                        first, last = (i == 0), (i == len(kbs) - 1)
                        P.mm(po[:, 0:tw], v_all[:, kb, h * 128:(h + 1) * 128], pt_[:, 0:tw], start=first, stop=last)
                        P.mm(pl[:, 0:tw], onesb[:], pt_[:, 0:tw], start=first, stop=last)
                    P.I("dve", "reciprocal", out=rinv[:, 0:tw], in_=pl[:, 0:tw])
                    P.I("dve", "tensor_tensor", out=ym[:, h, 0:tw], in0=po[:, 0:tw], in1=rinv[:, 0:tw], op=ALU.mult)
                P.dma("sp", y_mlaT.rearrange("(c p) t -> p c t", p=128)[:, :, t0:t0 + tw], ym[:, :, 0:tw])
        P.barrier()
        if stop_after == f"P4_{l}":
            return finish(nc, P, out_dram)

        last = (l == DEPTH - 1)
        PT = [t for t in TILES if not (last and t[2] == 1)]
        with ExitStack() as st:
            wg = sb(st, "wg", [128, 8, 3072], BF16)
            wbr = [sb(st, f"wbr{i}", [128, 4, 1024], BF16) for i in range(3)]
            wo = sb(st, "wo", [128, 8, 1024], BF16)
            wr32 = sb(st, "wr32", [128, 8, 32])
            P.dma("pool", wg[:], w_gate_in[l].rearrange("(kc p) n -> p kc n", p=128))
            for i, wsrc in enumerate((w_dn_in, w_sc_in, w_mla_in)):
                P.dma("pool", wbr[i][:], wsrc[l].rearrange("(kc p) n -> p kc n", p=128))
            P.dma("pool", wo[:], w_out_in[l].rearrange("(kc p) n -> p kc n", p=128))
            P.dma("sp", wr32[:], router_w_in[l].rearrange("(kc p) n -> p kc n", p=128))
            xb5 = sb(st, "p5xb", [128, 8, 512], BF16)
            ybr = [sb(st, f"p5y{i}", [128, 4, 512], BF16) for i in range(3)]
            ht5 = sb(st, "p5ht", [128, 8, 512])
            sig5 = [sb(st, f"p5sig{i}", [128, 512]) for i in range(2)]
            mrg = sb(st, "p5mrg", [128, 512])
            tm5 = sb(st, "p5tm", [128, 512])
            mg = sb(st, "p5mg", [128, 8, 512], BF16)
            sq5 = sb(st, "p5sq", [128, 8, 512], BF16)
            rs5 = sb(st, "p5rs", [128, 512])
            x32 = sb(st, "p5x32", [128, 8, 512])
            x2b = sb(st, "p5x2b", [128, 8, 512], BF16)
            lg = sb(st, "p5lg", [128, 4, 32])
            mx8 = sb(st, "p5mx", [128, 8])
            msk = sb(st, "p5msk", [128, 32])
            ee = sb(st, "p5e", [128, 32])
            sm = sb(st, "p5sm", [128, 2])
            gts = sb(st, "p5gts", [128, 4, 32])
            gTs = sb(st, "p5gT", [32, 512])
            nsig = 0
            for (t0, tw, s, s0, s1) in PT:
                nb = tw // 128
                P.dma("sp", xb5[:, :, 0:tw], xmT.rearrange("(c p) t -> p c t", p=128)[:, :, t0:t0 + tw])
                for i, ysrc in enumerate((y_dnT, y_scT, y_mlaT)):
                    P.dma("act", ybr[i][:, :, 0:tw], ysrc.rearrange("(c p) t -> p c t", p=128)[:, :, t0:t0 + tw])
                P.dma("sp", ht5[:, :, 0:tw], hT.rearrange("(c p) t -> p c t", p=128)[:, :, t0:t0 + tw])
                for c in range(8):
                    for br in range(3):
                        pgt = ps[br % 2]
                        for k in range(8):
                            P.mm(pgt[:, 0:tw], wg[:, k, br * 1024 + c * 128:br * 1024 + (c + 1) * 128], xb5[:, k, 0:tw], start=(k == 0), stop=(k == 7))
                        sg = sig5[nsig % 2]
                        nsig += 1
                        P.I("act", "activation", out=sg[:, 0:tw], in_=pgt[:, 0:tw], func=AF.Sigmoid,
                            bias=vv[:, V_BG + br * 8 + c:V_BG + br * 8 + c + 1])
                        ppr = ps[2 + br % 2]
                        for k in range(4):
                            P.mm(ppr[:, 0:tw], wbr[br][:, k, c * 128:(c + 1) * 128], ybr[br][:, k, 0:tw], start=(k == 0), stop=(k == 3))
                        if br == 0:
                            P.I("dve", "tensor_tensor", out=mrg[:, 0:tw], in0=ppr[:, 0:tw], in1=sg[:, 0:tw], op=ALU.mult)
                        else:
                            P.I("dve", "tensor_tensor", out=tm5[:, 0:tw], in0=ppr[:, 0:tw], in1=sg[:, 0:tw], op=ALU.mult)
                            if br == 1:
                                P.I("dve", "tensor_tensor", out=mrg[:, 0:tw], in0=mrg[:, 0:tw], in1=tm5[:, 0:tw], op=ALU.add)
                            else:
                                P.I("dve", "tensor_tensor", out=mg[:, c, 0:tw], in0=mrg[:, 0:tw], in1=tm5[:, 0:tw], op=ALU.add)
                for c in range(8):
                    pp = ps[4 + c % 2]
                    for k in range(8):
                        P.mm(pp[:, 0:tw], wo[:, k, c * 128:(c + 1) * 128], mg[:, k, 0:tw], start=(k == 0), stop=(k == 7))
                    P.I("dve", "scalar_tensor_tensor", out=ht5[:, c, 0:tw], in0=pp[:, 0:tw], scalar=m[:, s, 2, c:c + 1],
                        in1=ht5[:, c, 0:tw], op0=ALU.mult, op1=ALU.add)
                P.dma("sp", hT.rearrange("(c p) t -> p c t", p=128)[:, :, t0:t0 + tw], ht5[:, :, 0:tw])
                P.I("act", "activation", out=x32[:, :, 0:tw], in_=ht5[:, :, 0:tw], func=AF.Square)
                for c in range(8):
                    P.mm(ps[6][:, 0:tw], ones[:], x32[:, c, 0:tw], start=(c == 0), stop=(c == 7))
                P.I("act", "activation", out=rs5[:, 0:tw], in_=ps[6][:, 0:tw], func=AF.Sqrt, scale=1.0 / D, bias=EPS)
                P.I("dve", "reciprocal", out=rs5[:, 0:tw], in_=rs5[:, 0:tw])
                for c in range(8):
                    P.I("dve", "scalar_tensor_tensor", out=tm5[:, 0:tw], in0=ht5[:, c, 0:tw], scalar=m[:, s, 3, c:c + 1],
                        in1=rs5[:, 0:tw], op0=ALU.mult, op1=ALU.mult)
                    P.I("act", "activation", out=x32[:, c, 0:tw], in_=tm5[:, 0:tw], func=AF.Identity, bias=m[:, s, 4, c:c + 1])
                    P.I("dve", "tensor_copy", out=x2b[:, c, 0:tw], in_=x32[:, c, 0:tw])
                P.dma("sp", xm2T.rearrange("(c p) t -> p c t", p=128)[:, :, t0:t0 + tw], x2b[:, :, 0:tw])
                for b in range(nb):
                    for c in range(8):
                        P.mm(ps[7][:, b * 32:(b + 1) * 32], x32[:, c, b * 128:(b + 1) * 128], wr32[:, c, :], start=(c == 0), stop=(c == 7))
                P.I("dve", "tensor_tensor", out=lg[:, 0:nb, :], in0=ps[7][:, 0:nb * 32].rearrange("p (b e) -> p b e", e=32),
                    in1=vv[:, V_RB:V_RB + 32].unsqueeze(1).to_broadcast([128, nb, 32]), op=ALU.add)
                for b in range(nb):
                    P.I("dve", "max", out=mx8[:], in_=lg[:, b, :])
                    P.I("dve", "tensor_scalar", out=msk[:], in0=lg[:, b, :], scalar1=mx8[:, 3:4], scalar2=None, op0=ALU.is_ge)
                    P.I("dve", "tensor_scalar", out=sm[:, 0:1], in0=mx8[:, 0:1], scalar1=-1.0, scalar2=None, op0=ALU.mult)
                    P.I("act", "activation", out=ee[:], in_=lg[:, b, :], func=AF.Exp, bias=sm[:, 0:1])
                    P.I("dve", "tensor_tensor", out=ee[:], in0=ee[:], in1=msk[:], op=ALU.mult)
                    P.I("dve", "reduce_sum", out=sm[:, 1:2], in_=ee[:], axis=mybir.AxisListType.X)
                    P.I("dve", "reciprocal", out=sm[:, 1:2], in_=sm[:, 1:2])
                    P.I("dve", "tensor_scalar", out=gts[:, b, :], in0=ee[:], scalar1=sm[:, 1:2], scalar2=None, op0=ALU.mult)
                for b in range(nb):
                    P.tr(ps[6][0:32, b * 128:(b + 1) * 128], gts[:, b, :], ident[:])
                P.I("dve", "tensor_copy", out=gTs[:, 0:tw], in_=ps[6][0:32, 0:tw])
                P.dma("sp", gatesT[:, t0:t0 + tw], gTs[:, 0:tw])
        P.barrier()
        if stop_after == f"P5_{l}":
            return finish(nc, P, out_dram)

        with ExitStack() as st:
            GMAX = 1024
            w1b = [sb(st, f"w1b{i}", [128, 8, 2048], BF16) for i in range(2)]
            w2b = [sb(st, f"w2b{i}", [128, 8, 1024], BF16) for i in range(2)]
            yacc = sb(st, "yacc", [128, 8, GMAX])
            xg = sb(st, "xg", [128, 8, GMAX], BF16)
            gTg = sb(st, "gTg", [32, GMAX])
            actb = [sb(st, f"actb{i}", [128, 8, 512], BF16) for i in range(2)]
            gbb = [sb(st, f"m_gb{i}", [128, 512]) for i in range(2)]
            a1b = [sb(st, f"m_a1{i}", [128, 512]) for i in range(2)]
            a2b = [sb(st, f"m_a2{i}", [128, 512]) for i in range(2)]
            glub = [sb(st, f"m_glu{i}", [128, 512]) for i in range(2)]
            sgb = [sb(st, f"m_sig{i}", [128, 512]) for i in range(2)]
            linb = [sb(st, f"m_lin{i}", [128, 512]) for i in range(2)]
            lin = linb[0]
            nfc = 0
            htc = [sb(st, f"m_htc{i}", [128, 512]) for i in range(2)]
            selE = sb(st, "selE", [32, 128])
            b2sb = sb(st, "b2sb", [32, 1024])
            P.dma("sp", b2sb[:], expert_b2_in[l])
            pend = [None]

            def y_phase(wb2, ab, o, tw, e):
                for c in range(8):
                    py = ps[4 + c % 2]
                    for fc in range(8):
                        P.mm(py[:, 0:tw], wb2[:, fc, c * 128:(c + 1) * 128], ab[:, fc, 0:tw], start=(fc == 0), stop=(fc == 7))
                    if e == 0:
                        P.I("dve", "tensor_copy", out=yacc[:, c, o:o + tw], in_=py[:, 0:tw])
                    else:
                        P.I("dve", "tensor_tensor", out=yacc[:, c, o:o + tw], in0=py[:, 0:tw], in1=yacc[:, c, o:o + tw], op=ALU.add)

            groups = [(0, 1024), (1024, 1024), (2048, 1024), (3072, 1024)] + ([] if last else [(SEQ, 256)])
            groups = groups[:int(_os.environ.get("KDBG_GROUPS", "100"))]
            nact = 0
            for (g0, gw) in groups:
                s = 1 if g0 >= SEQ else 0
                P.dma("sp", xg[:, :, 0:gw], xm2T.rearrange("(c p) t -> p c t", p=128)[:, :, g0:g0 + gw])
                P.dma("sp", gTg[:, 0:gw], gatesT[:, g0:g0 + gw])
                gtiles = [(o, min(512, gw - o)) for o in range(0, gw, 512)]
                for e in range(ne):
                    wb1, wb2 = w1b[e % 2], w2b[e % 2]
                    P.dma("pool", wb1[:], w1_in[l, e].rearrange("(kc p) n -> p kc n", p=128))
                    P.dma("pool", wb2[:], w2_in[l, e].rearrange("(kc p) n -> p kc n", p=128))
                    P.I("dve", "tensor_scalar", out=selE[:], in0=ones[0:32, :], scalar1=ident[0:32, e:e + 1], scalar2=None, op0=ALU.mult)
                    w1v = wb1[:].rearrange("p k (f two) -> p k f two", two=2)
                    for (o, tw) in gtiles:
                        ab = actb[nact % 2]
                        gb = gbb[nact % 2]
                        nact += 1
                        P.mm(ps[6][:, 0:tw], selE[:], gTg[:, o:o + tw])
                        P.I("act", "activation", out=gb[:, 0:tw], in_=ps[6][:, 0:tw], func=AF.Copy)
                        for fc in range(8):
                            pg_, pl_ = ps[(fc % 2) * 2], ps[(fc % 2) * 2 + 1]
                            for k in range(8):
                                P.mm(pg_[:, 0:tw], w1v[:, k, fc * 128:(fc + 1) * 128, 0], xg[:, k, o:o + tw], start=(k == 0), stop=(k == 7))
                            for k in range(8):
                                P.mm(pl_[:, 0:tw], w1v[:, k, fc * 128:(fc + 1) * 128, 1], xg[:, k, o:o + tw], start=(k == 0), stop=(k == 7))
                            a1, a2, gl_, sg_, ln_ = a1b[nfc % 2], a2b[nfc % 2], glub[nfc % 2], sgb[nfc % 2], linb[nfc % 2]
                            nfc += 1
                            P.I("act", "activation", out=a1[:, 0:tw], in_=pg_[:, 0:tw], func=AF.Identity,
                                bias=vv[:, V_B1G + e * 8 + fc:V_B1G + e * 8 + fc + 1])
                            P.I("act", "activation", out=a2[:, 0:tw], in_=pl_[:, 0:tw], func=AF.Identity,
                                bias=vv[:, V_B1L + e * 8 + fc:V_B1L + e * 8 + fc + 1])
                            P.I("dve", "tensor_scalar_min", out=gl_[:, 0:tw], in0=a1[:, 0:tw], scalar1=7.0)
                            P.I("act", "activation", out=sg_[:, 0:tw], in_=gl_[:, 0:tw], func=AF.Sigmoid, scale=1.702)
                            P.I("dve", "tensor_scalar", out=ln_[:, 0:tw], in0=a2[:, 0:tw], scalar1=7.0, scalar2=-7.0, op0=ALU.min, op1=ALU.max)
                            P.I("dve", "scalar_tensor_tensor", out=ln_[:, 0:tw], in0=ln_[:, 0:tw], scalar=1.0, in1=gl_[:, 0:tw], op0=ALU.add, op1=ALU.mult)
                            P.I("dve", "tensor_tensor", out=ln_[:, 0:tw], in0=ln_[:, 0:tw], in1=sg_[:, 0:tw], op=ALU.mult)
                            P.I("dve", "tensor_tensor", out=ab[:, fc, 0:tw], in0=ln_[:, 0:tw], in1=gb[:, 0:tw], op=ALU.mult)
                        if pend[0] is not None:
                            y_phase(*pend[0])
                        pend[0] = (wb2, ab, o, tw, e)
                if pend[0] is not None:
                    y_phase(*pend[0])
                    pend[0] = None
                if "yaccT" in dbg:
                    P.dma("sp", yaccT.rearrange("(c p) t -> p c t", p=128)[:, :, g0:g0 + gw], yacc[:, :, 0:gw])
                nh = 0
                for (o, tw) in gtiles:
                    for c in range(8):
                        hc = htc[nh % 2]
                        nh += 1
                        P.dma("sp", hc[:, 0:tw], hT[c * 128:(c + 1) * 128, g0 + o:g0 + o + tw])
                        P.mm(ps[7][:, 0:tw], b2sb[:, c * 128:(c + 1) * 128], gTg[:, o:o + tw])
                        P.I("dve", "tensor_tensor", out=lin[:, 0:tw], in0=ps[7][:, 0:tw], in1=yacc[:, c, o:o + tw], op=ALU.add)
                        P.I("dve", "scalar_tensor_tensor", out=hc[:, 0:tw], in0=lin[:, 0:tw], scalar=m[:, s, 5, c:c + 1],
                            in1=hc[:, 0:tw], op0=ALU.mult, op1=ALU.add)
                        P.dma("sp", hT[c * 128:(c + 1) * 128, g0 + o:g0 + o + tw], hc[:, 0:tw])
        P.barrier()
        if stop_after == f"P6_{l}":
            return finish(nc, P, out_dram)

    with ExitStack() as st:
        vv = vecs[DEPTH - 1]
        htf = [sb(st, f"f_ht{i}", [128, 8, 512]) for i in range(2)]
        sqf = sb(st, "f_sq", [128, 8, 512])
        rsf = sb(st, "f_rs", [128, 512])
        xnf = sb(st, "f_xn", [128, 8, 512])
        otl = [sb(st, f"f_o{i}", [128, 1024]) for i in range(2)]
        no = 0
        for ti, t0 in enumerate(range(0, SEQ, 512)):
            ht = htf[ti % 2]
            P.dma("sp", ht[:], hT.rearrange("(c p) t -> p c t", p=128)[:, :, t0:t0 + 512])
            P.I("act", "activation", out=sqf[:], in_=ht[:], func=AF.Square)
            for c in range(8):
                P.mm(ps[0][:], ones[:], sqf[:, c, :], start=(c == 0), stop=(c == 7))
            P.I("act", "activation", out=rsf[:], in_=ps[0][:], func=AF.Sqrt, scale=1.0 / D, bias=EPS)
            P.I("dve", "reciprocal", out=rsf[:], in_=rsf[:])
            for c in range(8):
                P.I("dve", "scalar_tensor_tensor", out=xnf[:, c, :], in0=ht[:, c, :], scalar=vv[:, V_FNG + c:V_FNG + c + 1],
                    in1=rsf[:], op0=ALU.mult, op1=ALU.mult)
            for b in range(4):
                ot = otl[no % 2]
                no += 1
                pa, pb = ps[1 + (b % 2) * 2], ps[2 + (b % 2) * 2]
                for c in range(8):
                    pp = pa if c < 4 else pb
                    P.tr(pp[:, (c % 4) * 128:(c % 4 + 1) * 128], xnf[:, c, b * 128:(b + 1) * 128], ident[:])
                P.I("act", "activation", out=ot[:, 0:512], in_=pa[:], func=AF.Copy)
                P.I("dve", "tensor_copy", out=ot[:, 512:1024], in_=pb[:])
                P.dma("sp", out_dram[t0 + b * 128:t0 + (b + 1) * 128, :], ot[:])
    return finish(nc, P, out_dram)


def finish(nc, P, out_dram):
    P.barrier(engines=["sp"])
    return nc


def _pcol(v):
    v = np.asarray(v, np.float32)
    return np.ascontiguousarray(v.reshape(-1, 128).T)


def _consts():
    i = np.arange(128)
    same = (i[:, None] // 64) == (i[None, :] // 64)
    ident = np.eye(128, dtype=np.float32)
    U = (same & (i[:, None] <= i[None, :])).astype(np.float32)
    Lo = (same & (i[:, None] >= i[None, :])).astype(np.float32)
    SU = (same & (i[:, None] < i[None, :])).astype(np.float32)
    SLo = (same & (i[:, None] > i[None, :])).astype(np.float32)
    SC0 = np.repeat((i < 64).astype(np.float32)[:, None], 128, axis=1)
    SC1 = np.repeat((i >= 64).astype(np.float32)[:, None], 128, axis=1)
    return np.ascontiguousarray(np.concatenate([ident, U, Lo, SU, SLo, SC0, SC1], axis=1))


def _rope():
    t = np.arange(SEQ)
    row = (t // 64).astype(np.float32)
    col = (t % 64).astype(np.float32)
    inv = (np.float32(10000.0) ** (-np.arange(0, 32, 2, dtype=np.float32) / np.float32(32))).astype(np.float32)
    out = np.zeros((3, 128, NT), np.float32)
    out[0, :, SEQ:] = 1.0
    for r in range(64):
        a, within = r // 32, r % 32
        half, i = within // 16, within % 16
        ang = ((row if a == 0 else col) * inv[i]).astype(np.float32)
        out[0, r, :SEQ] = np.cos(ang)
        out[1, r, :SEQ] = np.sin(ang) * (-1.0 if half == 0 else 1.0)
        partner = r + 16 if half == 0 else r - 16
        out[2, partner, r] = 1.0
        out[2, 64 + partner, 64 + r] = 1.0
    out[0, 64:128] = out[0, 0:64]
    out[1, 64:128] = out[1, 0:64]
    return out


def _vecs(inp, l):
    v = np.zeros((128, NV), np.float32)
    v[:, V_ADAB:V_ADAB + 48] = _pcol(inp["ada_b"][l])
    v[:, V_N1:V_N1 + 8] = _pcol(inp["norm1_g"][l])
    v[:, V_N2:V_N2 + 8] = _pcol(inp["norm2_g"][l])
    v[:, V_BG:V_BG + 24] = _pcol(inp["b_branch_gate"][l])
    for tap in range(3):
        v[:, V_DNCW + tap * 12:V_DNCW + tap * 12 + 12] = _pcol(inp["dn_conv_w"][l][tap])
        v[:, V_SCCW + tap * 4:V_SCCW + tap * 4 + 4] = _pcol(inp["sc_conv_w"][l][tap])
    v[:, V_QNG:V_QNG + 2] = _pcol(inp["mla_q_norm_g"][l])
    v[:, V_KVNG:V_KVNG + 1] = _pcol(inp["mla_kv_norm_g"][l])
    v[:, V_DNG:V_DNG + 1] = _pcol(inp["dn_norm_g"][l])
    v[:, V_ALOG:V_ALOG + 8] = np.asarray(inp["dn_a_log"][l], np.float32).reshape(1, 8)
    v[:, V_DTB:V_DTB + 8] = np.asarray(inp["dn_dt_bias"][l], np.float32).reshape(1, 8)
    v[:, V_RB:V_RB + 32] = np.asarray(inp["router_b"][l], np.float32).reshape(1, 32)
    b1 = np.asarray(inp["expert_b1"][l], np.float32)
    for e in range(NE):
        v[:, V_B1G + e * 8:V_B1G + e * 8 + 8] = _pcol(b1[e, 0::2])
        v[:, V_B1L + e * 8:V_B1L + e * 8 + 8] = _pcol(b1[e, 1::2])
    v[:, V_FNG:V_FNG + 8] = _pcol(inp["final_norm_g"])
    return v


def make_in_maps(inp, names, ne=NE):
    f = lambda a: np.ascontiguousarray(np.asarray(a, np.float32))
    shared = {}
    shared["consts"] = _consts()
    shared["vecs"] = np.stack([_vecs(inp, l) for l in range(DEPTH)])
    shared["rope"] = _rope()
    for k in ["ada_w", "w_in", "mla_w_qb", "mla_w_kvb", "w_branch_gate", "w_branch_dn", "w_branch_sc", "w_branch_mla",
              "w_out", "router_w", "expert_b2"]:
        shared[k] = f(inp[k])
    for k in ["expert_w1", "expert_w2"]:
        if k in names:
            shared[k] = f(np.asarray(inp[k])[:, :ne])
    maps = []
    for b in range(8):
        m = dict(shared)
        m["x"] = f(inp["x"][b])
        m["ctx"] = f(inp["ctx"][b])
        m["cvec"] = np.ascontiguousarray(np.concatenate([_pcol(inp["c"][b]), _pcol(inp["c_ctx"])], axis=1))
        maps.append({k: v for k, v in m.items() if k in names})
    return maps


INPUT_NAMES = ["x", "ctx", "cvec", "consts", "vecs", "ada_w", "w_in", "mla_w_qb", "mla_w_kvb", "rope",
               "w_branch_gate", "w_branch_dn", "w_branch_sc", "w_branch_mla", "w_out", "router_w",
               "expert_w1", "expert_w2", "expert_b2"]


def kernel(**inputs):
    nc = build()
    maps = make_in_maps(inputs, INPUT_NAMES)
    res = run_bass_kernel_spmd(nc, maps, core_ids=list(range(8)))
    return np.stack([np.asarray(r["out"], np.float32) for r in res.results], axis=0)
```

```python
import numpy as np
from contextlib import ExitStack
import concourse.bass as bass
import concourse.mybir as mybir
from concourse.bass_utils import run_bass_kernel_spmd

F32 = mybir.dt.float32
BF16 = mybir.dt.bfloat16
AF = mybir.ActivationFunctionType
ALU = mybir.AluOpType

D = 1024
SEQ = 4096
CTX = 256
NT = SEQ + CTX
NCH = D // 128
DEPTH = 2
IN_COLS = 4048
NE = 32
FF = 1024
EPS = 1e-6

O_Q, O_K, O_V, O_Z, O_A, O_B, O_SH, O_SB, O_SC, O_QA, O_KV = 0, 512, 1024, 1536, 2048, 2056, 2064, 2576, 3088, 3600, 3856
PROJ_SRC = [(O_Q, 512), (O_K, 512), (O_V, 512), (O_Z, 512), (O_SH, 512), (O_SB, 512), (O_SC, 512), (O_QA, 256), (O_KV, 192)]
R_Q, R_K, R_V, R_Z, R_SH, R_SB, R_SC, R_QA, R_CKV, R_KR = 0, 512, 1024, 1536, 2048, 2560, 3072, 3584, 3840, 3968
PROJ_ROWS = 4032

V_ADAB, V_N1, V_N2, V_BG, V_DNCW, V_SCCW, V_QNG, V_KVNG, V_DNG, V_ALOG, V_DTB, V_RB, V_B1G, V_B1L, V_FNG = (
    0, 48, 56, 64, 88, 124, 136, 138, 139, 140, 148, 156, 188, 444, 700)
NV = 708


def _is_ap(v):
    return hasattr(v, "tensor") and hasattr(v, "ap")


class Prog:
    def __init__(self, nc, n_dma=24):
        self.nc = nc
        self.es = ExitStack()
        self.eng = {"pe": nc.tensor, "act": nc.scalar, "dve": nc.vector, "pool": nc.gpsimd, "sp": nc.sync}
        self.semobj = {}
        self.cnt = {}
        for e in ["pe", "act", "dve", "pool"]:
            self.semobj[e] = self.es.enter_context(nc.semaphore(f"s_{e}"))
            self.cnt[e] = 0
        self.ring = {}
        self.rnext = {}
        for q, n in (("sp", 12), ("act", 6), ("pool", 6), ("dve", 2), ("pe", 2)):
            self.ring[q] = []
            self.rnext[q] = 0
            for i in range(n):
                nm = f"d{q}{i}"
                self.semobj[nm] = self.es.enter_context(nc.semaphore(f"s_{nm}"))
                self.cnt[nm] = 0
                self.ring[q].append(nm)
        self.known = {e: {} for e in self.eng}
        self.W = {}
        self.R = {}
        self.nops = 0
        self.mute = False
        self._nm = ""
        import os as _os
        self.trace = bool(_os.environ.get("KDBG_TRACE", ""))
        self.maxops = int(_os.environ.get("KDBG_MAXOPS", "100000000"))

    @staticmethod
    def key(ap):
        return ap.tensor.name

    def op(self, eng, fn, reads=(), writes=(), signal=True, dma=False):
        if self.mute or self.nops >= self.maxops:
            return ("x", 0)
        writes = list(writes) + [k for k in reads if isinstance(k, str) and k.startswith("ps") and k not in writes]
        need = {}
        for k in reads:
            for s, v in self.W.get(k, {}).items():
                if need.get(s, 0) < v:
                    need[s] = v
        for k in writes:
            for s, v in self.W.get(k, {}).items():
                if need.get(s, 0) < v:
                    need[s] = v
            for s, v in self.R.get(k, {}).items():
                if need.get(s, 0) < v:
                    need[s] = v
        e = self.eng[eng]
        kn = self.known[eng]
        for s, v in need.items():
            if eng == "pe" and s == "pe":
                continue
            if kn.get(s, 0) >= v:
                continue
            e.wait_ge(self.semobj[s], v)
            kn[s] = v
        if dma:
            rs_ = self.ring[eng][self.rnext[eng]]
            if self.cnt[rs_] > kn.get(rs_, 0):
                e.wait_ge(self.semobj[rs_], self.cnt[rs_])
                kn[rs_] = self.cnt[rs_]
        ins = fn(e)
        if self.trace:
            print("OP", self.nops, eng, self._nm, list(writes), list(reads))
        self.nops += 1
        if dma:
            s = self.ring[eng][self.rnext[eng]]
            self.rnext[eng] = (self.rnext[eng] + 1) % len(self.ring[eng])
            self.cnt[s] += 16
            ins.then_inc(self.semobj[s], 16)
            ref = (s, self.cnt[s])
        elif signal:
            self.cnt[eng] += 1
            ins.then_inc(self.semobj[eng], 1)
            ref = (eng, self.cnt[eng])
        else:
            ref = (eng, self.cnt[eng] + 1)
        for k in reads:
            d = self.R.setdefault(k, {})
            if d.get(ref[0], 0) < ref[1]:
                d[ref[0]] = ref[1]
        for k in writes:
            d = self.W.setdefault(k, {})
            if d.get(ref[0], 0) < ref[1]:
                d[ref[0]] = ref[1]
        return ref

    def barrier(self, engines=None):
        if self.mute:
            return
        for eng, e in self.eng.items():
            if engines is not None and eng not in engines:
                continue
            kn = self.known[eng]
            for s, v in self.cnt.items():
                if v == 0 or (eng == "pe" and s == "pe"):
                    continue
                if kn.get(s, 0) >= v:
                    continue
                e.wait_ge(self.semobj[s], v)
                kn[s] = v

    def mm(self, out, lhsT, rhs, start=True, stop=True, rk=None, wk=None, **kw):
        reads = rk if rk is not None else [self.key(lhsT), self.key(rhs)]
        writes = wk if wk is not None else [self.key(out)]
        self._nm = "matmul"
        return self.op("pe", lambda e: e.matmul(out, lhsT, rhs, start=start, stop=stop, **kw), reads, writes, signal=stop)

    def tr(self, out, in_, ident, rk=None, wk=None):
        reads = rk if rk is not None else [self.key(in_), self.key(ident)]
        writes = wk if wk is not None else [self.key(out)]
        self._nm = "transpose"
        return self.op("pe", lambda e: e.transpose(out, in_, ident), reads, writes)

    def I(self, eng, meth, rk=None, wk=None, **kw):
        if rk is None:
            rk = [self.key(v) for k_, v in kw.items() if k_ not in ("out", "accum_out", "ap") and _is_ap(v)]
        if wk is None:
            wk = [self.key(kw[k_]) for k_ in ("out", "accum_out", "ap") if k_ in kw and kw[k_] is not None]
        self._nm = meth
        return self.op(eng, lambda e: getattr(e, meth)(**kw), rk, wk)

    def dma(self, q, out, in_, rk=None, wk=None, **kw):
        reads = rk if rk is not None else [self.key(in_)]
        writes = wk if wk is not None else [self.key(out)]
        self._nm = "dma"
        return self.op(q, lambda e: e.dma_start(out=out, in_=in_, **kw), reads, writes, dma=True)


def build(stop_after=None, dbg=(), ne=NE):
    nc = bass.Bass("TRN2", target_bir_lowering=False)
    P = Prog(nc)
    es = P.es
    dbg = set(dbg)

    def dram_in(name, shape, dt=F32):
        return nc.dram_tensor(name, list(shape), dt, kind="ExternalInput").ap()

    def dram_scr(name, shape, dt=F32):
        kind = "ExternalOutput" if name in dbg else "Internal"
        return nc.dram_tensor(name, list(shape), dt, kind=kind).ap()

    _zero = [False]

    _uid = [0]

    def sb(st, name, shape, dt=F32):
        _uid[0] += 1
        t = st.enter_context(nc.sbuf_tensor(f"{name}_u{_uid[0]}", list(shape), dt))
        if _zero[0]:
            P.I("dve", "memset", ap=t[:], constant=0.0)
        return t

    x_in = dram_in("x", [SEQ, D])
    ctx_in = dram_in("ctx", [CTX, D])
    cvec_in = dram_in("cvec", [128, 16])
    consts_in = dram_in("consts", [128, 7 * 128])
    vecs_in = dram_in("vecs", [DEPTH, 128, NV])
    ada_w_in = dram_in("ada_w", [DEPTH, D, 6 * D])
    w_in_in = dram_in("w_in", [DEPTH, D, IN_COLS])
    mla_w_qb_in = dram_in("mla_w_qb", [DEPTH, 256, 768])
    mla_w_kvb_in = dram_in("mla_w_kvb", [DEPTH, 128, 1024])
    w_gate_in = dram_in("w_branch_gate", [DEPTH, D, 3 * D])
    w_dn_in = dram_in("w_branch_dn", [DEPTH, 512, D])
    w_sc_in = dram_in("w_branch_sc", [DEPTH, 512, D])
    w_mla_in = dram_in("w_branch_mla", [DEPTH, 512, D])
    w_out_in = dram_in("w_out", [DEPTH, D, D])
    router_w_in = dram_in("router_w", [DEPTH, D, NE])
    w1_in = dram_in("expert_w1", [DEPTH, ne, D, 2 * FF])
    w2_in = dram_in("expert_w2", [DEPTH, ne, FF, D])
    expert_b2_in = dram_in("expert_b2", [DEPTH, NE, D])
    rope_in = dram_in("rope", [3, 128, NT])
    out_dram = nc.dram_tensor("out", [SEQ, D], F32, kind="ExternalOutput").ap()

    hT = dram_scr("hT", [D, NT])
    xmT = dram_scr("xmT", [D, NT], BF16)
    projT = dram_scr("projT", [PROJ_ROWS, NT])
    ab_tm = dram_scr("ab_tm", [NT, 16])
    mods_d = dram_scr("mods_d", [DEPTH, 128, 96])
    bg_tm = dram_scr("bg_tm", [NT, 16])
    qnT = dram_scr("qnT", [512, NT])
    knT = dram_scr("knT", [512, NT])
    qnTb = dram_scr("qnTb", [512, NT], BF16)
    knTb = dram_scr("knTb", [512, NT], BF16)
    k_tm = dram_scr("k_tm", [NT, 512])
    v_tm = dram_scr("v_tm", [NT, 512])
    o_dir = dram_scr("o_dir", [2, NT, 512])
    y_dnT = dram_scr("y_dnT", [512, NT], BF16)
    y_scT = dram_scr("y_scT", [512, NT], BF16)
    y_mlaT = dram_scr("y_mlaT", [512, NT], BF16)
    qnopeT = dram_scr("qnopeT", [512, NT], BF16)
    qropeT = dram_scr("qropeT", [256, NT], BF16)
    knopeT = dram_scr("knopeT", [512, NT], BF16)
    kropeT = dram_scr("kropeT", [128, NT], BF16)
    v_mla = dram_scr("v_mla", [NT, 512], BF16)
    xm2T = dram_scr("xm2T", [D, NT], BF16)
    gatesT = dram_scr("gatesT", [32, NT])
    yaccT = dram_scr("yaccT", [D, NT])

    ident = sb(es, "ident", [128, 128])
    cU = sb(es, "cU", [128, 128])
    cLo = sb(es, "cLo", [128, 128])
    cSU = sb(es, "cSU", [128, 128])
    cSLo = sb(es, "cSLo", [128, 128])
    cSC0 = sb(es, "cSC0", [128, 128])
    cSC1 = sb(es, "cSC1", [128, 128])
    ones = sb(es, "ones", [128, 128])
    ropeP = sb(es, "ropeP", [128, 128])
    cvec = sb(es, "cvec_sb", [128, 16])
    vecs = [sb(es, f"vecs{l}", [128, NV]) for l in range(DEPTH)]
    mods = [sb(es, f"mods{l}", [128, 2, 6, 8]) for l in range(DEPTH)]
    ps = [es.enter_context(nc.psum_tensor(f"ps{i}", [128, 512], F32)) for i in range(8)]

    import os as _os
    _only = _os.environ.get("KDBG_ONLY", "")
    for i, t in enumerate([ident, cU, cLo, cSU, cSLo, cSC0, cSC1]):
        P.dma("sp", t[:], consts_in[:, i * 128:(i + 1) * 128])
    P.dma("sp", cvec[:], cvec_in[:])
    P.dma("sp", ropeP[:], rope_in[2, :, 0:128])
    for l in range(DEPTH):
        P.dma("sp", vecs[l][:], vecs_in[l])
    P.I("dve", "memset", ap=ones[:], constant=1.0)
    for i in range(8):
        P.I("dve", "memset", ap=ps[i][:], constant=0.0)
    P.mute = bool(_only)

    with ExitStack() as st:
        xin = [sb(st, f"xin{i}", [128, D]) for i in range(2)]
        xo = [sb(st, f"xo{i}", [128, 8, 128]) for i in range(2)]
        for ti in range(NT // 128):
            src = x_in[ti * 128:(ti + 1) * 128, :] if ti < SEQ // 128 else ctx_in[(ti - SEQ // 128) * 128:(ti - SEQ // 128 + 1) * 128, :]
            xi = xin[ti % 2]
            P.dma("sp", xi[:], src)
            pt = ps[(ti % 2) * 2:(ti % 2) * 2 + 2]
            for c in range(8):
                P.tr(pt[c // 4][:, (c % 4) * 128:(c % 4 + 1) * 128], xi[:, c * 128:(c + 1) * 128], ident[:])
            o = xo[ti % 2]
            P.I("act", "activation", out=o[:, 0:4, :], in_=pt[0][:].rearrange("p (c t) -> p c t", c=4), func=AF.Copy)
            P.I("dve", "tensor_copy", out=o[:, 4:8, :], in_=pt[1][:].rearrange("p (c t) -> p c t", c=4))
            P.dma("sp", hT.rearrange("(c p) t -> p c t", p=128)[:, :, ti * 128:(ti + 1) * 128], o[:])
    P.barrier()

    with ExitStack() as st:
        silu_c = sb(st, "silu_c", [128, 8, 2])
        P.I("act", "activation", out=silu_c[:].rearrange("p c s -> p s c"), in_=cvec[:].rearrange("p (s c) -> p s c", s=2), func=AF.Silu)
        adaw = [sb(st, f"adaw{i}", [128, 8, 1024]) for i in range(2)]
        modraw = sb(st, "modraw", [128, 48, 2])
        for l in range(DEPTH):
            mp = ps[0]
            for jg in range(6):
                aw = adaw[jg % 2]
                P.dma("sp" if jg % 2 == 0 else "act", aw[:], ada_w_in[l].rearrange("(c p) n -> p c n", p=128)[:, :, jg * 1024:(jg + 1) * 1024])
                for jj in range(8):
                    j = jg * 8 + jj
                    for c in range(8):
                        P.mm(mp[:, j * 2:j * 2 + 2], aw[:, c, jj * 128:(jj + 1) * 128], silu_c[:, c, :], start=(c == 0), stop=(c == 7))
            P.I("dve", "tensor_copy", out=modraw[:].rearrange("p j s -> p (j s)"), in_=mp[:, 0:96])
            vv = vecs[l]
            m = mods[l]
            for s in range(2):
                md = sb(st, f"md{l}{s}", [128, 48])
                P.I("dve", "tensor_tensor", out=md[:], in0=modraw[:, :, s], in1=vv[:, V_ADAB:V_ADAB + 48], op=ALU.add)
                for half, vn in ((0, V_N1), (1, V_N2)):
                    b0 = half * 24
                    P.I("dve", "scalar_tensor_tensor", out=m[:, s, half * 3 + 0, :], in0=md[:, b0 + 8:b0 + 16], scalar=1.0,
                        in1=vv[:, vn:vn + 8], op0=ALU.add, op1=ALU.mult)
                    P.I("dve", "tensor_copy", out=m[:, s, half * 3 + 1, :], in_=md[:, b0:b0 + 8])
                    P.I("dve", "tensor_copy", out=m[:, s, half * 3 + 2, :], in_=md[:, b0 + 16:b0 + 24])
            if "mods_d" in dbg:
                P.dma("sp", mods_d[l], m[:].rearrange("p s k c -> p (s k c)"))
    P.barrier()
    if stop_after == "P0":
        return finish(nc, P, out_dram)

    for l in range(DEPTH):
        vv = vecs[l]
        m = mods[l]
        with ExitStack() as st:
            winb = sb(st, "winb", [128, 8, IN_COLS], BF16)
            wab = sb(st, "wab", [128, 8, 16])
            for c in range(8):
                P.dma("pool", winb[:, c, :], w_in_in[l, c * 128:(c + 1) * 128, :])
            P.dma("sp", wab[:], w_in_in[l].rearrange("(c p) n -> p c n", p=128)[:, :, O_A:O_A + 16])
            htl = [sb(st, f"htl{i}", [128, 8, 512]) for i in range(2)]
            sq = sb(st, "sq", [128, 8, 512])
            rstd = sb(st, "rstd", [128, 512])
            tmp = sb(st, "tmp1", [128, 512])
            xm32 = sb(st, "xm32", [128, 8, 512])
            xmb = [sb(st, f"xmb{i}", [128, 8, 512], BF16) for i in range(2)]
            abt = sb(st, "abt", [128, 4, 16])
            stg = [sb(st, f"stg{i}", [128, 512]) for i in range(4)]
            nstg = 0
            tiles = [(t0, 512, 0) for t0 in range(0, SEQ, 512)] + [(SEQ, 256, 1)]
            for ti, (t0, tw, s) in enumerate(tiles):
                ht = htl[ti % 2]
                xb = xmb[ti % 2]
                P.dma("sp", ht[:, :, 0:tw], hT.rearrange("(c p) t -> p c t", p=128)[:, :, t0:t0 + tw])
                P.I("act", "activation", out=sq[:, :, 0:tw], in_=ht[:, :, 0:tw], func=AF.Square)
                for c in range(8):
                    P.mm(ps[0][:, 0:tw], ones[:], sq[:, c, 0:tw], start=(c == 0), stop=(c == 7))
                P.I("act", "activation", out=rstd[:, 0:tw], in_=ps[0][:, 0:tw], func=AF.Sqrt, scale=1.0 / D, bias=EPS)
                P.I("dve", "reciprocal", out=rstd[:, 0:tw], in_=rstd[:, 0:tw])
                for c in range(8):
                    P.I("dve", "scalar_tensor_tensor", out=tmp[:, 0:tw], in0=ht[:, c, 0:tw], scalar=m[:, s, 0, c:c + 1],
                        in1=rstd[:, 0:tw], op0=ALU.mult, op1=ALU.mult)
                    P.I("act", "activation", out=xm32[:, c, 0:tw], in_=tmp[:, 0:tw], func=AF.Identity, bias=m[:, s, 1, c:c + 1])
                    P.I("dve", "tensor_copy", out=xb[:, c, 0:tw], in_=xm32[:, c, 0:tw])
                P.dma("sp", xmT.rearrange("(c p) t -> p c t", p=128)[:, :, t0:t0 + tw], xb[:, :, 0:tw])
                nsub = tw // 128
                for sbk in range(nsub):
                    for c in range(8):
                        P.mm(ps[1][:, sbk * 16:(sbk + 1) * 16], xm32[:, c, sbk * 128:(sbk + 1) * 128], wab[:, c, :], start=(c == 0), stop=(c == 7))
                P.I("dve", "tensor_copy", out=abt[:, 0:nsub, :], in_=ps[1][:, 0:nsub * 16].rearrange("p (s n) -> p s n", n=16))
                P.dma("sp", ab_tm[t0:t0 + tw, :].rearrange("(s p) n -> p s n", p=128), abt[:, 0:nsub, :])
                row = 0
                k = 0
                for (c0, wdt) in PROJ_SRC:
                    for o in range(0, wdt, 128):
                        mw = min(128, wdt - o)
                        pp = ps[2 + k % 4]
                        for c in range(8):
                            P.mm(pp[0:mw, 0:tw], winb[:, c, c0 + o:c0 + o + mw], xb[:, c, 0:tw], start=(c == 0), stop=(c == 7))
                        sg = stg[nstg % 4]
                        nstg += 1
                        if k % 2 == 0:
                            P.I("act", "activation", out=sg[0:mw, 0:tw], in_=pp[0:mw, 0:tw], func=AF.Copy)
                        else:
                            P.I("dve", "tensor_copy", out=sg[0:mw, 0:tw], in_=pp[0:mw, 0:tw])
                        P.dma("sp", projT[row:row + mw, t0:t0 + tw], sg[0:mw, 0:tw])
                        row += mw
                        k += 1
                assert row == PROJ_ROWS
        P.barrier()
        if stop_after == f"P1_{l}":
            return finish(nc, P, out_dram)

        with ExitStack() as st:
            abt_all = sb(st, "abt_all", [128, 34, 16])
            bg_all = sb(st, "bg_all", [128, 34, 16])
            gt1 = sb(st, "g_t1", [128, 34, 8])
            ea = sb(st, "ea", [128, 8])
            P.dma("sp", abt_all[:], ab_tm.rearrange("(s p) n -> p s n", p=128))
            P.I("dve", "tensor_tensor", out=gt1[:], in0=abt_all[:, :, 0:8],
                in1=vv[:, V_DTB:V_DTB + 8].unsqueeze(1).to_broadcast([128, 34, 8]), op=ALU.add)
            P.I("act", "activation", out=gt1[:], in_=gt1[:], func=AF.Exp)
            P.I("act", "activation", out=gt1[:], in_=gt1[:], func=AF.Ln, bias=1.0)
            P.I("act", "activation", out=ea[:], in_=vv[:, V_ALOG:V_ALOG + 8], func=AF.Exp)
            P.I("dve", "scalar_tensor_tensor", out=bg_all[:, :, 8:16], in0=gt1[:], scalar=-1.0,
                in1=ea[:].unsqueeze(1).to_broadcast([128, 34, 8]), op0=ALU.mult, op1=ALU.mult)
            P.I("act", "activation", out=gt1[:], in_=abt_all[:, :, 8:16], func=AF.Exp, scale=-1.0)
            P.I("dve", "tensor_scalar_add", out=gt1[:], in0=gt1[:], scalar1=1.0)
            P.I("dve", "reciprocal", out=bg_all[:, :, 0:8], in_=gt1[:])
            P.dma("sp", bg_tm.rearrange("(s p) n -> p s n", p=128), bg_all[:])
        P.barrier()

        with ExitStack() as st:
            raw = sb(st, "d1raw", [128, 12, 514])
            acc = sb(st, "d1acc", [128, 12, 512])
            sl = sb(st, "d1sl", [128, 12, 512])
            sq8 = sb(st, "d1sq", [128, 8, 512])
            rn = sb(st, "d1rn", [128, 512])
            qk = sb(st, "d1qk", [128, 8, 512])
            qkb = sb(st, "d1qkb", [128, 8, 512], BF16)
            tmt = sb(st, "d1tm", [128, 4, 1024])
            tiles = [(t0, 512, 0, SEQ) for t0 in range(0, SEQ, 512)] + [(SEQ, 256, SEQ, NT)]
            for ti, (t0, tw, s0, s1) in enumerate(tiles):
                lo, hi = max(t0 - 1, s0), min(t0 + tw + 1, s1)
                P.I("dve", "memset", ap=raw[:, :, 0:1], constant=0.0)
                P.I("dve", "memset", ap=raw[:, :, tw + 1:tw + 2], constant=0.0)
                P.dma("sp", raw[:, :, lo - (t0 - 1):hi - (t0 - 1)],
                      projT[R_Q:R_Q + 1536, :].rearrange("(c p) t -> p c t", p=128)[:, :, lo:hi])
                for j in range(12):
                    P.I("dve", "tensor_scalar", out=acc[:, j, 0:tw], in0=raw[:, j, 0:tw], scalar1=vv[:, V_DNCW + j:V_DNCW + j + 1],
                        scalar2=None, op0=ALU.mult)
                    P.I("dve", "scalar_tensor_tensor", out=acc[:, j, 0:tw], in0=raw[:, j, 1:tw + 1], scalar=vv[:, V_DNCW + 12 + j:V_DNCW + 13 + j],
                        in1=acc[:, j, 0:tw], op0=ALU.mult, op1=ALU.add)
                    P.I("dve", "scalar_tensor_tensor", out=acc[:, j, 0:tw], in0=raw[:, j, 2:tw + 2], scalar=vv[:, V_DNCW + 24 + j:V_DNCW + 25 + j],
                        in1=acc[:, j, 0:tw], op0=ALU.mult, op1=ALU.add)
                P.I("act", "activation", out=sl[:, :, 0:tw], in_=acc[:, :, 0:tw], func=AF.Silu)
                P.I("act", "activation", out=sq8[:, :, 0:tw], in_=sl[:, 0:8, 0:tw], func=AF.Square)
                for j in range(8):
                    pp = ps[j % 2]
                    P.mm(pp[:, 0:tw], ones[:], sq8[:, j, 0:tw])
                    P.I("act", "activation", out=rn[:, 0:tw], in_=pp[:, 0:tw], func=AF.Sqrt, bias=1e-6)
                    P.I("dve", "reciprocal", out=rn[:, 0:tw], in_=rn[:, 0:tw])
                    P.I("dve", "scalar_tensor_tensor", out=qk[:, j, 0:tw], in0=sl[:, j, 0:tw], scalar=(128.0 ** -0.5 if j < 4 else 1.0),
                        in1=rn[:, 0:tw], op0=ALU.mult, op1=ALU.mult)
                P.I("act", "activation", out=qkb[:, :, 0:tw], in_=qk[:, :, 0:tw], func=AF.Copy)
                P.dma("act", qnTb.rearrange("(c p) t -> p c t", p=128)[:, :, t0:t0 + tw], qkb[:, 0:4, 0:tw])
                P.dma("act", knTb.rearrange("(c p) t -> p c t", p=128)[:, :, t0:t0 + tw], qkb[:, 4:8, 0:tw])
                nb = tw // 128
                for blk in range(nb):
                    for j in range(8):
                        src = qk[:, 4 + j, blk * 128:(blk + 1) * 128] if j < 4 else sl[:, 4 + j, blk * 128:(blk + 1) * 128]
                        P.tr(ps[2 + j // 4][:, (j % 4) * 128:(j % 4 + 1) * 128], src, ident[:])
                    P.I("act", "activation", out=tmt[:, blk, 0:512], in_=ps[2][:], func=AF.Copy)
                    P.I("dve", "tensor_copy", out=tmt[:, blk, 512:1024], in_=ps[3][:])
                P.dma("sp", k_tm[t0:t0 + tw, :].rearrange("(b p) d -> p b d", p=128), tmt[:, 0:nb, 0:512])
                P.dma("sp", v_tm[t0:t0 + tw, :].rearrange("(b p) d -> p b d", p=128), tmt[:, 0:nb, 512:1024])
        P.barrier()
        if stop_after == f"D1_{l}":
            return finish(nc, P, out_dram)

        if _only == "D2":
            P.mute = False
        _zero[0] = False
        with ExitStack() as st:
            cSame = sb(st, "cSame", [128, 128])
            P.I("dve", "tensor_tensor", out=cSame[:], in0=cLo[:], in1=cSU[:], op=ALU.add)
            Sst = [sb(st, f"S{h}", [128, 128]) for h in range(4)]
            Sb = [sb(st, f"Sb{h}", [128, 128], BF16) for h in range(4)]
            qTb_l = [sb(st, f"d2qTb{i}", [128, 4, 128], BF16) for i in range(2)]
            kTb_l = [sb(st, f"d2kTb{i}", [128, 4, 128], BF16) for i in range(2)]
            identb = sb(st, "identb", [128, 128], BF16)
            P.I("dve", "tensor_copy", out=identb[:], in_=ident[:])
            ktm_l = [sb(st, f"d2ktm{i}", [128, 512]) for i in range(2)]
            vtm_l = [sb(st, f"d2vtm{i}", [128, 512]) for i in range(2)]
            ktm2_l = [sb(st, f"d2ktm2{i}", [64, 2, 512]) for i in range(2)]
            bgt_l = [sb(st, f"d2bg{i}", [128, 16]) for i in range(2)]
            gs = sb(st, "d2gs", [128, 24])
            egc = sb(st, "d2egc", [128, 4])
            sb1 = sb(st, "d2sb1", [128, 4])
            nbeta = sb(st, "d2nbeta", [128, 4])
            egcc = sb(st, "d2egcc", [64, 2, 4])
            edec = sb(st, "d2edec", [64, 2, 4])
            gtc = sb(st, "d2gtc", [128, 2, 4])
            Dm = [sb(st, f"d2D{h}", [128, 128]) for h in range(4)]
            D1m = [sb(st, f"d2D1{h}", [128, 128]) for h in range(4)]
            tmpm = [sb(st, f"d2tmp{h}", [128, 128]) for h in range(4)]
            Nm = [[sb(st, f"d2N{h}_{i}", [128, 128], BF16) for i in range(2)] for h in range(4)]
            NTm = [[sb(st, f"d2NT{h}_{i}", [128, 128], BF16) for i in range(2)] for h in range(4)]
            RT = [sb(st, f"d2RT{h}", [128, 128], BF16) for h in range(4)]
            Am = [sb(st, f"d2A{h}", [128, 128], BF16) for h in range(4)]
            vb = [sb(st, f"d2vb{h}", [128, 128], BF16) for h in range(4)]
            kbg = [sb(st, f"d2kbg{h}", [128, 128], BF16) for h in range(4)]
            um = [sb(st, f"d2u{h}", [64, 2, 128]) for h in range(4)]
            wT = [sb(st, f"d2wT{h}", [128, 128], BF16) for h in range(4)]
            ATm = [sb(st, f"d2AT{h}", [64, 2, 128], BF16) for h in range(4)]
            kdec = [sb(st, f"d2kdec{h}", [64, 2, 128], BF16) for h in range(4)]
            vnew = [sb(st, f"d2vnew{h}", [64, 128], BF16) for h in range(4)]
            avs = [sb(st, f"d2avs{h}", [64, 128]) for h in range(4)]
            osb = sb(st, "d2o", [64, 2, 512])
            H4 = range(4)
            for dr in range(2):
                Mcs = cU if dr == 0 else cLo
                Mstrict = cSLo if dr == 0 else cSU
                Mincl = cLo if dr == 0 else cU
                for h in H4:
                    P.I("dve", "memset", ap=Sst[h][:], constant=0.0)
                    P.I("dve", "memset", ap=Sb[h][:], constant=0.0)
                lat = list(range(0, 32)) if dr == 0 else list(range(31, -1, -1))
                cxt = [32, 33] if dr == 0 else [33, 32]
                import os as _os
                _lim = int(_os.environ.get("KDBG_D2TILES", "1000"))
                for tn, tix in enumerate((cxt + lat)[:_lim]):
                    t0 = tix * 128
                    qTb, kTb, ktm, vtm, ktm2, bgt = (x[tn % 2] for x in (qTb_l, kTb_l, ktm_l, vtm_l, ktm2_l, bgt_l))
                    P.dma("sp", qTb[:], qnTb.rearrange("(h p) t -> p h t", p=128)[:, :, t0:t0 + 128])
                    P.dma("act", kTb[:], knTb.rearrange("(h p) t -> p h t", p=128)[:, :, t0:t0 + 128])
                    P.dma("act", ktm[:], k_tm[t0:t0 + 128, :])
                    P.dma("act", vtm[:], v_tm[t0:t0 + 128, :])
                    P.dma("sp", ktm2[:], k_tm[t0:t0 + 128, :].rearrange("(c p) d -> p c d", p=64))
                    P.dma("sp", bgt[:], bg_tm[t0:t0 + 128, :])
                    g4 = bgt[:, 8 + dr * 4:12 + dr * 4]
                    b4 = bgt[:, dr * 4:dr * 4 + 4]
                    pg = ps[0]
                    P.mm(pg[:, 0:4], Mcs[:], g4)
                    P.mm(pg[:, 4:8], cSame[:], g4)
                    P.mm(pg[:, 8:12], cSC0[:], g4)
                    P.mm(pg[:, 12:16], cSC1[:], g4)
                    P.mm(pg[0:64, 16:20], Mcs[:, 0:64], g4)
                    P.mm(pg[0:64, 20:24], Mcs[:, 64:128], g4)
                    P.I("dve", "tensor_copy", out=gs[:, 0:16], in_=pg[:, 0:16])
                    P.I("dve", "tensor_copy", out=gs[0:64, 16:24], in_=pg[0:64, 16:24])
                    P.I("act", "activation", out=egc[:], in_=gs[:, 0:4], func=AF.Exp)
                    P.I("dve", "tensor_tensor", out=sb1[:], in0=egc[:], in1=b4, op=ALU.mult)
                    P.I("dve", "tensor_scalar", out=nbeta[:], in0=b4, scalar1=-1.0, scalar2=None, op0=ALU.mult)
                    P.I("act", "activation", out=egcc[:].rearrange("p c h -> p (c h)"), in_=gs[0:64, 16:24], func=AF.Exp)
                    P.I("dve", "tensor_tensor", out=edec[:].rearrange("p c h -> p (c h)"), in0=gs[0:64, 8:16], in1=gs[0:64, 16:24], op=ALU.subtract)
                    P.I("act", "activation", out=edec[:].rearrange("p c h -> p (c h)"), in_=edec[:].rearrange("p c h -> p (c h)"), func=AF.Exp)
                    P.I("act", "activation", out=gtc[:].rearrange("p c h -> p (c h)"), in_=gs[:, 8:16], func=AF.Exp)
                    for h in H4:
                        P.I("dve", "tensor_scalar", out=Dm[h][:], in0=ident[:], scalar1=gs[:, h:h + 1], scalar2=None, op0=ALU.mult)
                    for h in H4:
                        hs = slice(h * 128, (h + 1) * 128)
                        P.mm(ps[1][:, hs], ones[:], Dm[h][:])
                        P.mm(ps[2][:, hs], kTb[:, h, :], kTb[:, h, :])
                        P.mm(ps[3][:, hs], qTb[:, h, :], kTb[:, h, :])
                    for h in H4:
                        hs = slice(h * 128, (h + 1) * 128)
                        P.I("dve", "tensor_scalar", out=D1m[h][:], in0=ps[1][:, hs], scalar1=gs[:, h:h + 1], scalar2=0.0, op0=ALU.subtract, op1=ALU.max)
                        P.I("act", "activation", out=D1m[h][:], in_=D1m[h][:], func=AF.Exp, scale=-1.0)
                        P.I("dve", "tensor_tensor", out=tmpm[h][:], in0=ps[2][:, hs], in1=D1m[h][:], op=ALU.mult)
                        P.I("dve", "tensor_scalar", out=tmpm[h][:], in0=tmpm[h][:], scalar1=nbeta[:, h:h + 1], scalar2=None, op0=ALU.mult)
                        P.I("dve", "tensor_tensor", out=Nm[h][0][:], in0=tmpm[h][:], in1=Mstrict[:], op=ALU.mult)
                        P.I("dve", "tensor_tensor", out=Am[h][:], in0=ps[3][:, hs], in1=D1m[h][:], op=ALU.mult)
                        P.I("dve", "tensor_tensor", out=Am[h][:], in0=Am[h][:], in1=Mincl[:], op=ALU.mult)
                    for h in H4:
                        hs = slice(h * 128, (h + 1) * 128)
                        P.mm(ps[4 + h // 2][:, hs], Nm[h][0][:], identb[:])
                    for h in H4:
                        hs = slice(h * 128, (h + 1) * 128)
                        P.I("dve", "tensor_copy", out=NTm[h][0][:], in_=ps[4 + h // 2][:, hs])
                        P.I("dve", "tensor_tensor", out=RT[h][:], in0=ps[4 + h // 2][:, hs], in1=ident[:], op=ALU.add)
                    cur = 0
                    for kk in range(5):
                        nxt = 1 - cur
                        for h in H4:
                            hs = slice(h * 128, (h + 1) * 128)
                            P.mm(ps[4][:, hs], NTm[h][cur][:], Nm[h][cur][:])
                            if kk < 4:
                                P.mm(ps[5][:, hs], Nm[h][cur][:], NTm[h][cur][:])
                        for h in H4:
                            hs = slice(h * 128, (h + 1) * 128)
                            P.I("act", "activation", out=Nm[h][nxt][:], in_=ps[4][:, hs], func=AF.Copy)
                            if kk < 4:
                                P.I("dve", "tensor_copy", out=NTm[h][nxt][:], in_=ps[5][:, hs])
                        for h in H4:
                            hs = slice(h * 128, (h + 1) * 128)
                            P.mm(ps[6][:, hs], Nm[h][nxt][:], RT[h][:])
                        for h in H4:
                            hs = slice(h * 128, (h + 1) * 128)
                            P.I("dve", "tensor_tensor", out=RT[h][:], in0=ps[6][:, hs], in1=RT[h][:], op=ALU.add)
                        cur = nxt
                    for h in H4:
                        hs = slice(h * 128, (h + 1) * 128)
                        P.I("dve", "tensor_scalar", out=vb[h][:], in0=vtm[:, hs], scalar1=b4[:, h:h + 1], scalar2=None, op0=ALU.mult)
                        P.I("dve", "tensor_scalar", out=kbg[h][:], in0=ktm[:, hs], scalar1=sb1[:, h:h + 1], scalar2=None, op0=ALU.mult)
                        for c in range(2):
                            P.I("dve", "tensor_scalar", out=kdec[h][:, c, :], in0=ktm2[:, c, hs], scalar1=edec[:, c, h:h + 1], scalar2=None, op0=ALU.mult)
                    for h in H4:
                        hs = slice(h * 128, (h + 1) * 128)
                        P.mm(ps[1][0:64, hs], RT[h][:, 0:64], vb[h][:])
                        P.mm(ps[2][0:64, hs], RT[h][:, 64:128], vb[h][:])
                        P.mm(ps[3][:, hs], kbg[h][:], RT[h][:])
                        P.mm(ps[4][0:64, hs], Am[h][:, 0:64], identb[:])
                        P.mm(ps[5][0:64, hs], Am[h][:, 64:128], identb[:])
                    for h in H4:
                        hs = slice(h * 128, (h + 1) * 128)
                        P.I("act", "activation", out=um[h][:, 0, :], in_=ps[1][0:64, hs], func=AF.Copy)
                        P.I("dve", "tensor_copy", out=um[h][:, 1, :], in_=ps[2][0:64, hs])
                        P.I("act", "activation", out=wT[h][:], in_=ps[3][:, hs], func=AF.Copy)
                        P.I("dve", "tensor_copy", out=ATm[h][:, 0, :], in_=ps[4][0:64, hs])
                        P.I("act", "activation", out=ATm[h][:, 1, :], in_=ps[5][0:64, hs], func=AF.Copy)
                    for c in ([0, 1] if dr == 0 else [1, 0]):
                        cs = slice(c * 64, (c + 1) * 64)
                        for h in H4:
                            hs = slice(h * 128, (h + 1) * 128)
                            P.mm(ps[6][0:64, hs], wT[h][:, cs], Sb[h][:])
                            P.mm(ps[7][0:64, hs], qTb[:, h, cs], Sb[h][:])
                        for h in H4:
                            hs = slice(h * 128, (h + 1) * 128)
                            P.I("dve", "tensor_tensor", out=vnew[h][:], in0=um[h][:, c, :], in1=ps[6][0:64, hs], op=ALU.subtract)
                        for h in H4:
                            hs = slice(h * 128, (h + 1) * 128)
                            P.mm(ps[1][0:64, hs], ATm[h][:, c, cs], vnew[h][:])
                            P.mm(ps[2][:, hs], kdec[h][:, c, :], vnew[h][:])
                        for h in H4:
                            hs = slice(h * 128, (h + 1) * 128)
                            P.I("act", "activation", out=avs[h][:], in_=ps[1][0:64, hs], func=AF.Copy)
                            P.I("dve", "scalar_tensor_tensor", out=osb[:, c, hs], in0=ps[7][0:64, hs], scalar=egcc[:, c, h:h + 1],
                                in1=avs[h][:], op0=ALU.mult, op1=ALU.add)
                            P.I("dve", "scalar_tensor_tensor", out=Sst[h][:], in0=Sst[h][:], scalar=gtc[:, c, h:h + 1],
                                in1=ps[2][:, hs], op0=ALU.mult, op1=ALU.add)
                            P.I("act", "activation", out=Sb[h][:], in_=Sst[h][:], func=AF.Copy)
                    P.dma("sp", o_dir[dr, t0:t0 + 128, :].rearrange("(c p) d -> p c d", p=64), osb[:])
        _zero[0] = False
        P.barrier()
        if stop_after == f"D2_{l}":
            return finish(nc, P, out_dram)

        TILES = [(t0, 512, 0, 0, SEQ) for t0 in range(0, SEQ, 512)] + [(SEQ, 256, 1, SEQ, NT)]
        with ExitStack() as st:
            of = sb(st, "d3of", [128, 4, 512])
            ob = sb(st, "d3ob", [128, 4, 512])
            osq = sb(st, "d3sq", [128, 4, 512])
            ms = sb(st, "d3ms", [128, 16])
            zt = sb(st, "d3z", [128, 4, 512])
            ydn = sb(st, "d3y", [128, 4, 512], BF16)
            for (t0, tw, s, s0, s1) in TILES:
                nb = tw // 128
                P.dma("sp", of[:, 0:nb, :], o_dir[0, t0:t0 + tw, :].rearrange("(b p) d -> p b d", p=128))
                P.dma("act", ob[:, 0:nb, :], o_dir[1, t0:t0 + tw, :].rearrange("(b p) d -> p b d", p=128))
                P.dma("sp", zt[:, :, 0:tw], projT[R_Z:R_Z + 512, :].rearrange("(c p) t -> p c t", p=128)[:, :, t0:t0 + tw])
                P.I("dve", "tensor_tensor", out=of[:, 0:nb, :], in0=of[:, 0:nb, :], in1=ob[:, 0:nb, :], op=ALU.add)
                P.I("dve", "tensor_tensor", out=osq[:, 0:nb, :], in0=of[:, 0:nb, :], in1=of[:, 0:nb, :], op=ALU.mult)
                P.I("dve", "reduce_sum", out=ms[:, 0:nb * 4], in_=osq[:, 0:nb, :].rearrange("p b (h d) -> p (b h) d", h=4), axis=mybir.AxisListType.X)
                P.I("act", "activation", out=ms[:, 0:nb * 4], in_=ms[:, 0:nb * 4], func=AF.Sqrt, scale=1.0 / 128, bias=EPS)
                P.I("dve", "reciprocal", out=ms[:, 0:nb * 4], in_=ms[:, 0:nb * 4])
                P.I("dve", "tensor_tensor", out=of[:, 0:nb, :].rearrange("p b (h d) -> p (b h) d", h=4),
                    in0=of[:, 0:nb, :].rearrange("p b (h d) -> p (b h) d", h=4),
                    in1=ms[:, 0:nb * 4].unsqueeze(2).to_broadcast([128, nb * 4, 128]), op=ALU.mult)
                P.I("act", "activation", out=zt[:, :, 0:tw], in_=zt[:, :, 0:tw], func=AF.Silu)
                for h in range(4):
                    pp = ps[h % 2]
                    for b in range(nb):
                        P.tr(pp[:, b * 128:(b + 1) * 128], of[:, b, h * 128:(h + 1) * 128], ident[:])
                    P.I("dve", "scalar_tensor_tensor", out=ydn[:, h, 0:tw], in0=pp[:, 0:tw], scalar=vv[:, V_DNG:V_DNG + 1],
                        in1=zt[:, h, 0:tw], op0=ALU.mult, op1=ALU.mult)
                P.dma("sp", y_dnT.rearrange("(c p) t -> p c t", p=128)[:, :, t0:t0 + tw], ydn[:, :, 0:tw])
        P.barrier()

        with ExitStack() as st:
            shh = sb(st, "p3sh", [128, 4, 514])
            scc = sb(st, "p3sc", [128, 4, 514])
            sbb = sb(st, "p3sb", [128, 4, 512])
            acc3 = sb(st, "p3acc", [128, 4, 512])
            ysc = sb(st, "p3y", [128, 4, 512], BF16)
            for (t0, tw, s, s0, s1) in TILES:
                lo, hi = max(t0 - 1, s0), min(t0 + tw + 1, s1)
                for tt, r0 in ((shh, R_SH), (scc, R_SC)):
                    P.I("dve", "memset", ap=tt[:, :, 0:1], constant=0.0)
                    P.I("dve", "memset", ap=tt[:, :, tw + 1:tw + 2], constant=0.0)
                    P.dma("sp", tt[:, :, lo - (t0 - 1):hi - (t0 - 1)],
                          projT[r0:r0 + 512, :].rearrange("(c p) t -> p c t", p=128)[:, :, lo:hi])
                P.dma("act", sbb[:, :, 0:tw], projT[R_SB:R_SB + 512, :].rearrange("(c p) t -> p c t", p=128)[:, :, t0:t0 + tw])
                P.I("dve", "tensor_tensor", out=shh[:, :, 0:tw + 2], in0=shh[:, :, 0:tw + 2], in1=scc[:, :, 0:tw + 2], op=ALU.mult)
                for j in range(4):
                    P.I("dve", "tensor_scalar", out=acc3[:, j, 0:tw], in0=shh[:, j, 0:tw], scalar1=vv[:, V_SCCW + j:V_SCCW + j + 1],
                        scalar2=None, op0=ALU.mult)
                    P.I("dve", "scalar_tensor_tensor", out=acc3[:, j, 0:tw], in0=shh[:, j, 1:tw + 1], scalar=vv[:, V_SCCW + 4 + j:V_SCCW + 5 + j],
                        in1=acc3[:, j, 0:tw], op0=ALU.mult, op1=ALU.add)
                    P.I("dve", "scalar_tensor_tensor", out=acc3[:, j, 0:tw], in0=shh[:, j, 2:tw + 2], scalar=vv[:, V_SCCW + 8 + j:V_SCCW + 9 + j],
                        in1=acc3[:, j, 0:tw], op0=ALU.mult, op1=ALU.add)
                P.I("dve", "tensor_tensor", out=ysc[:, :, 0:tw], in0=acc3[:, :, 0:tw], in1=sbb[:, :, 0:tw], op=ALU.mult)
                P.dma("sp", y_scT.rearrange("(c p) t -> p c t", p=128)[:, :, t0:t0 + tw], ysc[:, :, 0:tw])
        P.barrier()
        if stop_after == f"P3_{l}":
            return finish(nc, P, out_dram)

        with ExitStack() as st:
            wqn = sb(st, "wqn", [128, 2, 4, 128], BF16)
            wqr = sb(st, "wqr", [128, 2, 4, 64], BF16)
            wkn = sb(st, "wkn", [128, 4, 128], BF16)
            wkv = sb(st, "wkv", [128, 4, 128], BF16)
            wq_v = mla_w_qb_in[l].rearrange("(kc p) (h x) -> p kc h x", p=128, x=192)
            for kc in range(2):
                P.dma("pool", wqn[:, kc, :, :], wq_v[:, kc, :, 0:128])
                P.dma("pool", wqr[:, kc, :, :], wq_v[:, kc, :, 128:192])
            wk_v = mla_w_kvb_in[l].rearrange("p (h x) -> p h x", x=256)
            P.dma("pool", wkn[:], wk_v[:, :, 0:128])
            P.dma("pool", wkv[:], wk_v[:, :, 128:256])
            qa = sb(st, "p4qa", [128, 2, 512])
            qsq = sb(st, "p4qsq", [128, 2, 512])
            rr = sb(st, "p4rr", [128, 512])
            qan = sb(st, "p4qan", [128, 2, 512], BF16)
            qn_o = sb(st, "p4qn", [128, 4, 512], BF16)
            qr_o = sb(st, "p4qr", [128, 2, 512], BF16)
            xr = sb(st, "p4xr", [128, 512])
            t1 = sb(st, "p4t1", [128, 512])
            t2 = sb(st, "p4t2", [128, 512])
            rc = sb(st, "p4rc", [128, 512])
            rs = sb(st, "p4rs", [128, 512])
            ckv = sb(st, "p4ckv", [128, 512])
            ckvn = sb(st, "p4ckvn", [128, 512], BF16)
            kn_o = sb(st, "p4kn", [128, 4, 512], BF16)
            v_o = sb(st, "p4v", [128, 4, 512], BF16)
            kr = sb(st, "p4kr", [128, 512])
            kr_o = sb(st, "p4kro", [128, 512], BF16)
            for (t0, tw, s, s0, s1) in TILES:
                nb = tw // 128
                P.dma("sp", qa[:, :, 0:tw], projT[R_QA:R_QA + 256, :].rearrange("(c p) t -> p c t", p=128)[:, :, t0:t0 + tw])
                P.dma("act", ckv[:, 0:tw], projT[R_CKV:R_CKV + 128, t0:t0 + tw])
                P.dma("sp", kr[0:64, 0:tw], projT[R_KR:R_KR + 64, t0:t0 + tw])
                P.dma("sp", kr[64:128, 0:tw], projT[R_KR:R_KR + 64, t0:t0 + tw])
                P.dma("act", rc[:, 0:tw], rope_in[0, :, t0:t0 + tw])
                P.dma("act", rs[:, 0:tw], rope_in[1, :, t0:t0 + tw])
                P.I("act", "activation", out=qsq[:, :, 0:tw], in_=qa[:, :, 0:tw], func=AF.Square)
                for c in range(2):
                    P.mm(ps[0][:, 0:tw], ones[:], qsq[:, c, 0:tw], start=(c == 0), stop=(c == 1))
                P.I("act", "activation", out=rr[:, 0:tw], in_=ps[0][:, 0:tw], func=AF.Sqrt, scale=1.0 / 256, bias=EPS)
                P.I("dve", "reciprocal", out=rr[:, 0:tw], in_=rr[:, 0:tw])
                for c in range(2):
                    P.I("dve", "scalar_tensor_tensor", out=qan[:, c, 0:tw], in0=qa[:, c, 0:tw], scalar=vv[:, V_QNG + c:V_QNG + c + 1],
                        in1=rr[:, 0:tw], op0=ALU.mult, op1=ALU.mult)
                for h in range(4):
                    pp = ps[1 + h % 2]
                    for kc in range(2):
                        P.mm(pp[:, 0:tw], wqn[:, kc, h, :], qan[:, kc, 0:tw], start=(kc == 0), stop=(kc == 1))
                    P.I("act", "activation", out=qn_o[:, h, 0:tw], in_=pp[:, 0:tw], func=AF.Copy)
                for r in range(2):
                    pp = ps[3]
                    for kc in range(2):
                        P.mm(pp[:, 0:tw], wqr[:, kc, 2 * r:2 * r + 2, :], qan[:, kc, 0:tw], start=(kc == 0), stop=(kc == 1))
                    P.I("act", "activation", out=xr[:, 0:tw], in_=pp[:, 0:tw], func=AF.Copy)
                    P.mm(ps[4][:, 0:tw], ropeP[:], xr[:, 0:tw])
                    P.I("dve", "tensor_tensor", out=t1[:, 0:tw], in0=xr[:, 0:tw], in1=rc[:, 0:tw], op=ALU.mult)
                    P.I("dve", "tensor_tensor", out=t2[:, 0:tw], in0=ps[4][:, 0:tw], in1=rs[:, 0:tw], op=ALU.mult)
                    P.I("dve", "tensor_tensor", out=qr_o[:, r, 0:tw], in0=t1[:, 0:tw], in1=t2[:, 0:tw], op=ALU.add)
                P.dma("sp", qnopeT.rearrange("(c p) t -> p c t", p=128)[:, :, t0:t0 + tw], qn_o[:, :, 0:tw])
                P.dma("sp", qropeT.rearrange("(c p) t -> p c t", p=128)[:, :, t0:t0 + tw], qr_o[:, :, 0:tw])
                P.I("act", "activation", out=t1[:, 0:tw], in_=ckv[:, 0:tw], func=AF.Square)
                P.mm(ps[0][:, 0:tw], ones[:], t1[:, 0:tw])
                P.I("act", "activation", out=rr[:, 0:tw], in_=ps[0][:, 0:tw], func=AF.Sqrt, scale=1.0 / 128, bias=EPS)
                P.I("dve", "reciprocal", out=rr[:, 0:tw], in_=rr[:, 0:tw])
                P.I("dve", "scalar_tensor_tensor", out=ckvn[:, 0:tw], in0=ckv[:, 0:tw], scalar=vv[:, V_KVNG:V_KVNG + 1],
                    in1=rr[:, 0:tw], op0=ALU.mult, op1=ALU.mult)
                for h in range(4):
                    pp = ps[1 + h % 2]
                    P.mm(pp[:, 0:tw], wkn[:, h, :], ckvn[:, 0:tw])
                    P.I("act", "activation", out=kn_o[:, h, 0:tw], in_=pp[:, 0:tw], func=AF.Copy)
                for b in range(nb):
                    pp = ps[5 + b % 2]
                    P.mm(pp[:, :], ckvn[:, b * 128:(b + 1) * 128], wkv[:].rearrange("p h d -> p (h d)"))
                    P.I("act", "activation", out=v_o[:, b, :], in_=pp[:, :], func=AF.Copy)
                P.dma("sp", knopeT.rearrange("(c p) t -> p c t", p=128)[:, :, t0:t0 + tw], kn_o[:, :, 0:tw])
                P.dma("sp", v_mla[t0:t0 + tw, :].rearrange("(b p) d -> p b d", p=128), v_o[:, 0:nb, :])
                P.mm(ps[4][:, 0:tw], ropeP[:], kr[:, 0:tw])
                P.I("dve", "tensor_tensor", out=t1[:, 0:tw], in0=kr[:, 0:tw], in1=rc[:, 0:tw], op=ALU.mult)
                P.I("dve", "tensor_tensor", out=t2[:, 0:tw], in0=ps[4][:, 0:tw], in1=rs[:, 0:tw], op=ALU.mult)
                P.I("dve", "tensor_tensor", out=kr_o[:, 0:tw], in0=t1[:, 0:tw], in1=t2[:, 0:tw], op=ALU.add)
                P.dma("sp", kropeT[:, t0:t0 + tw], kr_o[:, 0:tw])
        P.barrier()
        if stop_after == f"P4a_{l}":
            return finish(nc, P, out_dram)

        with ExitStack() as st:
            kn_all = sb(st, "kn_all", [128, 4, NT], BF16)
            kr_all = sb(st, "kr_all", [128, NT], BF16)
            v_all = sb(st, "v_all", [128, 34, 512], BF16)
            onesb = sb(st, "onesb", [128, 128], BF16)
            P.I("dve", "tensor_copy", out=onesb[:], in_=ones[:])
            P.dma("sp", kn_all[:], knopeT.rearrange("(c p) t -> p c t", p=128))
            P.dma("act", kr_all[:], kropeT[:, :])
            P.dma("sp", v_all[:], v_mla.rearrange("(b p) d -> p b d", p=128))
            qn_t = [sb(st, f"qn_t{i}", [128, 4, 512], BF16) for i in range(2)]
            qr_t = [sb(st, f"qr_t{i}", [128, 2, 512], BF16) for i in range(2)]
            ptb = [sb(st, f"ptb{i}", [128, 512], BF16) for i in range(2)]
            rinv = sb(st, "rinv", [128, 512])
            ym = sb(st, "ym", [128, 4, 512], BF16)
            SCALE = 192.0 ** -0.5
            for ti, (t0, tw, s, s0, s1) in enumerate(TILES):
                kbs = list(range(34)) if s == 0 else [32, 33]
                qn_ = qn_t[ti % 2]
                qr_ = qr_t[ti % 2]
                P.dma("sp", qn_[:, :, 0:tw], qnopeT.rearrange("(c p) t -> p c t", p=128)[:, :, t0:t0 + tw])
                P.dma("act", qr_[:, :, 0:tw], qropeT.rearrange("(c p) t -> p c t", p=128)[:, :, t0:t0 + tw])
                for h in range(4):
                    po = ps[2 + (h % 2) * 2]
                    pl = ps[3 + (h % 2) * 2]
                    hp = (h % 2) * 64

                    def emit_st(i, kb):
                        pst = ps[i % 2]
                        ks = slice(kb * 128, (kb + 1) * 128)
                        P.mm(pst[:, 0:tw], kn_all[:, h, ks], qn_[:, h, 0:tw], start=True, stop=False)
                        P.mm(pst[:, 0:tw], kr_all[hp:hp + 64, ks], qr_[hp:hp + 64, h // 2, 0:tw], start=False, stop=True)

                    emit_st(0, kbs[0])
                    for i, kb in enumerate(kbs):
                        if i + 1 < len(kbs):
                            emit_st(i + 1, kbs[i + 1])
                        pt_ = ptb[i % 2]
                        P.I("act", "activation", out=pt_[:, 0:tw], in_=ps[i % 2][:, 0:tw], func=AF.Exp, scale=SCALE)
                        first, last = (i == 0), (i == len(kbs) - 1)
                        P.mm(po[:, 0:tw], v_all[:, kb, h * 128:(h + 1) * 128], pt_[:, 0:tw], start=first, stop=last)
                        P.mm(pl[:, 0:tw], onesb[:], pt_[:, 0:tw], start=first, stop=last)
                    P.I("dve", "reciprocal", out=rinv[:, 0:tw], in_=pl[:, 0:tw])
                    P.I("dve", "tensor_tensor", out=ym[:, h, 0:tw], in0=po[:, 0:tw], in1=rinv[:, 0:tw], op=ALU.mult)
                P.dma("sp", y_mlaT.rearrange("(c p) t -> p c t", p=128)[:, :, t0:t0 + tw], ym[:, :, 0:tw])
        P.barrier()
        if stop_after == f"P4_{l}":
            return finish(nc, P, out_dram)

        last = (l == DEPTH - 1)
        PT = [t for t in TILES if not (last and t[2] == 1)]
        with ExitStack() as st:
            wg = sb(st, "wg", [128, 8, 3072], BF16)
            wbr = [sb(st, f"wbr{i}", [128, 4, 1024], BF16) for i in range(3)]
            wo = sb(st, "wo", [128, 8, 1024], BF16)
            wr32 = sb(st, "wr32", [128, 8, 32])
            P.dma("pool", wg[:], w_gate_in[l].rearrange("(kc p) n -> p kc n", p=128))
            for i, wsrc in enumerate((w_dn_in, w_sc_in, w_mla_in)):
                P.dma("pool", wbr[i][:], wsrc[l].rearrange("(kc p) n -> p kc n", p=128))
            P.dma("pool", wo[:], w_out_in[l].rearrange("(kc p) n -> p kc n", p=128))
            P.dma("sp", wr32[:], router_w_in[l].rearrange("(kc p) n -> p kc n", p=128))
            xb5 = sb(st, "p5xb", [128, 8, 512], BF16)
            ybr = [sb(st, f"p5y{i}", [128, 4, 512], BF16) for i in range(3)]
            ht5 = sb(st, "p5ht", [128, 8, 512])
            sig5 = [sb(st, f"p5sig{i}", [128, 512]) for i in range(2)]
            mrg = sb(st, "p5mrg", [128, 512])
            tm5 = sb(st, "p5tm", [128, 512])
            mg = sb(st, "p5mg", [128, 8, 512], BF16)
            sq5 = sb(st, "p5sq", [128, 8, 512], BF16)
            rs5 = sb(st, "p5rs", [128, 512])
            x32 = sb(st, "p5x32", [128, 8, 512])
            x2b = sb(st, "p5x2b", [128, 8, 512], BF16)
            lg = sb(st, "p5lg", [128, 4, 32])
            mx8 = sb(st, "p5mx", [128, 8])
            msk = sb(st, "p5msk", [128, 32])
            ee = sb(st, "p5e", [128, 32])
            sm = sb(st, "p5sm", [128, 2])
            gts = sb(st, "p5gts", [128, 4, 32])
            gTs = sb(st, "p5gT", [32, 512])
            nsig = 0
            for (t0, tw, s, s0, s1) in PT:
                nb = tw // 128
                P.dma("sp", xb5[:, :, 0:tw], xmT.rearrange("(c p) t -> p c t", p=128)[:, :, t0:t0 + tw])
                for i, ysrc in enumerate((y_dnT, y_scT, y_mlaT)):
                    P.dma("act", ybr[i][:, :, 0:tw], ysrc.rearrange("(c p) t -> p c t", p=128)[:, :, t0:t0 + tw])
                P.dma("sp", ht5[:, :, 0:tw], hT.rearrange("(c p) t -> p c t", p=128)[:, :, t0:t0 + tw])
                for c in range(8):
                    for br in range(3):
                        pgt = ps[br % 2]
                        for k in range(8):
                            P.mm(pgt[:, 0:tw], wg[:, k, br * 1024 + c * 128:br * 1024 + (c + 1) * 128], xb5[:, k, 0:tw], start=(k == 0), stop=(k == 7))
                        sg = sig5[nsig % 2]
                        nsig += 1
                        P.I("act", "activation", out=sg[:, 0:tw], in_=pgt[:, 0:tw], func=AF.Sigmoid,
                            bias=vv[:, V_BG + br * 8 + c:V_BG + br * 8 + c + 1])
                        ppr = ps[2 + br % 2]
                        for k in range(4):
                            P.mm(ppr[:, 0:tw], wbr[br][:, k, c * 128:(c + 1) * 128], ybr[br][:, k, 0:tw], start=(k == 0), stop=(k == 3))
                        if br == 0:
                            P.I("dve", "tensor_tensor", out=mrg[:, 0:tw], in0=ppr[:, 0:tw], in1=sg[:, 0:tw], op=ALU.mult)
                        else:
                            P.I("dve", "tensor_tensor", out=tm5[:, 0:tw], in0=ppr[:, 0:tw], in1=sg[:, 0:tw], op=ALU.mult)
                            if br == 1:
                                P.I("dve", "tensor_tensor", out=mrg[:, 0:tw], in0=mrg[:, 0:tw], in1=tm5[:, 0:tw], op=ALU.add)
                            else:
                                P.I("dve", "tensor_tensor", out=mg[:, c, 0:tw], in0=mrg[:, 0:tw], in1=tm5[:, 0:tw], op=ALU.add)
                for c in range(8):
                    pp = ps[4 + c % 2]
                    for k in range(8):
                        P.mm(pp[:, 0:tw], wo[:, k, c * 128:(c + 1) * 128], mg[:, k, 0:tw], start=(k == 0), stop=(k == 7))
                    P.I("dve", "scalar_tensor_tensor", out=ht5[:, c, 0:tw], in0=pp[:, 0:tw], scalar=m[:, s, 2, c:c + 1],
                        in1=ht5[:, c, 0:tw], op0=ALU.mult, op1=ALU.add)
                P.dma("sp", hT.rearrange("(c p) t -> p c t", p=128)[:, :, t0:t0 + tw], ht5[:, :, 0:tw])
                P.I("act", "activation", out=x32[:, :, 0:tw], in_=ht5[:, :, 0:tw], func=AF.Square)
                for c in range(8):
                    P.mm(ps[6][:, 0:tw], ones[:], x32[:, c, 0:tw], start=(c == 0), stop=(c == 7))
                P.I("act", "activation", out=rs5[:, 0:tw], in_=ps[6][:, 0:tw], func=AF.Sqrt, scale=1.0 / D, bias=EPS)
                P.I("dve", "reciprocal", out=rs5[:, 0:tw], in_=rs5[:, 0:tw])
                for c in range(8):
                    P.I("dve", "scalar_tensor_tensor", out=tm5[:, 0:tw], in0=ht5[:, c, 0:tw], scalar=m[:, s, 3, c:c + 1],
                        in1=rs5[:, 0:tw], op0=ALU.mult, op1=ALU.mult)
                    P.I("act", "activation", out=x32[:, c, 0:tw], in_=tm5[:, 0:tw], func=AF.Identity, bias=m[:, s, 4, c:c + 1])
                    P.I("dve", "tensor_copy", out=x2b[:, c, 0:tw], in_=x32[:, c, 0:tw])
                P.dma("sp", xm2T.rearrange("(c p) t -> p c t", p=128)[:, :, t0:t0 + tw], x2b[:, :, 0:tw])
                for b in range(nb):
                    for c in range(8):
                        P.mm(ps[7][:, b * 32:(b + 1) * 32], x32[:, c, b * 128:(b + 1) * 128], wr32[:, c, :], start=(c == 0), stop=(c == 7))
                P.I("dve", "tensor_tensor", out=lg[:, 0:nb, :], in0=ps[7][:, 0:nb * 32].rearrange("p (b e) -> p b e", e=32),
                    in1=vv[:, V_RB:V_RB + 32].unsqueeze(1).to_broadcast([128, nb, 32]), op=ALU.add)
                for b in range(nb):
                    P.I("dve", "max", out=mx8[:], in_=lg[:, b, :])
                    P.I("dve", "tensor_scalar", out=msk[:], in0=lg[:, b, :], scalar1=mx8[:, 3:4], scalar2=None, op0=ALU.is_ge)
                    P.I("dve", "tensor_scalar", out=sm[:, 0:1], in0=mx8[:, 0:1], scalar1=-1.0, scalar2=None, op0=ALU.mult)
                    P.I("act", "activation", out=ee[:], in_=lg[:, b, :], func=AF.Exp, bias=sm[:, 0:1])
                    P.I("dve", "tensor_tensor", out=ee[:], in0=ee[:], in1=msk[:], op=ALU.mult)
                    P.I("dve", "reduce_sum", out=sm[:, 1:2], in_=ee[:], axis=mybir.AxisListType.X)
                    P.I("dve", "reciprocal", out=sm[:, 1:2], in_=sm[:, 1:2])
                    P.I("dve", "tensor_scalar", out=gts[:, b, :], in0=ee[:], scalar1=sm[:, 1:2], scalar2=None, op0=ALU.mult)
                for b in range(nb):
                    P.tr(ps[6][0:32, b * 128:(b + 1) * 128], gts[:, b, :], ident[:])
                P.I("dve", "tensor_copy", out=gTs[:, 0:tw], in_=ps[6][0:32, 0:tw])
                P.dma("sp", gatesT[:, t0:t0 + tw], gTs[:, 0:tw])
        P.barrier()
        if stop_after == f"P5_{l}":
            return finish(nc, P, out_dram)

        with ExitStack() as st:
            GMAX = 1024
            w1b = [sb(st, f"w1b{i}", [128, 8, 2048], BF16) for i in range(2)]
            w2b = [sb(st, f"w2b{i}", [128, 8, 1024], BF16) for i in range(2)]
            yacc = sb(st, "yacc", [128, 8, GMAX])
            xg = sb(st, "xg", [128, 8, GMAX], BF16)
            gTg = sb(st, "gTg", [32, GMAX])
            actb = [sb(st, f"actb{i}", [128, 8, 512], BF16) for i in range(2)]
            gbb = [sb(st, f"m_gb{i}", [128, 512]) for i in range(2)]
            a1b = [sb(st, f"m_a1{i}", [128, 512]) for i in range(2)]
            a2b = [sb(st, f"m_a2{i}", [128, 512]) for i in range(2)]
            glub = [sb(st, f"m_glu{i}", [128, 512]) for i in range(2)]
            sgb = [sb(st, f"m_sig{i}", [128, 512]) for i in range(2)]
            linb = [sb(st, f"m_lin{i}", [128, 512]) for i in range(2)]
            lin = linb[0]
            nfc = 0
            htc = [sb(st, f"m_htc{i}", [128, 512]) for i in range(2)]
            selE = sb(st, "selE", [32, 128])
            b2sb = sb(st, "b2sb", [32, 1024])
            P.dma("sp", b2sb[:], expert_b2_in[l])
            pend = [None]

            def y_phase(wb2, ab, o, tw, e):
                for c in range(8):
                    py = ps[4 + c % 2]
                    for fc in range(8):
                        P.mm(py[:, 0:tw], wb2[:, fc, c * 128:(c + 1) * 128], ab[:, fc, 0:tw], start=(fc == 0), stop=(fc == 7))
                    if e == 0:
                        P.I("dve", "tensor_copy", out=yacc[:, c, o:o + tw], in_=py[:, 0:tw])
                    else:
                        P.I("dve", "tensor_tensor", out=yacc[:, c, o:o + tw], in0=py[:, 0:tw], in1=yacc[:, c, o:o + tw], op=ALU.add)

            groups = [(0, 1024), (1024, 1024), (2048, 1024), (3072, 1024)] + ([] if last else [(SEQ, 256)])
            groups = groups[:int(_os.environ.get("KDBG_GROUPS", "100"))]
            nact = 0
            for (g0, gw) in groups:
                s = 1 if g0 >= SEQ else 0
                P.dma("sp", xg[:, :, 0:gw], xm2T.rearrange("(c p) t -> p c t", p=128)[:, :, g0:g0 + gw])
                P.dma("sp", gTg[:, 0:gw], gatesT[:, g0:g0 + gw])
                gtiles = [(o, min(512, gw - o)) for o in range(0, gw, 512)]
                for e in range(ne):
                    wb1, wb2 = w1b[e % 2], w2b[e % 2]
                    P.dma("pool", wb1[:], w1_in[l, e].rearrange("(kc p) n -> p kc n", p=128))
                    P.dma("pool", wb2[:], w2_in[l, e].rearrange("(kc p) n -> p kc n", p=128))
                    P.I("dve", "tensor_scalar", out=selE[:], in0=ones[0:32, :], scalar1=ident[0:32, e:e + 1], scalar2=None, op0=ALU.mult)
                    w1v = wb1[:].rearrange("p k (f two) -> p k f two", two=2)
                    for (o, tw) in gtiles:
                        ab = actb[nact % 2]
                        gb = gbb[nact % 2]
                        nact += 1
                        P.mm(ps[6][:, 0:tw], selE[:], gTg[:, o:o + tw])
                        P.I("act", "activation", out=gb[:, 0:tw], in_=ps[6][:, 0:tw], func=AF.Copy)
                        for fc in range(8):
                            pg_, pl_ = ps[(fc % 2) * 2], ps[(fc % 2) * 2 + 1]
                            for k in range(8):
                                P.mm(pg_[:, 0:tw], w1v[:, k, fc * 128:(fc + 1) * 128, 0], xg[:, k, o:o + tw], start=(k == 0), stop=(k == 7))
                            for k in range(8):
                                P.mm(pl_[:, 0:tw], w1v[:, k, fc * 128:(fc + 1) * 128, 1], xg[:, k, o:o + tw], start=(k == 0), stop=(k == 7))
                            a1, a2, gl_, sg_, ln_ = a1b[nfc % 2], a2b[nfc % 2], glub[nfc % 2], sgb[nfc % 2], linb[nfc % 2]
                            nfc += 1
                            P.I("act", "activation", out=a1[:, 0:tw], in_=pg_[:, 0:tw], func=AF.Identity,
                                bias=vv[:, V_B1G + e * 8 + fc:V_B1G + e * 8 + fc + 1])
                            P.I("act", "activation", out=a2[:, 0:tw], in_=pl_[:, 0:tw], func=AF.Identity,
                                bias=vv[:, V_B1L + e * 8 + fc:V_B1L + e * 8 + fc + 1])
                            P.I("dve", "tensor_scalar_min", out=gl_[:, 0:tw], in0=a1[:, 0:tw], scalar1=7.0)
                            P.I("act", "activation", out=sg_[:, 0:tw], in_=gl_[:, 0:tw], func=AF.Sigmoid, scale=1.702)
                            P.I("dve", "tensor_scalar", out=ln_[:, 0:tw], in0=a2[:, 0:tw], scalar1=7.0, scalar2=-7.0, op0=ALU.min, op1=ALU.max)
                            P.I("dve", "scalar_tensor_tensor", out=ln_[:, 0:tw], in0=ln_[:, 0:tw], scalar=1.0, in1=gl_[:, 0:tw], op0=ALU.add, op1=ALU.mult)
                            P.I("dve", "tensor_tensor", out=ln_[:, 0:tw], in0=ln_[:, 0:tw], in1=sg_[:, 0:tw], op=ALU.mult)
                            P.I("dve", "tensor_tensor", out=ab[:, fc, 0:tw], in0=ln_[:, 0:tw], in1=gb[:, 0:tw], op=ALU.mult)
                        if pend[0] is not None:
                            y_phase(*pend[0])
                        pend[0] = (wb2, ab, o, tw, e)
                if pend[0] is not None:
                    y_phase(*pend[0])
                    pend[0] = None
                if "yaccT" in dbg:
                    P.dma("sp", yaccT.rearrange("(c p) t -> p c t", p=128)[:, :, g0:g0 + gw], yacc[:, :, 0:gw])
                nh = 0
                for (o, tw) in gtiles:
                    for c in range(8):
                        hc = htc[nh % 2]
                        nh += 1
                        P.dma("sp", hc[:, 0:tw], hT[c * 128:(c + 1) * 128, g0 + o:g0 + o + tw])
                        P.mm(ps[7][:, 0:tw], b2sb[:, c * 128:(c + 1) * 128], gTg[:, o:o + tw])
                        P.I("dve", "tensor_tensor", out=lin[:, 0:tw], in0=ps[7][:, 0:tw], in1=yacc[:, c, o:o + tw], op=ALU.add)
                        P.I("dve", "scalar_tensor_tensor", out=hc[:, 0:tw], in0=lin[:, 0:tw], scalar=m[:, s, 5, c:c + 1],
                            in1=hc[:, 0:tw], op0=ALU.mult, op1=ALU.add)
                        P.dma("sp", hT[c * 128:(c + 1) * 128, g0 + o:g0 + o + tw], hc[:, 0:tw])
        P.barrier()
        if stop_after == f"P6_{l}":
            return finish(nc, P, out_dram)

    with ExitStack() as st:
        vv = vecs[DEPTH - 1]
        htf = [sb(st, f"f_ht{i}", [128, 8, 512]) for i in range(2)]
        sqf = sb(st, "f_sq", [128, 8, 512])
        rsf = sb(st, "f_rs", [128, 512])
        xnf = sb(st, "f_xn", [128, 8, 512])
        otl = [sb(st, f"f_o{i}", [128, 1024]) for i in range(2)]
        no = 0
        for ti, t0 in enumerate(range(0, SEQ, 512)):
            ht = htf[ti % 2]
            P.dma("sp", ht[:], hT.rearrange("(c p) t -> p c t", p=128)[:, :, t0:t0 + 512])
            P.I("act", "activation", out=sqf[:], in_=ht[:], func=AF.Square)
            for c in range(8):
                P.mm(ps[0][:], ones[:], sqf[:, c, :], start=(c == 0), stop=(c == 7))
            P.I("act", "activation", out=rsf[:], in_=ps[0][:], func=AF.Sqrt, scale=1.0 / D, bias=EPS)
            P.I("dve", "reciprocal", out=rsf[:], in_=rsf[:])
            for c in range(8):
                P.I("dve", "scalar_tensor_tensor", out=xnf[:, c, :], in0=ht[:, c, :], scalar=vv[:, V_FNG + c:V_FNG + c + 1],
                    in1=rsf[:], op0=ALU.mult, op1=ALU.mult)
            for b in range(4):
                ot = otl[no % 2]
                no += 1
                pa, pb = ps[1 + (b % 2) * 2], ps[2 + (b % 2) * 2]
                for c in range(8):
                    pp = pa if c < 4 else pb
                    P.tr(pp[:, (c % 4) * 128:(c % 4 + 1) * 128], xnf[:, c, b * 128:(b + 1) * 128], ident[:])
                P.I("act", "activation", out=ot[:, 0:512], in_=pa[:], func=AF.Copy)
                P.I("dve", "tensor_copy", out=ot[:, 512:1024], in_=pb[:])
                P.dma("sp", out_dram[t0 + b * 128:t0 + (b + 1) * 128, :], ot[:])
    return finish(nc, P, out_dram)


def finish(nc, P, out_dram):
    P.barrier(engines=["sp"])
    return nc


def _pcol(v):
    v = np.asarray(v, np.float32)
    return np.ascontiguousarray(v.reshape(-1, 128).T)


def _consts():
    i = np.arange(128)
    same = (i[:, None] // 64) == (i[None, :] // 64)
    ident = np.eye(128, dtype=np.float32)
    U = (same & (i[:, None] <= i[None, :])).astype(np.float32)
    Lo = (same & (i[:, None] >= i[None, :])).astype(np.float32)
    SU = (same & (i[:, None] < i[None, :])).astype(np.float32)
    SLo = (same & (i[:, None] > i[None, :])).astype(np.float32)
    SC0 = np.repeat((i < 64).astype(np.float32)[:, None], 128, axis=1)
    SC1 = np.repeat((i >= 64).astype(np.float32)[:, None], 128, axis=1)
    return np.ascontiguousarray(np.concatenate([ident, U, Lo, SU, SLo, SC0, SC1], axis=1))


def _rope():
    t = np.arange(SEQ)
    row = (t // 64).astype(np.float32)
    col = (t % 64).astype(np.float32)
    inv = (np.float32(10000.0) ** (-np.arange(0, 32, 2, dtype=np.float32) / np.float32(32))).astype(np.float32)
    out = np.zeros((3, 128, NT), np.float32)
    out[0, :, SEQ:] = 1.0
    for r in range(64):
        a, within = r // 32, r % 32
        half, i = within // 16, within % 16
        ang = ((row if a == 0 else col) * inv[i]).astype(np.float32)
        out[0, r, :SEQ] = np.cos(ang)
        out[1, r, :SEQ] = np.sin(ang) * (-1.0 if half == 0 else 1.0)
        partner = r + 16 if half == 0 else r - 16
        out[2, partner, r] = 1.0
        out[2, 64 + partner, 64 + r] = 1.0
    out[0, 64:128] = out[0, 0:64]
    out[1, 64:128] = out[1, 0:64]
    return out


def _vecs(inp, l):
    v = np.zeros((128, NV), np.float32)
    v[:, V_ADAB:V_ADAB + 48] = _pcol(inp["ada_b"][l])
    v[:, V_N1:V_N1 + 8] = _pcol(inp["norm1_g"][l])
    v[:, V_N2:V_N2 + 8] = _pcol(inp["norm2_g"][l])
    v[:, V_BG:V_BG + 24] = _pcol(inp["b_branch_gate"][l])
    for tap in range(3):
        v[:, V_DNCW + tap * 12:V_DNCW + tap * 12 + 12] = _pcol(inp["dn_conv_w"][l][tap])
        v[:, V_SCCW + tap * 4:V_SCCW + tap * 4 + 4] = _pcol(inp["sc_conv_w"][l][tap])
    v[:, V_QNG:V_QNG + 2] = _pcol(inp["mla_q_norm_g"][l])
    v[:, V_KVNG:V_KVNG + 1] = _pcol(inp["mla_kv_norm_g"][l])
    v[:, V_DNG:V_DNG + 1] = _pcol(inp["dn_norm_g"][l])
    v[:, V_ALOG:V_ALOG + 8] = np.asarray(inp["dn_a_log"][l], np.float32).reshape(1, 8)
    v[:, V_DTB:V_DTB + 8] = np.asarray(inp["dn_dt_bias"][l], np.float32).reshape(1, 8)
    v[:, V_RB:V_RB + 32] = np.asarray(inp["router_b"][l], np.float32).reshape(1, 32)
    b1 = np.asarray(inp["expert_b1"][l], np.float32)
    for e in range(NE):
        v[:, V_B1G + e * 8:V_B1G + e * 8 + 8] = _pcol(b1[e, 0::2])
        v[:, V_B1L + e * 8:V_B1L + e * 8 + 8] = _pcol(b1[e, 1::2])
    v[:, V_FNG:V_FNG + 8] = _pcol(inp["final_norm_g"])
    return v


def make_in_maps(inp, names, ne=NE):
    f = lambda a: np.ascontiguousarray(np.asarray(a, np.float32))
    shared = {}
    shared["consts"] = _consts()
    shared["vecs"] = np.stack([_vecs(inp, l) for l in range(DEPTH)])
    shared["rope"] = _rope()
    for k in ["ada_w", "w_in", "mla_w_qb", "mla_w_kvb", "w_branch_gate", "w_branch_dn", "w_branch_sc", "w_branch_mla",
              "w_out", "router_w", "expert_b2"]:
        shared[k] = f(inp[k])
    for k in ["expert_w1", "expert_w2"]:
        if k in names:
            shared[k] = f(np.asarray(inp[k])[:, :ne])
    maps = []
    for b in range(8):
        m = dict(shared)
        m["x"] = f(inp["x"][b])
        m["ctx"] = f(inp["ctx"][b])
        m["cvec"] = np.ascontiguousarray(np.concatenate([_pcol(inp["c"][b]), _pcol(inp["c_ctx"])], axis=1))
        maps.append({k: v for k, v in m.items() if k in names})
    return maps


INPUT_NAMES = ["x", "ctx", "cvec", "consts", "vecs", "ada_w", "w_in", "mla_w_qb", "mla_w_kvb", "rope",
               "w_branch_gate", "w_branch_dn", "w_branch_sc", "w_branch_mla", "w_out", "router_w",
               "expert_w1", "expert_w2", "expert_b2"]


def kernel(**inputs):
    nc = build()
    maps = make_in_maps(inputs, INPUT_NAMES)
    res = run_bass_kernel_spmd(nc, maps, core_ids=list(range(8)))
    return np.stack([np.asarray(r["out"], np.float32) for r in res.results], axis=0)
```

```python
import numpy as np
from contextlib import ExitStack
import concourse.bass as bass
import concourse.mybir as mybir
from concourse.bass_utils import run_bass_kernel_spmd

F32 = mybir.dt.float32
BF16 = mybir.dt.bfloat16
AF = mybir.ActivationFunctionType
ALU = mybir.AluOpType

D = 1024
SEQ = 4096
CTX = 256
NT = SEQ + CTX
NCH = D // 128
DEPTH = 2
IN_COLS = 4048
NE = 32
FF = 1024
EPS = 1e-6

O_Q, O_K, O_V, O_Z, O_A, O_B, O_SH, O_SB, O_SC, O_QA, O_KV = 0, 512, 1024, 1536, 2048, 2056, 2064, 2576, 3088, 3600, 3856
PROJ_SRC = [(O_Q, 512), (O_K, 512), (O_V, 512), (O_Z, 512), (O_SH, 512), (O_SB, 512), (O_SC, 512), (O_QA, 256), (O_KV, 192)]
R_Q, R_K, R_V, R_Z, R_SH, R_SB, R_SC, R_QA, R_CKV, R_KR = 0, 512, 1024, 1536, 2048, 2560, 3072, 3584, 3840, 3968
PROJ_ROWS = 4032

V_ADAB, V_N1, V_N2, V_BG, V_DNCW, V_SCCW, V_QNG, V_KVNG, V_DNG, V_ALOG, V_DTB, V_RB, V_B1G, V_B1L, V_FNG = (
    0, 48, 56, 64, 88, 124, 136, 138, 139, 140, 148, 156, 188, 444, 700)
NV = 708


def _is_ap(v):
    return hasattr(v, "tensor") and hasattr(v, "ap")


class Prog:
    def __init__(self, nc, n_dma=24):
        self.nc = nc
        self.es = ExitStack()
        self.eng = {"pe": nc.tensor, "act": nc.scalar, "dve": nc.vector, "pool": nc.gpsimd, "sp": nc.sync}
        self.semobj = {}
        self.cnt = {}
        for e in ["pe", "act", "dve", "pool"]:
            self.semobj[e] = self.es.enter_context(nc.semaphore(f"s_{e}"))
            self.cnt[e] = 0
        self.ring = {}
        self.rnext = {}
        for q, n in (("sp", 12), ("act", 6), ("pool", 6), ("dve", 2), ("pe", 2)):
            self.ring[q] = []
            self.rnext[q] = 0
            for i in range(n):
                nm = f"d{q}{i}"
                self.semobj[nm] = self.es.enter_context(nc.semaphore(f"s_{nm}"))
                self.cnt[nm] = 0
                self.ring[q].append(nm)
        self.known = {e: {} for e in self.eng}
        self.W = {}
        self.R = {}
        self.nops = 0
        self.mute = False
        self._nm = ""
        import os as _os
        self.trace = bool(_os.environ.get("KDBG_TRACE", ""))
        self.maxops = int(_os.environ.get("KDBG_MAXOPS", "100000000"))

    @staticmethod
    def key(ap):
        return ap.tensor.name

    def op(self, eng, fn, reads=(), writes=(), signal=True, dma=False):
        if self.mute or self.nops >= self.maxops:
            return ("x", 0)
        writes = list(writes) + [k for k in reads if isinstance(k, str) and k.startswith("ps") and k not in writes]
        need = {}
        for k in reads:
            for s, v in self.W.get(k, {}).items():
                if need.get(s, 0) < v:
                    need[s] = v
        for k in writes:
            for s, v in self.W.get(k, {}).items():
                if need.get(s, 0) < v:
                    need[s] = v
            for s, v in self.R.get(k, {}).items():
                if need.get(s, 0) < v:
                    need[s] = v
        e = self.eng[eng]
        kn = self.known[eng]
        for s, v in need.items():
            if eng == "pe" and s == "pe":
                continue
            if kn.get(s, 0) >= v:
                continue
            e.wait_ge(self.semobj[s], v)
            kn[s] = v
        if dma:
            rs_ = self.ring[eng][self.rnext[eng]]
            if self.cnt[rs_] > kn.get(rs_, 0):
                e.wait_ge(self.semobj[rs_], self.cnt[rs_])
                kn[rs_] = self.cnt[rs_]
        ins = fn(e)
        if self.trace:
            print("OP", self.nops, eng, self._nm, list(writes), list(reads))
        self.nops += 1
        if dma:
            s = self.ring[eng][self.rnext[eng]]
            self.rnext[eng] = (self.rnext[eng] + 1) % len(self.ring[eng])
            self.cnt[s] += 16
            ins.then_inc(self.semobj[s], 16)
            ref = (s, self.cnt[s])
        elif signal:
            self.cnt[eng] += 1
            ins.then_inc(self.semobj[eng], 1)
            ref = (eng, self.cnt[eng])
        else:
            ref = (eng, self.cnt[eng] + 1)
        for k in reads:
            d = self.R.setdefault(k, {})
            if d.get(ref[0], 0) < ref[1]:
                d[ref[0]] = ref[1]
        for k in writes:
            d = self.W.setdefault(k, {})
            if d.get(ref[0], 0) < ref[1]:
                d[ref[0]] = ref[1]
        return ref

    def barrier(self, engines=None):
        if self.mute:
            return
        for eng, e in self.eng.items():
            if engines is not None and eng not in engines:
                continue
            kn = self.known[eng]
            for s, v in self.cnt.items():
                if v == 0 or (eng == "pe" and s == "pe"):
                    continue
                if kn.get(s, 0) >= v:
                    continue
                e.wait_ge(self.semobj[s], v)
                kn[s] = v

    def mm(self, out, lhsT, rhs, start=True, stop=True, rk=None, wk=None, **kw):
        reads = rk if rk is not None else [self.key(lhsT), self.key(rhs)]
        writes = wk if wk is not None else [self.key(out)]
        self._nm = "matmul"
        return self.op("pe", lambda e: e.matmul(out, lhsT, rhs, start=start, stop=stop, **kw), reads, writes, signal=stop)

    def tr(self, out, in_, ident, rk=None, wk=None):
        reads = rk if rk is not None else [self.key(in_), self.key(ident)]
        writes = wk if wk is not None else [self.key(out)]
        self._nm = "transpose"
        return self.op("pe", lambda e: e.transpose(out, in_, ident), reads, writes)

    def I(self, eng, meth, rk=None, wk=None, **kw):
        if rk is None:
            rk = [self.key(v) for k_, v in kw.items() if k_ not in ("out", "accum_out", "ap") and _is_ap(v)]
        if wk is None:
            wk = [self.key(kw[k_]) for k_ in ("out", "accum_out", "ap") if k_ in kw and kw[k_] is not None]
        self._nm = meth
        return self.op(eng, lambda e: getattr(e, meth)(**kw), rk, wk)

    def dma(self, q, out, in_, rk=None, wk=None, **kw):
        reads = rk if rk is not None else [self.key(in_)]
        writes = wk if wk is not None else [self.key(out)]
        self._nm = "dma"
        return self.op(q, lambda e: e.dma_start(out=out, in_=in_, **kw), reads, writes, dma=True)


def build(stop_after=None, dbg=(), ne=NE):
    nc = bass.Bass("TRN2", target_bir_lowering=False)
    P = Prog(nc)
    es = P.es
    dbg = set(dbg)

    def dram_in(name, shape, dt=F32):
        return nc.dram_tensor(name, list(shape), dt, kind="ExternalInput").ap()

    def dram_scr(name, shape, dt=F32):
        kind = "ExternalOutput" if name in dbg else "Internal"
        return nc.dram_tensor(name, list(shape), dt, kind=kind).ap()

    _zero = [False]

    _uid = [0]

    def sb(st, name, shape, dt=F32):
        _uid[0] += 1
        t = st.enter_context(nc.sbuf_tensor(f"{name}_u{_uid[0]}", list(shape), dt))
        if _zero[0]:
            P.I("dve", "memset", ap=t[:], constant=0.0)
        return t

    x_in = dram_in("x", [SEQ, D])
    ctx_in = dram_in("ctx", [CTX, D])
    cvec_in = dram_in("cvec", [128, 16])
    consts_in = dram_in("consts", [128, 7 * 128])
    vecs_in = dram_in("vecs", [DEPTH, 128, NV])
    ada_w_in = dram_in("ada_w", [DEPTH, D, 6 * D])
    w_in_in = dram_in("w_in", [DEPTH, D, IN_COLS])
    mla_w_qb_in = dram_in("mla_w_qb", [DEPTH, 256, 768])
    mla_w_kvb_in = dram_in("mla_w_kvb", [DEPTH, 128, 1024])
    w_gate_in = dram_in("w_branch_gate", [DEPTH, D, 3 * D])
    w_dn_in = dram_in("w_branch_dn", [DEPTH, 512, D])
    w_sc_in = dram_in("w_branch_sc", [DEPTH, 512, D])
    w_mla_in = dram_in("w_branch_mla", [DEPTH, 512, D])
    w_out_in = dram_in("w_out", [DEPTH, D, D])
    router_w_in = dram_in("router_w", [DEPTH, D, NE])
    w1_in = dram_in("expert_w1", [DEPTH, ne, D, 2 * FF])
    w2_in = dram_in("expert_w2", [DEPTH, ne, FF, D])
    expert_b2_in = dram_in("expert_b2", [DEPTH, NE, D])
    rope_in = dram_in("rope", [3, 128, NT])
    out_dram = nc.dram_tensor("out", [SEQ, D], F32, kind="ExternalOutput").ap()

    hT = dram_scr("hT", [D, NT])
    xmT = dram_scr("xmT", [D, NT], BF16)
    projT = dram_scr("projT", [PROJ_ROWS, NT])
    ab_tm = dram_scr("ab_tm", [NT, 16])
    mods_d = dram_scr("mods_d", [DEPTH, 128, 96])
    bg_tm = dram_scr("bg_tm", [NT, 16])
    qnT = dram_scr("qnT", [512, NT])
    knT = dram_scr("knT", [512, NT])
    qnTb = dram_scr("qnTb", [512, NT], BF16)
    knTb = dram_scr("knTb", [512, NT], BF16)
    k_tm = dram_scr("k_tm", [NT, 512])
    v_tm = dram_scr("v_tm", [NT, 512])
    o_dir = dram_scr("o_dir", [2, NT, 512])
    y_dnT = dram_scr("y_dnT", [512, NT], BF16)
    y_scT = dram_scr("y_scT", [512, NT], BF16)
    y_mlaT = dram_scr("y_mlaT", [512, NT], BF16)
    qnopeT = dram_scr("qnopeT", [512, NT], BF16)
    qropeT = dram_scr("qropeT", [256, NT], BF16)
    knopeT = dram_scr("knopeT", [512, NT], BF16)
    kropeT = dram_scr("kropeT", [128, NT], BF16)
    v_mla = dram_scr("v_mla", [NT, 512], BF16)
    xm2T = dram_scr("xm2T", [D, NT], BF16)
    gatesT = dram_scr("gatesT", [32, NT])
    yaccT = dram_scr("yaccT", [D, NT])

    ident = sb(es, "ident", [128, 128])
    cU = sb(es, "cU", [128, 128])
    cLo = sb(es, "cLo", [128, 128])
    cSU = sb(es, "cSU", [128, 128])
    cSLo = sb(es, "cSLo", [128, 128])
    cSC0 = sb(es, "cSC0", [128, 128])
    cSC1 = sb(es, "cSC1", [128, 128])
    ones = sb(es, "ones", [128, 128])
    ropeP = sb(es, "ropeP", [128, 128])
    cvec = sb(es, "cvec_sb", [128, 16])
    vecs = [sb(es, f"vecs{l}", [128, NV]) for l in range(DEPTH)]
    mods = [sb(es, f"mods{l}", [128, 2, 6, 8]) for l in range(DEPTH)]
    ps = [es.enter_context(nc.psum_tensor(f"ps{i}", [128, 512], F32)) for i in range(8)]

    import os as _os
    _only = _os.environ.get("KDBG_ONLY", "")
    for i, t in enumerate([ident, cU, cLo, cSU, cSLo, cSC0, cSC1]):
        P.dma("sp", t[:], consts_in[:, i * 128:(i + 1) * 128])
    P.dma("sp", cvec[:], cvec_in[:])
    P.dma("sp", ropeP[:], rope_in[2, :, 0:128])
    for l in range(DEPTH):
        P.dma("sp", vecs[l][:], vecs_in[l])
    P.I("dve", "memset", ap=ones[:], constant=1.0)
    for i in range(8):
        P.I("dve", "memset", ap=ps[i][:], constant=0.0)
    P.mute = bool(_only)

    with ExitStack() as st:
        xin = [sb(st, f"xin{i}", [128, D]) for i in range(2)]
        xo = [sb(st, f"xo{i}", [128, 8, 128]) for i in range(2)]
        for ti in range(NT // 128):
            src = x_in[ti * 128:(ti + 1) * 128, :] if ti < SEQ // 128 else ctx_in[(ti - SEQ // 128) * 128:(ti - SEQ // 128 + 1) * 128, :]
            xi = xin[ti % 2]
            P.dma("sp", xi[:], src)
            pt = ps[(ti % 2) * 2:(ti % 2) * 2 + 2]
            for c in range(8):
                P.tr(pt[c // 4][:, (c % 4) * 128:(c % 4 + 1) * 128], xi[:, c * 128:(c + 1) * 128], ident[:])
            o = xo[ti % 2]
            P.I("act", "activation", out=o[:, 0:4, :], in_=pt[0][:].rearrange("p (c t) -> p c t", c=4), func=AF.Copy)
            P.I("dve", "tensor_copy", out=o[:, 4:8, :], in_=pt[1][:].rearrange("p (c t) -> p c t", c=4))
            P.dma("sp", hT.rearrange("(c p) t -> p c t", p=128)[:, :, ti * 128:(ti + 1) * 128], o[:])
    P.barrier()

    with ExitStack() as st:
        silu_c = sb(st, "silu_c", [128, 8, 2])
        P.I("act", "activation", out=silu_c[:].rearrange("p c s -> p s c"), in_=cvec[:].rearrange("p (s c) -> p s c", s=2), func=AF.Silu)
        adaw = [sb(st, f"adaw{i}", [128, 8, 1024]) for i in range(2)]
        modraw = sb(st, "modraw", [128, 48, 2])
        for l in range(DEPTH):
            mp = ps[0]
            for jg in range(6):
                aw = adaw[jg % 2]
                P.dma("sp" if jg % 2 == 0 else "act", aw[:], ada_w_in[l].rearrange("(c p) n -> p c n", p=128)[:, :, jg * 1024:(jg + 1) * 1024])
                for jj in range(8):
                    j = jg * 8 + jj
                    for c in range(8):
                        P.mm(mp[:, j * 2:j * 2 + 2], aw[:, c, jj * 128:(jj + 1) * 128], silu_c[:, c, :], start=(c == 0), stop=(c == 7))
            P.I("dve", "tensor_copy", out=modraw[:].rearrange("p j s -> p (j s)"), in_=mp[:, 0:96])
            vv = vecs[l]
            m = mods[l]
            for s in range(2):
                md = sb(st, f"md{l}{s}", [128, 48])
                P.I("dve", "tensor_tensor", out=md[:], in0=modraw[:, :, s], in1=vv[:, V_ADAB:V_ADAB + 48], op=ALU.add)
                for half, vn in ((0, V_N1), (1, V_N2)):
                    b0 = half * 24
                    P.I("dve", "scalar_tensor_tensor", out=m[:, s, half * 3 + 0, :], in0=md[:, b0 + 8:b0 + 16], scalar=1.0,
                        in1=vv[:, vn:vn + 8], op0=ALU.add, op1=ALU.mult)
                    P.I("dve", "tensor_copy", out=m[:, s, half * 3 + 1, :], in_=md[:, b0:b0 + 8])
                    P.I("dve", "tensor_copy", out=m[:, s, half * 3 + 2, :], in_=md[:, b0 + 16:b0 + 24])
            if "mods_d" in dbg:
                P.dma("sp", mods_d[l], m[:].rearrange("p s k c -> p (s k c)"))
    P.barrier()
    if stop_after == "P0":
        return finish(nc, P, out_dram)

    for l in range(DEPTH):
        vv = vecs[l]
        m = mods[l]
        with ExitStack() as st:
            winb = sb(st, "winb", [128, 8, IN_COLS], BF16)
            wab = sb(st, "wab", [128, 8, 16])
            for c in range(8):
                P.dma("pool", winb[:, c, :], w_in_in[l, c * 128:(c + 1) * 128, :])
            P.dma("sp", wab[:], w_in_in[l].rearrange("(c p) n -> p c n", p=128)[:, :, O_A:O_A + 16])
            htl = [sb(st, f"htl{i}", [128, 8, 512]) for i in range(2)]
            sq = sb(st, "sq", [128, 8, 512])
            rstd = sb(st, "rstd", [128, 512])
            tmp = sb(st, "tmp1", [128, 512])
            xm32 = sb(st, "xm32", [128, 8, 512])
            xmb = [sb(st, f"xmb{i}", [128, 8, 512], BF16) for i in range(2)]
            abt = sb(st, "abt", [128, 4, 16])
            stg = [sb(st, f"stg{i}", [128, 512]) for i in range(4)]
            nstg = 0
            tiles = [(t0, 512, 0) for t0 in range(0, SEQ, 512)] + [(SEQ, 256, 1)]
            for ti, (t0, tw, s) in enumerate(tiles):
                ht = htl[ti % 2]
                xb = xmb[ti % 2]
                P.dma("sp", ht[:, :, 0:tw], hT.rearrange("(c p) t -> p c t", p=128)[:, :, t0:t0 + tw])
                P.I("act", "activation", out=sq[:, :, 0:tw], in_=ht[:, :, 0:tw], func=AF.Square)
                for c in range(8):
                    P.mm(ps[0][:, 0:tw], ones[:], sq[:, c, 0:tw], start=(c == 0), stop=(c == 7))
                P.I("act", "activation", out=rstd[:, 0:tw], in_=ps[0][:, 0:tw], func=AF.Sqrt, scale=1.0 / D, bias=EPS)
                P.I("dve", "reciprocal", out=rstd[:, 0:tw], in_=rstd[:, 0:tw])
                for c in range(8):
                    P.I("dve", "scalar_tensor_tensor", out=tmp[:, 0:tw], in0=ht[:, c, 0:tw], scalar=m[:, s, 0, c:c + 1],
                        in1=rstd[:, 0:tw], op0=ALU.mult, op1=ALU.mult)
                    P.I("act", "activation", out=xm32[:, c, 0:tw], in_=tmp[:, 0:tw], func=AF.Identity, bias=m[:, s, 1, c:c + 1])
                    P.I("dve", "tensor_copy", out=xb[:, c, 0:tw], in_=xm32[:, c, 0:tw])
                P.dma("sp", xmT.rearrange("(c p) t -> p c t", p=128)[:, :, t0:t0 + tw], xb[:, :, 0:tw])
                nsub = tw // 128
                for sbk in range(nsub):
                    for c in range(8):
                        P.mm(ps[1][:, sbk * 16:(sbk + 1) * 16], xm32[:, c, sbk * 128:(sbk + 1) * 128], wab[:, c, :], start=(c == 0), stop=(c == 7))
                P.I("dve", "tensor_copy", out=abt[:, 0:nsub, :], in_=ps[1][:, 0:nsub * 16].rearrange("p (s n) -> p s n", n=16))
                P.dma("sp", ab_tm[t0:t0 + tw, :].rearrange("(s p) n -> p s n", p=128), abt[:, 0:nsub, :])
                row = 0
                k = 0
                for (c0, wdt) in PROJ_SRC:
                    for o in range(0, wdt, 128):
                        mw = min(128, wdt - o)
                        pp = ps[2 + k % 4]
                        for c in range(8):
                            P.mm(pp[0:mw, 0:tw], winb[:, c, c0 + o:c0 + o + mw], xb[:, c, 0:tw], start=(c == 0), stop=(c == 7))
                        sg = stg[nstg % 4]
                        nstg += 1
                        if k % 2 == 0:
                            P.I("act", "activation", out=sg[0:mw, 0:tw], in_=pp[0:mw, 0:tw], func=AF.Copy)
                        else:
                            P.I("dve", "tensor_copy", out=sg[0:mw, 0:tw], in_=pp[0:mw, 0:tw])
                        P.dma("sp", projT[row:row + mw, t0:t0 + tw], sg[0:mw, 0:tw])
                        row += mw
                        k += 1
                assert row == PROJ_ROWS
        P.barrier()
        if stop_after == f"P1_{l}":
            return finish(nc, P, out_dram)

        with ExitStack() as st:
            abt_all = sb(st, "abt_all", [128, 34, 16])
            bg_all = sb(st, "bg_all", [128, 34, 16])
            gt1 = sb(st, "g_t1", [128, 34, 8])
            ea = sb(st, "ea", [128, 8])
            P.dma("sp", abt_all[:], ab_tm.rearrange("(s p) n -> p s n", p=128))
            P.I("dve", "tensor_tensor", out=gt1[:], in0=abt_all[:, :, 0:8],
                in1=vv[:, V_DTB:V_DTB + 8].unsqueeze(1).to_broadcast([128, 34, 8]), op=ALU.add)
            P.I("act", "activation", out=gt1[:], in_=gt1[:], func=AF.Exp)
            P.I("act", "activation", out=gt1[:], in_=gt1[:], func=AF.Ln, bias=1.0)
            P.I("act", "activation", out=ea[:], in_=vv[:, V_ALOG:V_ALOG + 8], func=AF.Exp)
            P.I("dve", "scalar_tensor_tensor", out=bg_all[:, :, 8:16], in0=gt1[:], scalar=-1.0,
                in1=ea[:].unsqueeze(1).to_broadcast([128, 34, 8]), op0=ALU.mult, op1=ALU.mult)
            P.I("act", "activation", out=gt1[:], in_=abt_all[:, :, 8:16], func=AF.Exp, scale=-1.0)
            P.I("dve", "tensor_scalar_add", out=gt1[:], in0=gt1[:], scalar1=1.0)
            P.I("dve", "reciprocal", out=bg_all[:, :, 0:8], in_=gt1[:])
            P.dma("sp", bg_tm.rearrange("(s p) n -> p s n", p=128), bg_all[:])
        P.barrier()

        with ExitStack() as st:
            raw = sb(st, "d1raw", [128, 12, 514])
            acc = sb(st, "d1acc", [128, 12, 512])
            sl = sb(st, "d1sl", [128, 12, 512])
            sq8 = sb(st, "d1sq", [128, 8, 512])
            rn = sb(st, "d1rn", [128, 512])
            qk = sb(st, "d1qk", [128, 8, 512])
            qkb = sb(st, "d1qkb", [128, 8, 512], BF16)
            tmt = sb(st, "d1tm", [128, 4, 1024])
            tiles = [(t0, 512, 0, SEQ) for t0 in range(0, SEQ, 512)] + [(SEQ, 256, SEQ, NT)]
            for ti, (t0, tw, s0, s1) in enumerate(tiles):
                lo, hi = max(t0 - 1, s0), min(t0 + tw + 1, s1)
                P.I("dve", "memset", ap=raw[:, :, 0:1], constant=0.0)
                P.I("dve", "memset", ap=raw[:, :, tw + 1:tw + 2], constant=0.0)
                P.dma("sp", raw[:, :, lo - (t0 - 1):hi - (t0 - 1)],
                      projT[R_Q:R_Q + 1536, :].rearrange("(c p) t -> p c t", p=128)[:, :, lo:hi])
                for j in range(12):
                    P.I("dve", "tensor_scalar", out=acc[:, j, 0:tw], in0=raw[:, j, 0:tw], scalar1=vv[:, V_DNCW + j:V_DNCW + j + 1],
                        scalar2=None, op0=ALU.mult)
                    P.I("dve", "scalar_tensor_tensor", out=acc[:, j, 0:tw], in0=raw[:, j, 1:tw + 1], scalar=vv[:, V_DNCW + 12 + j:V_DNCW + 13 + j],
                        in1=acc[:, j, 0:tw], op0=ALU.mult, op1=ALU.add)
                    P.I("dve", "scalar_tensor_tensor", out=acc[:, j, 0:tw], in0=raw[:, j, 2:tw + 2], scalar=vv[:, V_DNCW + 24 + j:V_DNCW + 25 + j],
                        in1=acc[:, j, 0:tw], op0=ALU.mult, op1=ALU.add)
                P.I("act", "activation", out=sl[:, :, 0:tw], in_=acc[:, :, 0:tw], func=AF.Silu)
                P.I("act", "activation", out=sq8[:, :, 0:tw], in_=sl[:, 0:8, 0:tw], func=AF.Square)
                for j in range(8):
                    pp = ps[j % 2]
                    P.mm(pp[:, 0:tw], ones[:], sq8[:, j, 0:tw])
                    P.I("act", "activation", out=rn[:, 0:tw], in_=pp[:, 0:tw], func=AF.Sqrt, bias=1e-6)
                    P.I("dve", "reciprocal", out=rn[:, 0:tw], in_=rn[:, 0:tw])
                    P.I("dve", "scalar_tensor_tensor", out=qk[:, j, 0:tw], in0=sl[:, j, 0:tw], scalar=(128.0 ** -0.5 if j < 4 else 1.0),
                        in1=rn[:, 0:tw], op0=ALU.mult, op1=ALU.mult)
                P.I("act", "activation", out=qkb[:, :, 0:tw], in_=qk[:, :, 0:tw], func=AF.Copy)
                P.dma("act", qnTb.rearrange("(c p) t -> p c t", p=128)[:, :, t0:t0 + tw], qkb[:, 0:4, 0:tw])
                P.dma("act", knTb.rearrange("(c p) t -> p c t", p=128)[:, :, t0:t0 + tw], qkb[:, 4:8, 0:tw])
                nb = tw // 128
                for blk in range(nb):
                    for j in range(8):
                        src = qk[:, 4 + j, blk * 128:(blk + 1) * 128] if j < 4 else sl[:, 4 + j, blk * 128:(blk + 1) * 128]
                        P.tr(ps[2 + j // 4][:, (j % 4) * 128:(j % 4 + 1) * 128], src, ident[:])
                    P.I("act", "activation", out=tmt[:, blk, 0:512], in_=ps[2][:], func=AF.Copy)
                    P.I("dve", "tensor_copy", out=tmt[:, blk, 512:1024], in_=ps[3][:])
                P.dma("sp", k_tm[t0:t0 + tw, :].rearrange("(b p) d -> p b d", p=128), tmt[:, 0:nb, 0:512])
                P.dma("sp", v_tm[t0:t0 + tw, :].rearrange("(b p) d -> p b d", p=128), tmt[:, 0:nb, 512:1024])
        P.barrier()
        if stop_after == f"D1_{l}":
            return finish(nc, P, out_dram)

        if _only == "D2":
            P.mute = False
        _zero[0] = False
        with ExitStack() as st:
            cSame = sb(st, "cSame", [128, 128])
            P.I("dve", "tensor_tensor", out=cSame[:], in0=cLo[:], in1=cSU[:], op=ALU.add)
            identb = sb(st, "identb", [128, 128], BF16)
            P.I("dve", "tensor_copy", out=identb[:], in_=ident[:])
            def run_dir(dr):
                Sst = [sb(st, f"S{h}", [128, 128]) for h in range(4)]
                Sb = [sb(st, f"Sb{h}", [128, 128], BF16) for h in range(4)]
                qTb_l = [sb(st, f"d2qTb{i}", [128, 4, 128], BF16) for i in range(2)]
                kTb_l = [sb(st, f"d2kTb{i}", [128, 4, 128], BF16) for i in range(2)]
                ktm_l = [sb(st, f"d2ktm{i}", [128, 512]) for i in range(2)]
                vtm_l = [sb(st, f"d2vtm{i}", [128, 512]) for i in range(2)]
                ktm2_l = [sb(st, f"d2ktm2{i}", [64, 2, 512]) for i in range(2)]
                bgt_l = [sb(st, f"d2bg{i}", [128, 16]) for i in range(2)]
                gs = sb(st, "d2gs", [128, 24])
                egc = sb(st, "d2egc", [128, 4])
                sb1 = sb(st, "d2sb1", [128, 4])
                nbeta = sb(st, "d2nbeta", [128, 4])
                egcc = sb(st, "d2egcc", [64, 2, 4])
                edec = sb(st, "d2edec", [64, 2, 4])
                gtc = sb(st, "d2gtc", [128, 2, 4])
                Dm = [sb(st, f"d2D{h}", [128, 128]) for h in range(4)]
                D1m = [sb(st, f"d2D1{h}", [128, 128]) for h in range(4)]
                tmpm = [sb(st, f"d2tmp{h}", [128, 128]) for h in range(4)]
                Nm = [[sb(st, f"d2N{h}_{i}", [128, 128], BF16) for i in range(2)] for h in range(4)]
                NTm = [[sb(st, f"d2NT{h}_{i}", [128, 128], BF16) for i in range(2)] for h in range(4)]
                RT = [sb(st, f"d2RT{h}", [128, 128], BF16) for h in range(4)]
                Am = [sb(st, f"d2A{h}", [128, 128], BF16) for h in range(4)]
                vb = [sb(st, f"d2vb{h}", [128, 128], BF16) for h in range(4)]
                kbg = [sb(st, f"d2kbg{h}", [128, 128], BF16) for h in range(4)]
                um = [sb(st, f"d2u{h}", [64, 2, 128]) for h in range(4)]
                wT = [sb(st, f"d2wT{h}", [128, 128], BF16) for h in range(4)]
                ATm = [sb(st, f"d2AT{h}", [64, 2, 128], BF16) for h in range(4)]
                kdec = [sb(st, f"d2kdec{h}", [64, 2, 128], BF16) for h in range(4)]
                vnew = [sb(st, f"d2vnew{h}", [64, 128], BF16) for h in range(4)]
                avs = [sb(st, f"d2avs{h}", [64, 128]) for h in range(4)]
                osb = sb(st, "d2o", [64, 2, 512])
                H4 = range(4)
                q0, q1, q2, q3 = (ps[0:4] if dr == 0 else ps[4:8])
                Mcs = cU if dr == 0 else cLo
                Mstrict = cSLo if dr == 0 else cSU
                Mincl = cLo if dr == 0 else cU
                for h in H4:
                    P.I("dve", "memset", ap=Sst[h][:], constant=0.0)
                    P.I("dve", "memset", ap=Sb[h][:], constant=0.0)
                lat = list(range(0, 32)) if dr == 0 else list(range(31, -1, -1))
                cxt = [32, 33] if dr == 0 else [33, 32]
                import os as _os
                _lim = int(_os.environ.get("KDBG_D2TILES", "1000"))
                for tn, tix in enumerate((cxt + lat)[:_lim]):
                    t0 = tix * 128
                    qTb, kTb, ktm, vtm, ktm2, bgt = (x[tn % 2] for x in (qTb_l, kTb_l, ktm_l, vtm_l, ktm2_l, bgt_l))
                    P.dma("sp", qTb[:], qnTb.rearrange("(h p) t -> p h t", p=128)[:, :, t0:t0 + 128])
                    P.dma("act", kTb[:], knTb.rearrange("(h p) t -> p h t", p=128)[:, :, t0:t0 + 128])
                    P.dma("act", ktm[:], k_tm[t0:t0 + 128, :])
                    P.dma("act", vtm[:], v_tm[t0:t0 + 128, :])
                    P.dma("sp", ktm2[:], k_tm[t0:t0 + 128, :].rearrange("(c p) d -> p c d", p=64))
                    P.dma("sp", bgt[:], bg_tm[t0:t0 + 128, :])
                    g4 = bgt[:, 8 + dr * 4:12 + dr * 4]
                    b4 = bgt[:, dr * 4:dr * 4 + 4]
                    pg = q0
                    P.mm(pg[:, 0:4], Mcs[:], g4)
                    P.mm(pg[:, 4:8], cSame[:], g4)
                    P.mm(pg[:, 8:12], cSC0[:], g4)
                    P.mm(pg[:, 12:16], cSC1[:], g4)
                    P.mm(pg[0:64, 16:20], Mcs[:, 0:64], g4)
                    P.mm(pg[0:64, 20:24], Mcs[:, 64:128], g4)
                    P.I("dve", "tensor_copy", out=gs[:, 0:16], in_=pg[:, 0:16])
                    P.I("dve", "tensor_copy", out=gs[0:64, 16:24], in_=pg[0:64, 16:24])
                    P.I("act", "activation", out=egc[:], in_=gs[:, 0:4], func=AF.Exp)
                    P.I("dve", "tensor_tensor", out=sb1[:], in0=egc[:], in1=b4, op=ALU.mult)
                    P.I("dve", "tensor_scalar", out=nbeta[:], in0=b4, scalar1=-1.0, scalar2=None, op0=ALU.mult)
                    P.I("act", "activation", out=egcc[:].rearrange("p c h -> p (c h)"), in_=gs[0:64, 16:24], func=AF.Exp)
                    P.I("dve", "tensor_tensor", out=edec[:].rearrange("p c h -> p (c h)"), in0=gs[0:64, 8:16], in1=gs[0:64, 16:24], op=ALU.subtract)
                    P.I("act", "activation", out=edec[:].rearrange("p c h -> p (c h)"), in_=edec[:].rearrange("p c h -> p (c h)"), func=AF.Exp)
                    P.I("act", "activation", out=gtc[:].rearrange("p c h -> p (c h)"), in_=gs[:, 8:16], func=AF.Exp)
                    yield
                    for h in H4:
                        P.I("dve", "tensor_scalar", out=Dm[h][:], in0=ident[:], scalar1=gs[:, h:h + 1], scalar2=None, op0=ALU.mult)
                    yield
                    for h in H4:
                        hs = slice(h * 128, (h + 1) * 128)
                        P.mm(q1[:, hs], ones[:], Dm[h][:])
                        P.mm(q2[:, hs], kTb[:, h, :], kTb[:, h, :])
                        P.mm(q3[:, hs], qTb[:, h, :], kTb[:, h, :])
                    yield
                    for h in H4:
                        hs = slice(h * 128, (h + 1) * 128)
                        P.I("dve", "tensor_scalar", out=D1m[h][:], in0=q1[:, hs], scalar1=gs[:, h:h + 1], scalar2=0.0, op0=ALU.subtract, op1=ALU.max)
                        P.I("act", "activation", out=D1m[h][:], in_=D1m[h][:], func=AF.Exp, scale=-1.0)
                        P.I("dve", "tensor_tensor", out=tmpm[h][:], in0=q2[:, hs], in1=D1m[h][:], op=ALU.mult)
                        P.I("dve", "tensor_scalar", out=tmpm[h][:], in0=tmpm[h][:], scalar1=nbeta[:, h:h + 1], scalar2=None, op0=ALU.mult)
                        P.I("dve", "tensor_tensor", out=Nm[h][0][:], in0=tmpm[h][:], in1=Mstrict[:], op=ALU.mult)
                        P.I("dve", "tensor_tensor", out=Am[h][:], in0=q3[:, hs], in1=D1m[h][:], op=ALU.mult)
                        P.I("dve", "tensor_tensor", out=Am[h][:], in0=Am[h][:], in1=Mincl[:], op=ALU.mult)
                    yield
                    for h in H4:
                        hs = slice(h * 128, (h + 1) * 128)
                        P.mm((q0 if h < 2 else q1)[:, hs], Nm[h][0][:], identb[:])
                    yield
                    for h in H4:
                        hs = slice(h * 128, (h + 1) * 128)
                        P.I("dve", "tensor_copy", out=NTm[h][0][:], in_=(q0 if h < 2 else q1)[:, hs])
                        P.I("dve", "tensor_tensor", out=RT[h][:], in0=(q0 if h < 2 else q1)[:, hs], in1=ident[:], op=ALU.add)
                    cur = 0
                    yield
                    for kk in range(5):
                        nxt = 1 - cur
                        yield
                        for h in H4:
                            hs = slice(h * 128, (h + 1) * 128)
                            P.mm(q0[:, hs], NTm[h][cur][:], Nm[h][cur][:])
                            if kk < 4:
                                P.mm(q1[:, hs], Nm[h][cur][:], NTm[h][cur][:])
                        yield
                        for h in H4:
                            hs = slice(h * 128, (h + 1) * 128)
                            P.I("act", "activation", out=Nm[h][nxt][:], in_=q0[:, hs], func=AF.Copy)
                            if kk < 4:
                                P.I("dve", "tensor_copy", out=NTm[h][nxt][:], in_=q1[:, hs])
                        yield
                        for h in H4:
                            hs = slice(h * 128, (h + 1) * 128)
                            P.mm(q2[:, hs], Nm[h][nxt][:], RT[h][:])
                        yield
                        for h in H4:
                            hs = slice(h * 128, (h + 1) * 128)
                            P.I("dve", "tensor_tensor", out=RT[h][:], in0=q2[:, hs], in1=RT[h][:], op=ALU.add)
                        cur = nxt
                    yield
                    for h in H4:
                        hs = slice(h * 128, (h + 1) * 128)
                        P.I("dve", "tensor_scalar", out=vb[h][:], in0=vtm[:, hs], scalar1=b4[:, h:h + 1], scalar2=None, op0=ALU.mult)
                        P.I("dve", "tensor_scalar", out=kbg[h][:], in0=ktm[:, hs], scalar1=sb1[:, h:h + 1], scalar2=None, op0=ALU.mult)
                        for c in range(2):
                            P.I("dve", "tensor_scalar", out=kdec[h][:, c, :], in0=ktm2[:, c, hs], scalar1=edec[:, c, h:h + 1], scalar2=None, op0=ALU.mult)
                    yield
                    for h in H4:
                        hs = slice(h * 128, (h + 1) * 128)
                        P.mm(q0[0:64, hs], RT[h][:, 0:64], vb[h][:])
                        P.mm(q1[0:64, hs], RT[h][:, 64:128], vb[h][:])
                        P.mm(q3[:, hs], kbg[h][:], RT[h][:])
                        P.mm(q2[0:64, hs], Am[h][:, 0:64], identb[:])
                    yield
                    for h in H4:
                        hs = slice(h * 128, (h + 1) * 128)
                        P.I("act", "activation", out=um[h][:, 0, :], in_=q0[0:64, hs], func=AF.Copy)
                        P.I("dve", "tensor_copy", out=um[h][:, 1, :], in_=q1[0:64, hs])
                        P.I("act", "activation", out=wT[h][:], in_=q3[:, hs], func=AF.Copy)
                        P.I("dve", "tensor_copy", out=ATm[h][:, 0, :], in_=q2[0:64, hs])
                    yield
                    for h in H4:
                        hs = slice(h * 128, (h + 1) * 128)
                        P.mm(q0[0:64, hs], Am[h][:, 64:128], identb[:])
                    yield
                    for h in H4:
                        hs = slice(h * 128, (h + 1) * 128)
                        P.I("act", "activation", out=ATm[h][:, 1, :], in_=q0[0:64, hs], func=AF.Copy)
                    yield
                    for c in ([0, 1] if dr == 0 else [1, 0]):
                        cs = slice(c * 64, (c + 1) * 64)
                        yield
                        for h in H4:
                            hs = slice(h * 128, (h + 1) * 128)
                            P.mm(q1[0:64, hs], wT[h][:, cs], Sb[h][:])
                            P.mm(q2[0:64, hs], qTb[:, h, cs], Sb[h][:])
                        yield
                        for h in H4:
                            hs = slice(h * 128, (h + 1) * 128)
                            P.I("dve", "tensor_tensor", out=vnew[h][:], in0=um[h][:, c, :], in1=q1[0:64, hs], op=ALU.subtract)
                        yield
                        for h in H4:
                            hs = slice(h * 128, (h + 1) * 128)
                            P.mm(q3[0:64, hs], ATm[h][:, c, cs], vnew[h][:])
                            P.mm(q0[:, hs], kdec[h][:, c, :], vnew[h][:])
                        yield
                        for h in H4:
                            hs = slice(h * 128, (h + 1) * 128)
                            P.I("act", "activation", out=avs[h][:], in_=q3[0:64, hs], func=AF.Copy)
                            P.I("dve", "scalar_tensor_tensor", out=osb[:, c, hs], in0=q2[0:64, hs], scalar=egcc[:, c, h:h + 1],
                                in1=avs[h][:], op0=ALU.mult, op1=ALU.add)
                            P.I("dve", "scalar_tensor_tensor", out=Sst[h][:], in0=Sst[h][:], scalar=gtc[:, c, h:h + 1],
                                in1=q0[:, hs], op0=ALU.mult, op1=ALU.add)
                            P.I("act", "activation", out=Sb[h][:], in_=Sst[h][:], func=AF.Copy)
                    P.dma("sp", o_dir[dr, t0:t0 + 128, :].rearrange("(c p) d -> p c d", p=64), osb[:])
            gens = [run_dir(0), run_dir(1)]
            while gens:
                for g_ in list(gens):
                    try:
                        next(g_)
                    except StopIteration:
                        gens.remove(g_)
        _zero[0] = False
        P.barrier()
        if stop_after == f"D2_{l}":
            return finish(nc, P, out_dram)

        TILES = [(t0, 512, 0, 0, SEQ) for t0 in range(0, SEQ, 512)] + [(SEQ, 256, 1, SEQ, NT)]
        with ExitStack() as st:
            of = sb(st, "d3of", [128, 4, 512])
            ob = sb(st, "d3ob", [128, 4, 512])
            osq = sb(st, "d3sq", [128, 4, 512])
            ms = sb(st, "d3ms", [128, 16])
            zt = sb(st, "d3z", [128, 4, 512])
            ydn = sb(st, "d3y", [128, 4, 512], BF16)
            for (t0, tw, s, s0, s1) in TILES:
                nb = tw // 128
                P.dma("sp", of[:, 0:nb, :], o_dir[0, t0:t0 + tw, :].rearrange("(b p) d -> p b d", p=128))
                P.dma("act", ob[:, 0:nb, :], o_dir[1, t0:t0 + tw, :].rearrange("(b p) d -> p b d", p=128))
                P.dma("sp", zt[:, :, 0:tw], projT[R_Z:R_Z + 512, :].rearrange("(c p) t -> p c t", p=128)[:, :, t0:t0 + tw])
                P.I("dve", "tensor_tensor", out=of[:, 0:nb, :], in0=of[:, 0:nb, :], in1=ob[:, 0:nb, :], op=ALU.add)
                P.I("dve", "tensor_tensor", out=osq[:, 0:nb, :], in0=of[:, 0:nb, :], in1=of[:, 0:nb, :], op=ALU.mult)
                P.I("dve", "reduce_sum", out=ms[:, 0:nb * 4], in_=osq[:, 0:nb, :].rearrange("p b (h d) -> p (b h) d", h=4), axis=mybir.AxisListType.X)
                P.I("act", "activation", out=ms[:, 0:nb * 4], in_=ms[:, 0:nb * 4], func=AF.Sqrt, scale=1.0 / 128, bias=EPS)
                P.I("dve", "reciprocal", out=ms[:, 0:nb * 4], in_=ms[:, 0:nb * 4])
                P.I("dve", "tensor_tensor", out=of[:, 0:nb, :].rearrange("p b (h d) -> p (b h) d", h=4),
                    in0=of[:, 0:nb, :].rearrange("p b (h d) -> p (b h) d", h=4),
                    in1=ms[:, 0:nb * 4].unsqueeze(2).to_broadcast([128, nb * 4, 128]), op=ALU.mult)
                P.I("act", "activation", out=zt[:, :, 0:tw], in_=zt[:, :, 0:tw], func=AF.Silu)
                for h in range(4):
                    pp = ps[h % 2]
                    for b in range(nb):
                        P.tr(pp[:, b * 128:(b + 1) * 128], of[:, b, h * 128:(h + 1) * 128], ident[:])
                    P.I("dve", "scalar_tensor_tensor", out=ydn[:, h, 0:tw], in0=pp[:, 0:tw], scalar=vv[:, V_DNG:V_DNG + 1],
                        in1=zt[:, h, 0:tw], op0=ALU.mult, op1=ALU.mult)
                P.dma("sp", y_dnT.rearrange("(c p) t -> p c t", p=128)[:, :, t0:t0 + tw], ydn[:, :, 0:tw])
        P.barrier()

        with ExitStack() as st:
            shh = sb(st, "p3sh", [128, 4, 514])
            scc = sb(st, "p3sc", [128, 4, 514])
            sbb = sb(st, "p3sb", [128, 4, 512])
            acc3 = sb(st, "p3acc", [128, 4, 512])
            ysc = sb(st, "p3y", [128, 4, 512], BF16)
            for (t0, tw, s, s0, s1) in TILES:
                lo, hi = max(t0 - 1, s0), min(t0 + tw + 1, s1)
                for tt, r0 in ((shh, R_SH), (scc, R_SC)):
                    P.I("dve", "memset", ap=tt[:, :, 0:1], constant=0.0)
                    P.I("dve", "memset", ap=tt[:, :, tw + 1:tw + 2], constant=0.0)
                    P.dma("sp", tt[:, :, lo - (t0 - 1):hi - (t0 - 1)],
                          projT[r0:r0 + 512, :].rearrange("(c p) t -> p c t", p=128)[:, :, lo:hi])
                P.dma("act", sbb[:, :, 0:tw], projT[R_SB:R_SB + 512, :].rearrange("(c p) t -> p c t", p=128)[:, :, t0:t0 + tw])
                P.I("dve", "tensor_tensor", out=shh[:, :, 0:tw + 2], in0=shh[:, :, 0:tw + 2], in1=scc[:, :, 0:tw + 2], op=ALU.mult)
                for j in range(4):
                    P.I("dve", "tensor_scalar", out=acc3[:, j, 0:tw], in0=shh[:, j, 0:tw], scalar1=vv[:, V_SCCW + j:V_SCCW + j + 1],
                        scalar2=None, op0=ALU.mult)
                    P.I("dve", "scalar_tensor_tensor", out=acc3[:, j, 0:tw], in0=shh[:, j, 1:tw + 1], scalar=vv[:, V_SCCW + 4 + j:V_SCCW + 5 + j],
                        in1=acc3[:, j, 0:tw], op0=ALU.mult, op1=ALU.add)
                    P.I("dve", "scalar_tensor_tensor", out=acc3[:, j, 0:tw], in0=shh[:, j, 2:tw + 2], scalar=vv[:, V_SCCW + 8 + j:V_SCCW + 9 + j],
                        in1=acc3[:, j, 0:tw], op0=ALU.mult, op1=ALU.add)
                P.I("dve", "tensor_tensor", out=ysc[:, :, 0:tw], in0=acc3[:, :, 0:tw], in1=sbb[:, :, 0:tw], op=ALU.mult)
                P.dma("sp", y_scT.rearrange("(c p) t -> p c t", p=128)[:, :, t0:t0 + tw], ysc[:, :, 0:tw])
        P.barrier()
        if stop_after == f"P3_{l}":
            return finish(nc, P, out_dram)

        with ExitStack() as st:
            wqn = sb(st, "wqn", [128, 2, 4, 128], BF16)
            wqr = sb(st, "wqr", [128, 2, 4, 64], BF16)
            wkn = sb(st, "wkn", [128, 4, 128], BF16)
            wkv = sb(st, "wkv", [128, 4, 128], BF16)
            wq_v = mla_w_qb_in[l].rearrange("(kc p) (h x) -> p kc h x", p=128, x=192)
            for kc in range(2):
                P.dma("pool", wqn[:, kc, :, :], wq_v[:, kc, :, 0:128])
                P.dma("pool", wqr[:, kc, :, :], wq_v[:, kc, :, 128:192])
            wk_v = mla_w_kvb_in[l].rearrange("p (h x) -> p h x", x=256)
            P.dma("pool", wkn[:], wk_v[:, :, 0:128])
            P.dma("pool", wkv[:], wk_v[:, :, 128:256])
            qa = sb(st, "p4qa", [128, 2, 512])
            qsq = sb(st, "p4qsq", [128, 2, 512])
            rr = sb(st, "p4rr", [128, 512])
            qan = sb(st, "p4qan", [128, 2, 512], BF16)
            qn_o = sb(st, "p4qn", [128, 4, 512], BF16)
            qr_o = sb(st, "p4qr", [128, 2, 512], BF16)
            xr = sb(st, "p4xr", [128, 512])
            t1 = sb(st, "p4t1", [128, 512])
            t2 = sb(st, "p4t2", [128, 512])
            rc = sb(st, "p4rc", [128, 512])
            rs = sb(st, "p4rs", [128, 512])
            ckv = sb(st, "p4ckv", [128, 512])
            ckvn = sb(st, "p4ckvn", [128, 512], BF16)
            kn_o = sb(st, "p4kn", [128, 4, 512], BF16)
            v_o = sb(st, "p4v", [128, 4, 512], BF16)
            kr = sb(st, "p4kr", [128, 512])
            kr_o = sb(st, "p4kro", [128, 512], BF16)
            for (t0, tw, s, s0, s1) in TILES:
                nb = tw // 128
                P.dma("sp", qa[:, :, 0:tw], projT[R_QA:R_QA + 256, :].rearrange("(c p) t -> p c t", p=128)[:, :, t0:t0 + tw])
                P.dma("act", ckv[:, 0:tw], projT[R_CKV:R_CKV + 128, t0:t0 + tw])
                P.dma("sp", kr[0:64, 0:tw], projT[R_KR:R_KR + 64, t0:t0 + tw])
                P.dma("sp", kr[64:128, 0:tw], projT[R_KR:R_KR + 64, t0:t0 + tw])
                P.dma("act", rc[:, 0:tw], rope_in[0, :, t0:t0 + tw])
                P.dma("act", rs[:, 0:tw], rope_in[1, :, t0:t0 + tw])
                P.I("act", "activation", out=qsq[:, :, 0:tw], in_=qa[:, :, 0:tw], func=AF.Square)
                for c in range(2):
                    P.mm(ps[0][:, 0:tw], ones[:], qsq[:, c, 0:tw], start=(c == 0), stop=(c == 1))
                P.I("act", "activation", out=rr[:, 0:tw], in_=ps[0][:, 0:tw], func=AF.Sqrt, scale=1.0 / 256, bias=EPS)
                P.I("dve", "reciprocal", out=rr[:, 0:tw], in_=rr[:, 0:tw])
                for c in range(2):
                    P.I("dve", "scalar_tensor_tensor", out=qan[:, c, 0:tw], in0=qa[:, c, 0:tw], scalar=vv[:, V_QNG + c:V_QNG + c + 1],
                        in1=rr[:, 0:tw], op0=ALU.mult, op1=ALU.mult)
                for h in range(4):
                    pp = ps[1 + h % 2]
                    for kc in range(2):
                        P.mm(pp[:, 0:tw], wqn[:, kc, h, :], qan[:, kc, 0:tw], start=(kc == 0), stop=(kc == 1))
                    P.I("act", "activation", out=qn_o[:, h, 0:tw], in_=pp[:, 0:tw], func=AF.Copy)
                for r in range(2):
                    pp = ps[3]
                    for kc in range(2):
                        P.mm(pp[:, 0:tw], wqr[:, kc, 2 * r:2 * r + 2, :], qan[:, kc, 0:tw], start=(kc == 0), stop=(kc == 1))
                    P.I("act", "activation", out=xr[:, 0:tw], in_=pp[:, 0:tw], func=AF.Copy)
                    P.mm(ps[4][:, 0:tw], ropeP[:], xr[:, 0:tw])
                    P.I("dve", "tensor_tensor", out=t1[:, 0:tw], in0=xr[:, 0:tw], in1=rc[:, 0:tw], op=ALU.mult)
                    P.I("dve", "tensor_tensor", out=t2[:, 0:tw], in0=ps[4][:, 0:tw], in1=rs[:, 0:tw], op=ALU.mult)
                    P.I("dve", "tensor_tensor", out=qr_o[:, r, 0:tw], in0=t1[:, 0:tw], in1=t2[:, 0:tw], op=ALU.add)
                P.dma("sp", qnopeT.rearrange("(c p) t -> p c t", p=128)[:, :, t0:t0 + tw], qn_o[:, :, 0:tw])
                P.dma("sp", qropeT.rearrange("(c p) t -> p c t", p=128)[:, :, t0:t0 + tw], qr_o[:, :, 0:tw])
                P.I("act", "activation", out=t1[:, 0:tw], in_=ckv[:, 0:tw], func=AF.Square)
                P.mm(ps[0][:, 0:tw], ones[:], t1[:, 0:tw])
                P.I("act", "activation", out=rr[:, 0:tw], in_=ps[0][:, 0:tw], func=AF.Sqrt, scale=1.0 / 128, bias=EPS)
                P.I("dve", "reciprocal", out=rr[:, 0:tw], in_=rr[:, 0:tw])
                P.I("dve", "scalar_tensor_tensor", out=ckvn[:, 0:tw], in0=ckv[:, 0:tw], scalar=vv[:, V_KVNG:V_KVNG + 1],
                    in1=rr[:, 0:tw], op0=ALU.mult, op1=ALU.mult)
                for h in range(4):
                    pp = ps[1 + h % 2]
                    P.mm(pp[:, 0:tw], wkn[:, h, :], ckvn[:, 0:tw])
                    P.I("act", "activation", out=kn_o[:, h, 0:tw], in_=pp[:, 0:tw], func=AF.Copy)
                for b in range(nb):
                    pp = ps[5 + b % 2]
                    P.mm(pp[:, :], ckvn[:, b * 128:(b + 1) * 128], wkv[:].rearrange("p h d -> p (h d)"))
                    P.I("act", "activation", out=v_o[:, b, :], in_=pp[:, :], func=AF.Copy)
                P.dma("sp", knopeT.rearrange("(c p) t -> p c t", p=128)[:, :, t0:t0 + tw], kn_o[:, :, 0:tw])
                P.dma("sp", v_mla[t0:t0 + tw, :].rearrange("(b p) d -> p b d", p=128), v_o[:, 0:nb, :])
                P.mm(ps[4][:, 0:tw], ropeP[:], kr[:, 0:tw])
                P.I("dve", "tensor_tensor", out=t1[:, 0:tw], in0=kr[:, 0:tw], in1=rc[:, 0:tw], op=ALU.mult)
                P.I("dve", "tensor_tensor", out=t2[:, 0:tw], in0=ps[4][:, 0:tw], in1=rs[:, 0:tw], op=ALU.mult)
                P.I("dve", "tensor_tensor", out=kr_o[:, 0:tw], in0=t1[:, 0:tw], in1=t2[:, 0:tw], op=ALU.add)
                P.dma("sp", kropeT[:, t0:t0 + tw], kr_o[:, 0:tw])
        P.barrier()
        if stop_after == f"P4a_{l}":
            return finish(nc, P, out_dram)

        with ExitStack() as st:
            kn_all = sb(st, "kn_all", [128, 4, NT], BF16)
            kr_all = sb(st, "kr_all", [128, NT], BF16)
            v_all = sb(st, "v_all", [128, 34, 512], BF16)
            onesb = sb(st, "onesb", [128, 128], BF16)
            P.I("dve", "tensor_copy", out=onesb[:], in_=ones[:])
            P.dma("sp", kn_all[:], knopeT.rearrange("(c p) t -> p c t", p=128))
            P.dma("act", kr_all[:], kropeT[:, :])
            P.dma("sp", v_all[:], v_mla.rearrange("(b p) d -> p b d", p=128))
            qn_t = [sb(st, f"qn_t{i}", [128, 4, 512], BF16) for i in range(2)]
            qr_t = [sb(st, f"qr_t{i}", [128, 2, 512], BF16) for i in range(2)]
            ptb = [sb(st, f"ptb{i}", [128, 512], BF16) for i in range(2)]
            rinv = sb(st, "rinv", [128, 512])
            ym = sb(st, "ym", [128, 4, 512], BF16)
            SCALE = 192.0 ** -0.5
            for ti, (t0, tw, s, s0, s1) in enumerate(TILES):
                kbs = list(range(34)) if s == 0 else [32, 33]
                qn_ = qn_t[ti % 2]
                qr_ = qr_t[ti % 2]
                P.dma("sp", qn_[:, :, 0:tw], qnopeT.rearrange("(c p) t -> p c t", p=128)[:, :, t0:t0 + tw])
                P.dma("act", qr_[:, :, 0:tw], qropeT.rearrange("(c p) t -> p c t", p=128)[:, :, t0:t0 + tw])
                for h in range(4):
                    po = ps[2 + (h % 2) * 2]
                    pl = ps[3 + (h % 2) * 2]
                    hp = (h % 2) * 64

                    def emit_st(i, kb):
                        pst = ps[i % 2]
                        ks = slice(kb * 128, (kb + 1) * 128)
                        P.mm(pst[:, 0:tw], kn_all[:, h, ks], qn_[:, h, 0:tw], start=True, stop=False)
                        P.mm(pst[:, 0:tw], kr_all[hp:hp + 64, ks], qr_[hp:hp + 64, h // 2, 0:tw], start=False, stop=True)

                    emit_st(0, kbs[0])
                    for i, kb in enumerate(kbs):
                        if i + 1 < len(kbs):
                            emit_st(i + 1, kbs[i + 1])
                        pt_ = ptb[i % 2]
                        P.I("act", "activation", out=pt_[:, 0:tw], in_=ps[i % 2][:, 0:tw], func=AF.Exp, scale=SCALE)
                        first, last = (i == 0), (i == len(kbs) - 1)
                        P.mm(po[:, 0:tw], v_all[:, kb, h * 128:(h + 1) * 128], pt_[:, 0:tw], start=first, stop=last)
                        P.mm(pl[:, 0:tw], onesb[:], pt_[:, 0:tw], start=first, stop=last)
                    P.I("dve", "reciprocal", out=rinv[:, 0:tw], in_=pl[:, 0:tw])
                    P.I("dve", "tensor_tensor", out=ym[:, h, 0:tw], in0=po[:, 0:tw], in1=rinv[:, 0:tw], op=ALU.mult)
                P.dma("sp", y_mlaT.rearrange("(c p) t -> p c t", p=128)[:, :, t0:t0 + tw], ym[:, :, 0:tw])
        P.barrier()
        if stop_after == f"P4_{l}":
            return finish(nc, P, out_dram)

        last = (l == DEPTH - 1)
        PT = [t for t in TILES if not (last and t[2] == 1)]
        with ExitStack() as st:
            wg = sb(st, "wg", [128, 8, 3072], BF16)
            wbr = [sb(st, f"wbr{i}", [128, 4, 1024], BF16) for i in range(3)]
            wo = sb(st, "wo", [128, 8, 1024], BF16)
            wr32 = sb(st, "wr32", [128, 8, 32])
            P.dma("pool", wg[:], w_gate_in[l].rearrange("(kc p) n -> p kc n", p=128))
            for i, wsrc in enumerate((w_dn_in, w_sc_in, w_mla_in)):
                P.dma("pool", wbr[i][:], wsrc[l].rearrange("(kc p) n -> p kc n", p=128))
            P.dma("pool", wo[:], w_out_in[l].rearrange("(kc p) n -> p kc n", p=128))
            P.dma("sp", wr32[:], router_w_in[l].rearrange("(kc p) n -> p kc n", p=128))
            xb5 = sb(st, "p5xb", [128, 8, 512], BF16)
            ybr = [sb(st, f"p5y{i}", [128, 4, 512], BF16) for i in range(3)]
            ht5 = sb(st, "p5ht", [128, 8, 512])
            sig5 = [sb(st, f"p5sig{i}", [128, 512]) for i in range(2)]
            mrg = sb(st, "p5mrg", [128, 512])
            tm5 = sb(st, "p5tm", [128, 512])
            mg = sb(st, "p5mg", [128, 8, 512], BF16)
            sq5 = sb(st, "p5sq", [128, 8, 512], BF16)
            rs5 = sb(st, "p5rs", [128, 512])
            x32 = sb(st, "p5x32", [128, 8, 512])
            x2b = sb(st, "p5x2b", [128, 8, 512], BF16)
            lg = sb(st, "p5lg", [128, 4, 32])
            mx8 = sb(st, "p5mx", [128, 8])
            msk = sb(st, "p5msk", [128, 32])
            ee = sb(st, "p5e", [128, 32])
            sm = sb(st, "p5sm", [128, 2])
            gts = sb(st, "p5gts", [128, 4, 32])
            gTs = sb(st, "p5gT", [32, 512])
            nsig = 0
            for (t0, tw, s, s0, s1) in PT:
                nb = tw // 128
                P.dma("sp", xb5[:, :, 0:tw], xmT.rearrange("(c p) t -> p c t", p=128)[:, :, t0:t0 + tw])
                for i, ysrc in enumerate((y_dnT, y_scT, y_mlaT)):
                    P.dma("act", ybr[i][:, :, 0:tw], ysrc.rearrange("(c p) t -> p c t", p=128)[:, :, t0:t0 + tw])
                P.dma("sp", ht5[:, :, 0:tw], hT.rearrange("(c p) t -> p c t", p=128)[:, :, t0:t0 + tw])
                for c in range(8):
                    for br in range(3):
                        pgt = ps[br % 2]
                        for k in range(8):
                            P.mm(pgt[:, 0:tw], wg[:, k, br * 1024 + c * 128:br * 1024 + (c + 1) * 128], xb5[:, k, 0:tw], start=(k == 0), stop=(k == 7))
                        sg = sig5[nsig % 2]
                        nsig += 1
                        P.I("act", "activation", out=sg[:, 0:tw], in_=pgt[:, 0:tw], func=AF.Sigmoid,
                            bias=vv[:, V_BG + br * 8 + c:V_BG + br * 8 + c + 1])
                        ppr = ps[2 + br % 2]
                        for k in range(4):
                            P.mm(ppr[:, 0:tw], wbr[br][:, k, c * 128:(c + 1) * 128], ybr[br][:, k, 0:tw], start=(k == 0), stop=(k == 3))
                        if br == 0:
                            P.I("dve", "tensor_tensor", out=mrg[:, 0:tw], in0=ppr[:, 0:tw], in1=sg[:, 0:tw], op=ALU.mult)
                        else:
                            P.I("dve", "tensor_tensor", out=tm5[:, 0:tw], in0=ppr[:, 0:tw], in1=sg[:, 0:tw], op=ALU.mult)
                            if br == 1:
                                P.I("dve", "tensor_tensor", out=mrg[:, 0:tw], in0=mrg[:, 0:tw], in1=tm5[:, 0:tw], op=ALU.add)
                            else:
                                P.I("dve", "tensor_tensor", out=mg[:, c, 0:tw], in0=mrg[:, 0:tw], in1=tm5[:, 0:tw], op=ALU.add)
                for c in range(8):
                    pp = ps[4 + c % 2]
                    for k in range(8):
                        P.mm(pp[:, 0:tw], wo[:, k, c * 128:(c + 1) * 128], mg[:, k, 0:tw], start=(k == 0), stop=(k == 7))
                    P.I("dve", "scalar_tensor_tensor", out=ht5[:, c, 0:tw], in0=pp[:, 0:tw], scalar=m[:, s, 2, c:c + 1],
                        in1=ht5[:, c, 0:tw], op0=ALU.mult, op1=ALU.add)
                P.dma("sp", hT.rearrange("(c p) t -> p c t", p=128)[:, :, t0:t0 + tw], ht5[:, :, 0:tw])
                P.I("act", "activation", out=x32[:, :, 0:tw], in_=ht5[:, :, 0:tw], func=AF.Square)
                for c in range(8):
                    P.mm(ps[6][:, 0:tw], ones[:], x32[:, c, 0:tw], start=(c == 0), stop=(c == 7))
                P.I("act", "activation", out=rs5[:, 0:tw], in_=ps[6][:, 0:tw], func=AF.Sqrt, scale=1.0 / D, bias=EPS)
                P.I("dve", "reciprocal", out=rs5[:, 0:tw], in_=rs5[:, 0:tw])
                for c in range(8):
                    P.I("dve", "scalar_tensor_tensor", out=tm5[:, 0:tw], in0=ht5[:, c, 0:tw], scalar=m[:, s, 3, c:c + 1],
                        in1=rs5[:, 0:tw], op0=ALU.mult, op1=ALU.mult)
                    P.I("act", "activation", out=x32[:, c, 0:tw], in_=tm5[:, 0:tw], func=AF.Identity, bias=m[:, s, 4, c:c + 1])
                    P.I("dve", "tensor_copy", out=x2b[:, c, 0:tw], in_=x32[:, c, 0:tw])
                P.dma("sp", xm2T.rearrange("(c p) t -> p c t", p=128)[:, :, t0:t0 + tw], x2b[:, :, 0:tw])
                for b in range(nb):
                    for c in range(8):
                        P.mm(ps[7][:, b * 32:(b + 1) * 32], x32[:, c, b * 128:(b + 1) * 128], wr32[:, c, :], start=(c == 0), stop=(c == 7))
                P.I("dve", "tensor_tensor", out=lg[:, 0:nb, :], in0=ps[7][:, 0:nb * 32].rearrange("p (b e) -> p b e", e=32),
                    in1=vv[:, V_RB:V_RB + 32].unsqueeze(1).to_broadcast([128, nb, 32]), op=ALU.add)
                for b in range(nb):
                    P.I("dve", "max", out=mx8[:], in_=lg[:, b, :])
                    P.I("dve", "tensor_scalar", out=msk[:], in0=lg[:, b, :], scalar1=mx8[:, 3:4], scalar2=None, op0=ALU.is_ge)
                    P.I("dve", "tensor_scalar", out=sm[:, 0:1], in0=mx8[:, 0:1], scalar1=-1.0, scalar2=None, op0=ALU.mult)
                    P.I("act", "activation", out=ee[:], in_=lg[:, b, :], func=AF.Exp, bias=sm[:, 0:1])
                    P.I("dve", "tensor_tensor", out=ee[:], in0=ee[:], in1=msk[:], op=ALU.mult)
                    P.I("dve", "reduce_sum", out=sm[:, 1:2], in_=ee[:], axis=mybir.AxisListType.X)
                    P.I("dve", "reciprocal", out=sm[:, 1:2], in_=sm[:, 1:2])
                    P.I("dve", "tensor_scalar", out=gts[:, b, :], in0=ee[:], scalar1=sm[:, 1:2], scalar2=None, op0=ALU.mult)
                for b in range(nb):
                    P.tr(ps[6][0:32, b * 128:(b + 1) * 128], gts[:, b, :], ident[:])
                P.I("dve", "tensor_copy", out=gTs[:, 0:tw], in_=ps[6][0:32, 0:tw])
                P.dma("sp", gatesT[:, t0:t0 + tw], gTs[:, 0:tw])
        P.barrier()
        if stop_after == f"P5_{l}":
            return finish(nc, P, out_dram)

        with ExitStack() as st:
            GMAX = 1024
            w1b = [sb(st, f"w1b{i}", [128, 8, 2048], BF16) for i in range(2)]
            w2b = [sb(st, f"w2b{i}", [128, 8, 1024], BF16) for i in range(2)]
            yacc = sb(st, "yacc", [128, 8, GMAX])
            xg = sb(st, "xg", [128, 8, GMAX], BF16)
            gTg = sb(st, "gTg", [32, GMAX])
            actb = [sb(st, f"actb{i}", [128, 8, 512], BF16) for i in range(2)]
            gbb = [sb(st, f"m_gb{i}", [128, 512]) for i in range(2)]
            a1b = [sb(st, f"m_a1{i}", [128, 512]) for i in range(2)]
            a2b = [sb(st, f"m_a2{i}", [128, 512]) for i in range(2)]
            glub = [sb(st, f"m_glu{i}", [128, 512]) for i in range(2)]
            sgb = [sb(st, f"m_sig{i}", [128, 512]) for i in range(2)]
            linb = [sb(st, f"m_lin{i}", [128, 512]) for i in range(2)]
            lin = linb[0]
            nfc = 0
            htc = [sb(st, f"m_htc{i}", [128, 512]) for i in range(2)]
            selE = sb(st, "selE", [32, 128])
            b2sb = sb(st, "b2sb", [32, 1024])
            P.dma("sp", b2sb[:], expert_b2_in[l])
            pend = [None]

            def y_phase(wb2, ab, o, tw, e):
                for c in range(8):
                    py = ps[4 + c % 2]
                    for fc in range(8):
                        P.mm(py[:, 0:tw], wb2[:, fc, c * 128:(c + 1) * 128], ab[:, fc, 0:tw], start=(fc == 0), stop=(fc == 7))
                    if e == 0:
                        P.I("dve", "tensor_copy", out=yacc[:, c, o:o + tw], in_=py[:, 0:tw])
                    else:
                        P.I("dve", "tensor_tensor", out=yacc[:, c, o:o + tw], in0=py[:, 0:tw], in1=yacc[:, c, o:o + tw], op=ALU.add)

            groups = [(0, 1024), (1024, 1024), (2048, 1024), (3072, 1024)] + ([] if last else [(SEQ, 256)])
            groups = groups[:int(_os.environ.get("KDBG_GROUPS", "100"))]
            nact = 0
            for (g0, gw) in groups:
                s = 1 if g0 >= SEQ else 0
                P.dma("sp", xg[:, :, 0:gw], xm2T.rearrange("(c p) t -> p c t", p=128)[:, :, g0:g0 + gw])
                P.dma("sp", gTg[:, 0:gw], gatesT[:, g0:g0 + gw])
                gtiles = [(o, min(512, gw - o)) for o in range(0, gw, 512)]
                for e in range(ne):
                    wb1, wb2 = w1b[e % 2], w2b[e % 2]
                    P.dma("pool", wb1[:], w1_in[l, e].rearrange("(kc p) n -> p kc n", p=128))
                    P.dma("pool", wb2[:], w2_in[l, e].rearrange("(kc p) n -> p kc n", p=128))
                    P.I("dve", "tensor_scalar", out=selE[:], in0=ones[0:32, :], scalar1=ident[0:32, e:e + 1], scalar2=None, op0=ALU.mult)
                    w1v = wb1[:].rearrange("p k (f two) -> p k f two", two=2)
                    for (o, tw) in gtiles:
                        ab = actb[nact % 2]
                        gb = gbb[nact % 2]
                        nact += 1
                        P.mm(ps[6][:, 0:tw], selE[:], gTg[:, o:o + tw])
                        P.I("act", "activation", out=gb[:, 0:tw], in_=ps[6][:, 0:tw], func=AF.Copy)
                        for fc in range(8):
                            pg_, pl_ = ps[(fc % 2) * 2], ps[(fc % 2) * 2 + 1]
                            for k in range(8):
                                P.mm(pg_[:, 0:tw], w1v[:, k, fc * 128:(fc + 1) * 128, 0], xg[:, k, o:o + tw], start=(k == 0), stop=(k == 7))
                            for k in range(8):
                                P.mm(pl_[:, 0:tw], w1v[:, k, fc * 128:(fc + 1) * 128, 1], xg[:, k, o:o + tw], start=(k == 0), stop=(k == 7))
                            a1, a2, gl_, sg_, ln_ = a1b[nfc % 2], a2b[nfc % 2], glub[nfc % 2], sgb[nfc % 2], linb[nfc % 2]
                            nfc += 1
                            P.I("act", "activation", out=a1[:, 0:tw], in_=pg_[:, 0:tw], func=AF.Identity,
                                bias=vv[:, V_B1G + e * 8 + fc:V_B1G + e * 8 + fc + 1])
                            P.I("act", "activation", out=a2[:, 0:tw], in_=pl_[:, 0:tw], func=AF.Identity,
                                bias=vv[:, V_B1L + e * 8 + fc:V_B1L + e * 8 + fc + 1])
                            P.I("dve", "tensor_scalar_min", out=gl_[:, 0:tw], in0=a1[:, 0:tw], scalar1=7.0)
                            P.I("act", "activation", out=sg_[:, 0:tw], in_=gl_[:, 0:tw], func=AF.Sigmoid, scale=1.702)
                            P.I("dve", "tensor_scalar", out=ln_[:, 0:tw], in0=a2[:, 0:tw], scalar1=7.0, scalar2=-7.0, op0=ALU.min, op1=ALU.max)
                            P.I("dve", "scalar_tensor_tensor", out=ln_[:, 0:tw], in0=ln_[:, 0:tw], scalar=1.0, in1=gl_[:, 0:tw], op0=ALU.add, op1=ALU.mult)
                            P.I("dve", "tensor_tensor", out=ln_[:, 0:tw], in0=ln_[:, 0:tw], in1=sg_[:, 0:tw], op=ALU.mult)
                            P.I("dve", "tensor_tensor", out=ab[:, fc, 0:tw], in0=ln_[:, 0:tw], in1=gb[:, 0:tw], op=ALU.mult)
                        if pend[0] is not None:
                            y_phase(*pend[0])
                        pend[0] = (wb2, ab, o, tw, e)
                if pend[0] is not None:
                    y_phase(*pend[0])
                    pend[0] = None
                if "yaccT" in dbg:
                    P.dma("sp", yaccT.rearrange("(c p) t -> p c t", p=128)[:, :, g0:g0 + gw], yacc[:, :, 0:gw])
                nh = 0
                for (o, tw) in gtiles:
                    for c in range(8):
                        hc = htc[nh % 2]
                        nh += 1
                        P.dma("sp", hc[:, 0:tw], hT[c * 128:(c + 1) * 128, g0 + o:g0 + o + tw])
                        P.mm(ps[7][:, 0:tw], b2sb[:, c * 128:(c + 1) * 128], gTg[:, o:o + tw])
                        P.I("dve", "tensor_tensor", out=lin[:, 0:tw], in0=ps[7][:, 0:tw], in1=yacc[:, c, o:o + tw], op=ALU.add)
                        P.I("dve", "scalar_tensor_tensor", out=hc[:, 0:tw], in0=lin[:, 0:tw], scalar=m[:, s, 5, c:c + 1],
                            in1=hc[:, 0:tw], op0=ALU.mult, op1=ALU.add)
                        P.dma("sp", hT[c * 128:(c + 1) * 128, g0 + o:g0 + o + tw], hc[:, 0:tw])
        P.barrier()
        if stop_after == f"P6_{l}":
            return finish(nc, P, out_dram)

    with ExitStack() as st:
        vv = vecs[DEPTH - 1]
        htf = [sb(st, f"f_ht{i}", [128, 8, 512]) for i in range(2)]
        sqf = sb(st, "f_sq", [128, 8, 512])
        rsf = sb(st, "f_rs", [128, 512])
        xnf = sb(st, "f_xn", [128, 8, 512])
        otl = [sb(st, f"f_o{i}", [128, 1024]) for i in range(2)]
        no = 0
        for ti, t0 in enumerate(range(0, SEQ, 512)):
            ht = htf[ti % 2]
            P.dma("sp", ht[:], hT.rearrange("(c p) t -> p c t", p=128)[:, :, t0:t0 + 512])
            P.I("act", "activation", out=sqf[:], in_=ht[:], func=AF.Square)
            for c in range(8):
                P.mm(ps[0][:], ones[:], sqf[:, c, :], start=(c == 0), stop=(c == 7))
            P.I("act", "activation", out=rsf[:], in_=ps[0][:], func=AF.Sqrt, scale=1.0 / D, bias=EPS)
            P.I("dve", "reciprocal", out=rsf[:], in_=rsf[:])
            for c in range(8):
                P.I("dve", "scalar_tensor_tensor", out=xnf[:, c, :], in0=ht[:, c, :], scalar=vv[:, V_FNG + c:V_FNG + c + 1],
                    in1=rsf[:], op0=ALU.mult, op1=ALU.mult)
            for b in range(4):
                ot = otl[no % 2]
                no += 1
                pa, pb = ps[1 + (b % 2) * 2], ps[2 + (b % 2) * 2]
                for c in range(8):
                    pp = pa if c < 4 else pb
                    P.tr(pp[:, (c % 4) * 128:(c % 4 + 1) * 128], xnf[:, c, b * 128:(b + 1) * 128], ident[:])
                P.I("act", "activation", out=ot[:, 0:512], in_=pa[:], func=AF.Copy)
                P.I("dve", "tensor_copy", out=ot[:, 512:1024], in_=pb[:])
                P.dma("sp", out_dram[t0 + b * 128:t0 + (b + 1) * 128, :], ot[:])
    return finish(nc, P, out_dram)


def finish(nc, P, out_dram):
    P.barrier(engines=["sp"])
    return nc


def _pcol(v):
    v = np.asarray(v, np.float32)
    return np.ascontiguousarray(v.reshape(-1, 128).T)


def _consts():
    i = np.arange(128)
    same = (i[:, None] // 64) == (i[None, :] // 64)
    ident = np.eye(128, dtype=np.float32)
    U = (same & (i[:, None] <= i[None, :])).astype(np.float32)
    Lo = (same & (i[:, None] >= i[None, :])).astype(np.float32)
    SU = (same & (i[:, None] < i[None, :])).astype(np.float32)
    SLo = (same & (i[:, None] > i[None, :])).astype(np.float32)
    SC0 = np.repeat((i < 64).astype(np.float32)[:, None], 128, axis=1)
    SC1 = np.repeat((i >= 64).astype(np.float32)[:, None], 128, axis=1)
    return np.ascontiguousarray(np.concatenate([ident, U, Lo, SU, SLo, SC0, SC1], axis=1))


def _rope():
    t = np.arange(SEQ)
    row = (t // 64).astype(np.float32)
    col = (t % 64).astype(np.float32)
    inv = (np.float32(10000.0) ** (-np.arange(0, 32, 2, dtype=np.float32) / np.float32(32))).astype(np.float32)
    out = np.zeros((3, 128, NT), np.float32)
    out[0, :, SEQ:] = 1.0
    for r in range(64):
        a, within = r // 32, r % 32
        half, i = within // 16, within % 16
        ang = ((row if a == 0 else col) * inv[i]).astype(np.float32)
        out[0, r, :SEQ] = np.cos(ang)
        out[1, r, :SEQ] = np.sin(ang) * (-1.0 if half == 0 else 1.0)
        partner = r + 16 if half == 0 else r - 16
        out[2, partner, r] = 1.0
        out[2, 64 + partner, 64 + r] = 1.0
    out[0, 64:128] = out[0, 0:64]
    out[1, 64:128] = out[1, 0:64]
    return out


def _vecs(inp, l):
    v = np.zeros((128, NV), np.float32)
    v[:, V_ADAB:V_ADAB + 48] = _pcol(inp["ada_b"][l])
    v[:, V_N1:V_N1 + 8] = _pcol(inp["norm1_g"][l])
    v[:, V_N2:V_N2 + 8] = _pcol(inp["norm2_g"][l])
    v[:, V_BG:V_BG + 24] = _pcol(inp["b_branch_gate"][l])
    for tap in range(3):
        v[:, V_DNCW + tap * 12:V_DNCW + tap * 12 + 12] = _pcol(inp["dn_conv_w"][l][tap])
        v[:, V_SCCW + tap * 4:V_SCCW + tap * 4 + 4] = _pcol(inp["sc_conv_w"][l][tap])
    v[:, V_QNG:V_QNG + 2] = _pcol(inp["mla_q_norm_g"][l])
    v[:, V_KVNG:V_KVNG + 1] = _pcol(inp["mla_kv_norm_g"][l])
    v[:, V_DNG:V_DNG + 1] = _pcol(inp["dn_norm_g"][l])
    v[:, V_ALOG:V_ALOG + 8] = np.asarray(inp["dn_a_log"][l], np.float32).reshape(1, 8)
    v[:, V_DTB:V_DTB + 8] = np.asarray(inp["dn_dt_bias"][l], np.float32).reshape(1, 8)
    v[:, V_RB:V_RB + 32] = np.asarray(inp["router_b"][l], np.float32).reshape(1, 32)
    b1 = np.asarray(inp["expert_b1"][l], np.float32)
    for e in range(NE):
        v[:, V_B1G + e * 8:V_B1G + e * 8 + 8] = _pcol(b1[e, 0::2])
        v[:, V_B1L + e * 8:V_B1L + e * 8 + 8] = _pcol(b1[e, 1::2])
    v[:, V_FNG:V_FNG + 8] = _pcol(inp["final_norm_g"])
    return v


def make_in_maps(inp, names, ne=NE):
    f = lambda a: np.ascontiguousarray(np.asarray(a, np.float32))
    shared = {}
    shared["consts"] = _consts()
    shared["vecs"] = np.stack([_vecs(inp, l) for l in range(DEPTH)])
    shared["rope"] = _rope()
    for k in ["ada_w", "w_in", "mla_w_qb", "mla_w_kvb", "w_branch_gate", "w_branch_dn", "w_branch_sc", "w_branch_mla",
              "w_out", "router_w", "expert_b2"]:
        shared[k] = f(inp[k])
    for k in ["expert_w1", "expert_w2"]:
        if k in names:
            shared[k] = f(np.asarray(inp[k])[:, :ne])
    maps = []
    for b in range(8):
        m = dict(shared)
        m["x"] = f(inp["x"][b])
        m["ctx"] = f(inp["ctx"][b])
        m["cvec"] = np.ascontiguousarray(np.concatenate([_pcol(inp["c"][b]), _pcol(inp["c_ctx"])], axis=1))
        maps.append({k: v for k, v in m.items() if k in names})
    return maps


INPUT_NAMES = ["x", "ctx", "cvec", "consts", "vecs", "ada_w", "w_in", "mla_w_qb", "mla_w_kvb", "rope",
               "w_branch_gate", "w_branch_dn", "w_branch_sc", "w_branch_mla", "w_out", "router_w",
               "expert_w1", "expert_w2", "expert_b2"]


def kernel(**inputs):
    nc = build()
    maps = make_in_maps(inputs, INPUT_NAMES)
    res = run_bass_kernel_spmd(nc, maps, core_ids=list(range(8)))
    return np.stack([np.asarray(r["out"], np.float32) for r in res.results], axis=0)
```
